# Optimizing a Trainium2 kernel written in Bass

```python
import numpy as np
import jax, jax.numpy as jnp
from jax import lax

D_MODEL = 2048
BATCH = 4
SEQ = 2048
DEPTH = 2

GRID_W = 64
CTX_LEN = 256
D_BRANCH = D_MODEL // 2
N_BRANCHES = 3
HEAD_DIM = 128
N_HEADS = D_BRANCH // HEAD_DIM
NA_KH_MAX = 8
NA_KW = 16
ROPE_THETA = 10000.0
ROPE_AXIS_DIM = HEAD_DIM // 2
CONV_WIDTH = 3
CHUNK = 128
GMLP_GROUP_DIM = 128
GMLP_GROUPS = D_BRANCH // GMLP_GROUP_DIM
N_EXPERTS = 16
CAPACITY_FACTOR = 2
EXPERT_FF = D_MODEL
NORM_EPS = 1e-6
NEG_INF = -1e30
PROJ_SIZES = (D_BRANCH,) * 8 + (N_BRANCHES * D_MODEL,)
PROJ_SPLITS = tuple(int(s) for s in np.cumsum(PROJ_SIZES[:-1]))
D_PROJ = sum(PROJ_SIZES)

kernel_name = 'hybrid_na_conv_gmlp_ec_moe_prefix_dit'


def rms_norm(x, g):
    xf = x.astype(jnp.float32)
    xf = xf * lax.rsqrt(jnp.mean(xf * xf, axis=-1, keepdims=True) + NORM_EPS)
    return (xf * g.astype(jnp.float32)).astype(x.dtype)


def layer_norm(x, g):
    xf = x.astype(jnp.float32)
    mu = jnp.mean(xf, axis=-1, keepdims=True)
    var = jnp.mean(jnp.square(xf - mu), axis=-1, keepdims=True)
    return ((xf - mu) * lax.rsqrt(var + NORM_EPS) * g.astype(jnp.float32)).astype(x.dtype)


def modulate(x, g, shift, scale):
    return rms_norm(x, g) * (1 + scale) + shift


def split_heads(t):
    b, n, _ = t.shape
    return t.reshape(b, n, N_HEADS, HEAD_DIM).transpose(0, 2, 1, 3)


def merge_heads(t):
    b, h, n, hd = t.shape
    return t.transpose(0, 2, 1, 3).reshape(b, n, h * hd)


def axial_rope(t):
    b, h, n, hd = t.shape
    pos = jnp.arange(n)
    rc = jnp.stack([pos // GRID_W, pos % GRID_W], axis=-1).astype(jnp.float32)
    inv = ROPE_THETA ** (-jnp.arange(0, ROPE_AXIS_DIM, 2, dtype=jnp.float32) / ROPE_AXIS_DIM)
    ang = rc[:, :, None] * inv
    cos, sin = jnp.cos(ang), jnp.sin(ang)
    tf = t.astype(jnp.float32).reshape(b, h, n, 2, 2, ROPE_AXIS_DIM // 2)
    t1, t2 = tf[..., 0, :], tf[..., 1, :]
    out = jnp.stack([t1 * cos - t2 * sin, t1 * sin + t2 * cos], axis=-2)
    return out.reshape(b, h, n, hd).astype(t.dtype)


def dense_attention(q, k, v):
    s = jnp.einsum('bhqd,bhkd->bhqk', q, k).astype(jnp.float32) * (q.shape[-1] ** -0.5)
    p = jax.nn.softmax(s, axis=-1).astype(v.dtype)
    return jnp.einsum('bhqk,bhkd->bhqd', p, v)


def neighbourhood_attention(q, k, v, k_ctx, v_ctx, rpb):
    b, h, n, hd = q.shape
    rows = n // GRID_W
    kh = min(NA_KH_MAX, rows)
    n_cb = GRID_W // NA_KW
    kb = 2 * NA_KW
    scale = hd ** -0.5
    q_col = np.arange(GRID_W).reshape(n_cb, NA_KW)
    blk_start = np.clip(np.arange(n_cb) * NA_KW - NA_KW // 2, 0, GRID_W - kb)
    k_col = blk_start[:, None] + np.arange(kb)
    win_start = np.clip(q_col - NA_KW // 2, 0, GRID_W - NA_KW)
    col_ok = (k_col[:, None, :] >= win_start[..., None]) & (k_col[:, None, :] < win_start[..., None] + NA_KW)
    col_ok = np.broadcast_to(col_ok[:, :, None, :], (n_cb, NA_KW, kh, kb)).reshape(n_cb, NA_KW, kh * kb)
    dc_idx = np.clip(k_col[:, None, :] - q_col[..., None] + NA_KW - 1, 0, 2 * NA_KW - 2)
    qg = q.reshape(b, h, rows, n_cb, NA_KW, hd)
    kg = k.reshape(b, h, rows, GRID_W, hd)
    vg = v.reshape(b, h, rows, GRID_W, hd)

    def row_block(r):
        rs = jnp.clip(r - kh // 2, 0, rows - kh)

        def gather(t):
            band = lax.dynamic_slice_in_dim(t, rs, kh, axis=2)
            blk = band[:, :, :, k_col]
            return blk.transpose(0, 1, 3, 2, 4, 5).reshape(b, h, n_cb, kh * kb, hd)

        kblk, vblk = gather(kg), gather(vg)
        qr = lax.dynamic_index_in_dim(qg, r, axis=2, keepdims=False)
        dr_idx = rs + jnp.arange(kh) - r + NA_KH_MAX - 1
        bias = rpb[:, dr_idx[None, None, :, None], dc_idx[:, :, None, :]]
        bias = jnp.where(col_ok, bias.reshape(h, n_cb, NA_KW, kh * kb).astype(jnp.float32), NEG_INF)
        s_loc = jnp.einsum('bhcqd,bhckd->bhcqk', qr, kblk).astype(jnp.float32) * scale + bias
        s_ctx = jnp.einsum('bhcqd,bhld->bhcql', qr, k_ctx).astype(jnp.float32) * scale
        p = jax.nn.softmax(jnp.concatenate([s_loc, s_ctx], axis=-1), axis=-1).astype(v.dtype)
        return (jnp.einsum('bhcqk,bhckd->bhcqd', p[..., :kh * kb], vblk)
                + jnp.einsum('bhcql,bhld->bhcqd', p[..., kh * kb:], v_ctx))

    out = lax.map(row_block, jnp.arange(rows))
    return out.transpose(1, 2, 0, 3, 4, 5).reshape(b, h, n, hd)


def short_conv(xc, bg, cg, w):
    z = cg * xc
    zp = jnp.pad(z, ((0, 0), (1, 1), (0, 0)))
    y = zp[:, :-2] * w[0] + zp[:, 1:-1] * w[1] + zp[:, 2:] * w[2]
    return bg * y


def chunk_gmlp(u, v, ln_g, w_s, b_s):
    b, n, _ = u.shape
    u = jax.nn.gelu(u)
    v = layer_norm(jax.nn.gelu(v), ln_g)
    vc = v.reshape(b, n // CHUNK, CHUNK, GMLP_GROUPS, GMLP_GROUP_DIM)
    s = jnp.einsum('gpq,bnqgc->bnpgc', w_s, vc) + b_s.T[None, None, :, :, None]
    return u * s.reshape(b, n, D_BRANCH)


def merge_branches(attn_o, parts, conv_w, ln_g, w_s, b_s, w_branch, w_out):
    _, _, _, xc, bg, cg, u, v, gate_logits = parts
    conv_o = short_conv(xc, bg, cg, conv_w)
    gmlp_o = chunk_gmlp(u, v, ln_g, w_s, b_s)
    b, n, _ = gate_logits.shape
    gates = jax.nn.sigmoid(gate_logits.astype(jnp.float32)).astype(attn_o.dtype).reshape(b, n, N_BRANCHES, D_MODEL)
    br = jnp.stack([attn_o, conv_o, gmlp_o], axis=2)
    proj = jnp.einsum('bnic,icd->bnid', br, w_branch)
    return jnp.sum(gates * proj, axis=2) @ w_out


def expert_choice_moe(h, w_router, b_router, w_gate, w_up, w_down):
    b, n, _ = h.shape
    cap = CAPACITY_FACTOR * n // N_EXPERTS
    logits = (h @ w_router).astype(jnp.float32) + b_router.astype(jnp.float32)
    aff = jax.nn.softmax(logits, axis=-1)
    gate, idx = lax.top_k(jnp.swapaxes(aff, 1, 2), cap)
    bidx = jnp.arange(b)[:, None, None]
    xs = h[bidx, idx]
    hg = jnp.einsum('becd,edf->becf', xs, w_gate)
    hu = jnp.einsum('becd,edf->becf', xs, w_up)
    y = jnp.einsum('becf,efd->becd', jax.nn.silu(hg) * hu, w_down)
    y = y * gate[..., None].astype(y.dtype)
    return jnp.zeros_like(h).at[bidx, idx].add(y)


def project(h, w_in):
    q, k, v, xc, bg, cg, u, vs, g = jnp.split(h @ w_in, PROJ_SPLITS, axis=-1)
    return (split_heads(q), split_heads(k), split_heads(v), xc, bg, cg, u, vs, g)


def setup_inputs(seed: int = 0) -> dict:
    key = jax.random.key(seed)
    ks = jax.random.split(key, 24)

    def nrm(k, shape, scale):
        return jax.random.normal(k, shape, dtype=jnp.float32) * scale

    L, D, E, F = DEPTH, D_MODEL, N_EXPERTS, EXPERT_FF
    return {
        'x': nrm(ks[0], (BATCH, SEQ, D), 1.0),
        'c': nrm(ks[1], (BATCH, D), 1.0),
        'ctx': nrm(ks[2], (BATCH, CTX_LEN, D), 1.0),
        'c_ctx': nrm(ks[3], (D,), 1.0),
        'w_mod': nrm(ks[4], (L, D, 6 * D), D ** -0.5),
        'b_mod': nrm(ks[5], (L, 6 * D), 0.02),
        'norm1_g': 1.0 + nrm(ks[6], (L, D), 0.02),
        'w_in': nrm(ks[7], (L, D, D_PROJ), D ** -0.5),
        'na_rpb': nrm(ks[8], (L, N_HEADS, 2 * NA_KH_MAX - 1, 2 * NA_KW - 1), 0.1),
        'conv_w': nrm(ks[9], (L, CONV_WIDTH, D_BRANCH), CONV_WIDTH ** -0.5),
        'gmlp_ln_g': 1.0 + nrm(ks[10], (L, D_BRANCH), 0.02),
        'w_spatial': nrm(ks[11], (L, GMLP_GROUPS, CHUNK, CHUNK), CHUNK ** -0.5),
        'b_spatial': 1.0 + nrm(ks[12], (L, GMLP_GROUPS, CHUNK), 0.02),
        'w_branch': nrm(ks[13], (L, N_BRANCHES, D_BRANCH, D), D_BRANCH ** -0.5),
        'w_out': nrm(ks[14], (L, D, D), D ** -0.5),
        'norm2_g': 1.0 + nrm(ks[15], (L, D), 0.02),
        'w_router': nrm(ks[16], (L, D, E), D ** -0.5),
        'b_router': nrm(ks[17], (L, E), 0.01),
        'w_e_gate': nrm(ks[18], (L, E, D, F), D ** -0.5),
        'w_e_up': nrm(ks[19], (L, E, D, F), D ** -0.5),
        'w_e_down': nrm(ks[20], (L, E, F, D), F ** -0.5),
        'final_g': 1.0 + nrm(ks[21], (D,), 0.02),
    }


def reference(x, c, ctx, c_ctx, w_mod, b_mod, norm1_g, w_in, na_rpb, conv_w, gmlp_ln_g,
              w_spatial, b_spatial, w_branch, w_out, norm2_g, w_router, b_router,
              w_e_gate, w_e_up, w_e_down, final_g):
    b = x.shape[0]
    ctx_s = ctx
    for layer in range(DEPTH):
        last = layer == DEPTH - 1
        mod = (jax.nn.silu(c) @ w_mod[layer] + b_mod[layer]).reshape(b, 6, D_MODEL)[:, :, None, :]
        mod_c = (jax.nn.silu(c_ctx) @ w_mod[layer] + b_mod[layer]).reshape(6, D_MODEL)
        mix_args = (conv_w[layer], gmlp_ln_g[layer], w_spatial[layer], b_spatial[layer],
                    w_branch[layer], w_out[layer])
        moe_args = (w_router[layer], b_router[layer], w_e_gate[layer], w_e_up[layer], w_e_down[layer])

        hc = modulate(ctx_s, norm1_g[layer], mod_c[0], mod_c[1])
        if last:
            kv_c = hc @ w_in[layer][:, D_BRANCH:3 * D_BRANCH]
            k_c, v_c = split_heads(kv_c[..., :D_BRANCH]), split_heads(kv_c[..., D_BRANCH:])
        else:
            parts_c = project(hc, w_in[layer])
            q_c, k_c, v_c = parts_c[0], parts_c[1], parts_c[2]
            attn_c = merge_heads(dense_attention(q_c, k_c, v_c))
            ctx_new = ctx_s + mod_c[2] * merge_branches(attn_c, parts_c, *mix_args)
            hc2 = modulate(ctx_new, norm2_g[layer], mod_c[3], mod_c[4])
            ctx_new = ctx_new + mod_c[5] * expert_choice_moe(hc2, *moe_args)

        h = modulate(x, norm1_g[layer], mod[:, 0], mod[:, 1])
        parts = project(h, w_in[layer])
        q, k, v = axial_rope(parts[0]), axial_rope(parts[1]), parts[2]
        attn = merge_heads(neighbourhood_attention(q, k, v, k_c, v_c, na_rpb[layer]))
        x = x + mod[:, 2] * merge_branches(attn, parts, *mix_args)
        h2 = modulate(x, norm2_g[layer], mod[:, 3], mod[:, 4])
        x = x + mod[:, 5] * expert_choice_moe(h2, *moe_args)

        if not last:
            ctx_s = ctx_new
    return rms_norm(x, final_g)
```

```python
import numpy as np
from contextlib import ExitStack
from collections import defaultdict
import concourse.bass as bass
import concourse.mybir as mybir
from concourse.bass_utils import run_bass_kernel_spmd

F32 = mybir.dt.float32
BF16 = mybir.dt.bfloat16
AF = mybir.ActivationFunctionType
ALU = mybir.AluOpType
AX = mybir.AxisListType

D = 2048
L = 2
SEQ = 2048
CTX = 256
T = SEQ + CTX
NT = T // 128
DB = 1024
DPROJ = 14336
NE = 16
CAP = 256
CAPC = 32
NS = CAP + CAPC
EPS = 1e-6
NEG = -30000.0
TGS = [(0, 512), (512, 512), (1024, 512), (1536, 512), (2048, 256)]


class Res:
    __slots__ = ("name", "last_w", "readers")

    def __init__(self, name=""):
        self.name = name
        self.last_w = None
        self.readers = {}


class Op:
    __slots__ = ("eng", "fn", "deps", "needed", "token", "dma", "has_reads")


BLK = {"pe": "tensor", "act": "scalar", "dve": "vector", "pool": "gpsimd", "sp": "sync"}


class Stage:
    def __init__(self, nc, name):
        self.nc = nc
        self.name = name
        self.ops = []
        self.es = ExitStack()
        self._n = 0
        self.touched = []
        self.lag = 0

    def sb(self, shape, dtype, nm="t"):
        self._n += 1
        return self.es.enter_context(self.nc.sbuf_tensor(f"{self.name}_{nm}{self._n}", list(shape), dtype))

    def ps(self, shape, dtype=F32, nm="p"):
        self._n += 1
        return self.es.enter_context(self.nc.psum_tensor(f"{self.name}_{nm}{self._n}", list(shape), dtype))

    def op(self, eng, fn, reads=(), writes=(), dma=None):
        o = Op()
        o.eng = eng
        o.fn = fn
        o.dma = dma
        o.needed = False
        o.token = None
        o.has_reads = len(reads) > 0
        deps = {}
        self.touched.extend(reads)
        self.touched.extend(writes)
        for r in reads:
            if r.last_w is not None:
                deps[id(r.last_w)] = r.last_w
        for w in writes:
            if w.last_w is not None:
                deps[id(w.last_w)] = w.last_w
            for rd in w.readers.values():
                deps[id(rd)] = rd
        key = eng if dma is None else ("dma", dma)
        for r in reads:
            r.readers[key] = o
        for w in writes:
            w.last_w = o
            w.readers = {}
        o.deps = [d for d in deps.values()
                  if d is not o and not (d.eng == "pe" and eng == "pe" and d.dma is None and dma is None)]
        for d in o.deps:
            d.needed = True
        self.ops.append(o)
        return o

    def run(self):
        nc = self.nc
        cnt = defaultdict(int)
        for o in self.ops:
            if o.dma is not None:
                k = ("dma", o.dma)
                cnt[k] += 16
                o.token = (k, cnt[k])
            elif o.needed:
                k = ("eng", o.eng)
                cnt[k] += 1
                o.token = (k, cnt[k])
        sems = {}
        handles = []
        for i, k in enumerate(cnt):
            sems[k] = nc.alloc_semaphore(f"{self.name}_s{i}")
            handles.append(sems[k])
        with nc.Block() as blk:
            for eng, bname in BLK.items():
                ops_e = [o for o in self.ops if o.eng == eng]
                if not ops_e:
                    continue
                if eng == "sp" and self.lag > 0:
                    outl, pend = [], []
                    for o in ops_e:
                        if o.dma is not None and o.has_reads:
                            pend.append([o, 0])
                        else:
                            outl.append(o)
                            for pp_ in pend:
                                pp_[1] += 1
                            while pend and pend[0][1] >= self.lag:
                                outl.append(pend.pop(0)[0])
                    outl.extend(pp_[0] for pp_ in pend)
                    ops_e = outl

                def body(e, ops_e=ops_e):
                    waited = {}
                    fin = {}
                    for o in ops_e:
                        need = {}
                        for d in o.deps:
                            k, v = d.token
                            if need.get(k, 0) < v:
                                need[k] = v
                        for k, v in need.items():
                            if waited.get(k, 0) < v:
                                e.wait_ge(sems[k], v)
                                waited[k] = v
                        ins = o.fn(e)
                        if o.token is not None:
                            ins.then_inc(sems[o.token[0]], 16 if o.dma is not None else 1)
                            if o.dma is not None:
                                fin[o.token[0]] = o.token[1]
                    for k, v in fin.items():
                        if waited.get(k, 0) < v:
                            e.wait_ge(sems[k], v)

                getattr(blk, bname)(body)
        for r in self.touched:
            r.last_w = None
            r.readers = {}
        self.es.close()
        nc.all_engine_barrier()
        nc.clear_and_free_semaphores(handles)
        nc.all_engine_barrier()


def _dma(out, in_):
    return lambda e: e.dma_start(out=out, in_=in_)


def build(nc, dbg=None, nlayers=L, Lw=L, NEw=NE):
    dbg = dbg or set()

    def din(name, shape, dt=F32):
        return nc.dram_tensor(name, list(shape), dt, kind="ExternalInput").ap()

    def dscr(name, shape, dt):
        kind = "ExternalOutput" if name in dbg else "Internal"
        return nc.dram_tensor(name, list(shape), dt, kind=kind).ap()

    xin = din("xin", [T, D])
    c2T = din("c2T", [128, 16, 2])
    w_mod = din("w_mod", [Lw, D, 6 * D])
    b_mod = din("b_mod", [Lw, 6 * D])
    norm1_g = din("norm1_g", [Lw, D])
    w_in = din("w_in", [Lw, D, DPROJ])
    rpb_tab = din("rpb_tab", [Lw, 8, 128, 5, 896])
    conv_wT = din("conv_wT", [Lw, 128, 8, 3])
    ln_g = din("gmlp_ln_g", [Lw, DB])
    wsT = din("wsT", [Lw, 128, 8, 128])
    b_sp = din("b_spatial", [Lw, DB])
    w_branch = din("w_branch", [Lw, 3, DB, D])
    w_out = din("w_out", [Lw, D, D])
    norm2_g = din("norm2_g", [Lw, D])
    w_router = din("w_router", [Lw, D, NE])
    b_router = din("b_router", [Lw, NE])
    w_eg = din("w_e_gate", [Lw, NEw, D, D])
    w_eu = din("w_e_up", [Lw, NEw, D, D])
    w_ed = din("w_e_down", [Lw, NEw, D, D])
    final_g = din("final_g", [1, D])
    ident_d = din("ident", [128, 128])
    pm_d = din("pm", [128, 128])
    cos_d = din("cos", [128, SEQ])
    sin_d = din("sin", [128, SEQ])
    sel_d = din("selT", [16, 16, 128])
    iota_d = din("iota_s", [128, 256])
    iop_d = din("iop", [128, 2])
    out = nc.dram_tensor("out", [SEQ, D], F32, kind="ExternalOutput").ap()

    MOD = dscr("MOD", [2, 6 * D], F32)
    PROJ = dscr("PROJ", [DPROJ, T], BF16)
    V_tm = dscr("V_tm", [T, DB], BF16)
    VS_tm = dscr("VS_tm", [T, DB], BF16)
    BR = dscr("BR", [3, DB, T], BF16)
    MERGED = dscr("MERGED", [D, T], BF16)
    XA = dscr("XA", [T, D], F32)
    XB = dscr("XB", [T, D], F32)
    H2 = dscr("H2", [T, D], BF16)
    GT = dscr("GT", [NT, 128, NE, 2, 128], BF16)
    XS = dscr("XS", [NE, D, NS], BF16)
    Y = dscr("Y", [NE, CAP, D], BF16)
    YC = dscr("YC", [NE, CAPC, D], BF16)
    DBG_AFF = dscr("DBG_AFF", [128, NT, NE], F32)
    DBG_HT = dscr("DBG_HT", [128, 16, T], BF16)

    top = ExitStack()

    def tsb(name, shape, dt):
        return top.enter_context(nc.sbuf_tensor(name, list(shape), dt))

    ident_f = tsb("ident_f", [128, 128], F32)
    ident_b = tsb("ident_b", [128, 128], BF16)
    ones_b = tsb("ones_b", [128, 128], BF16)
    AFF = tsb("AFF", [128, NT, NE], F32)
    POS = tsb("POS", [128, NT, NE], F32)
    MSK = tsb("MSK", [128, NT, NE], F32)

    st = Stage(nc, "c0")
    r_if, r_ib, r_ob = Res(), Res(), Res()
    st.op("sp", _dma(ident_f[:], ident_d), writes=[r_if], dma="a")
    st.op("dve", lambda e: e.tensor_copy(ident_b[:], ident_f[:]), reads=[r_if], writes=[r_ib])
    st.op("dve", lambda e: e.memset(ones_b[:], 1.0), writes=[r_ob])
    st.run()

    def stage_mod(l):
        st = Stage(nc, f"mod{l}")
        cT = st.sb([128, 16, 2], F32)
        sc = st.sb([128, 16, 2], BF16)
        b2 = st.sb([2, 6 * D], F32)
        msb = st.sb([2, 6 * D], F32)
        ws = [st.sb([128, 16, 512], BF16, "w") for _ in range(3)]
        rw = [Res() for _ in range(3)]
        pss = [st.ps([2, 512]) for _ in range(2)]
        rps = [Res() for _ in range(2)]
        r_c, r_sc, r_b2, r_m = Res(), Res(), Res(), Res()
        st.op("sp", _dma(cT[:], c2T), writes=[r_c], dma="c")
        st.op("sp", _dma(b2[:], b_mod[l:l + 1, :].partition_broadcast(2)), writes=[r_b2], dma="b")
        st.op("act", lambda e: e.activation(sc[:], cT[:], AF.Silu), reads=[r_c], writes=[r_sc])
        NG = 24

        def ld(g):
            st.op("pool", _dma(ws[g % 3][:], w_mod[l, :, g * 512:(g + 1) * 512].rearrange("(kc p) c -> p kc c", p=128)),
                  writes=[rw[g % 3]], dma=f"w{g % 3}")

        ld(0)
        ld(1)
        for g in range(NG):
            if g + 2 < NG:
                ld(g + 2)
            w = ws[g % 3]
            p = pss[g % 2]

            def mm(e, w=w, p=p):
                for kc in range(16):
                    ins = e.matmul(p[:], sc[:, kc, :], w[:, kc, :], start=(kc == 0), stop=(kc == 15))
                return ins

            st.op("pe", mm, reads=[rw[g % 3], r_sc], writes=[rps[g % 2]])
            st.op("dve", lambda e, p=p, g=g: e.tensor_tensor(msb[:, g * 512:(g + 1) * 512], p[:], b2[:, g * 512:(g + 1) * 512], ALU.add),
                  reads=[rps[g % 2], r_b2], writes=[r_m])
        st.op("sp", _dma(MOD, msb[:]), reads=[r_m], dma="o")
        st.run()

    def load_bc(st, dst_ap, src_row_ap, res, key):
        st.op("sp", _dma(dst_ap, src_row_ap.partition_broadcast(128)), writes=[res], dma=key)

    def stage_norm(l, which, Xsrc, hT, r_hT):
        st = Stage(nc, f"n{which}_{l}")
        st.lag = 1
        gsrc = norm1_g if which == 1 else norm2_g
        jo = 0 if which == 1 else 3
        gm = st.sb([128, 2, D], F32)
        sh = st.sb([128, 2, D], F32)
        gb = st.sb([128, D], F32)
        r_gm, r_sh, r_gb = Res(), Res(), Res()
        load_bc(st, gb[:], gsrc[l:l + 1, :], r_gb, "g")
        for r in range(2):
            load_bc(st, gm[:, r, :], MOD[r:r + 1, (jo + 1) * D:(jo + 2) * D], r_gm, f"gm{r}")
            load_bc(st, sh[:, r, :], MOD[r:r + 1, jo * D:(jo + 1) * D], r_sh, f"sh{r}")
            st.op("dve", lambda e, r=r: e.scalar_tensor_tensor(gm[:, r, :], gm[:, r, :], 1.0, gb[:], ALU.add, ALU.mult),
                  reads=[r_gm, r_gb], writes=[r_gm])
        xt = [st.sb([128, D], F32, "x") for _ in range(2)]
        rx = [Res() for _ in range(2)]
        junk = st.sb([128, D], BF16)
        r_junk = Res()
        ss = [st.sb([128, 4], F32, "ss") for _ in range(2)]
        rss = [Res() for _ in range(2)]
        tt = [st.sb([128, D], F32, "tt") for _ in range(2)]
        rtt = [Res() for _ in range(2)]
        if which == 1:
            hb = [st.sb([128, D], BF16, "hb") for _ in range(2)]
            rhb = [Res() for _ in range(2)]
            pst = [st.ps([128, 8, 128], BF16) for _ in range(4)]
            rpst = [Res() for _ in range(4)]
        else:
            hb = [st.sb([128, D], BF16, "hb") for _ in range(2)]
            rhb = [Res() for _ in range(2)]
            pst = [st.ps([128, 4, 128], F32) for _ in range(4)]
            rpst = [Res() for _ in range(4)]
            h2T = [st.sb([128, 16, 128], F32, "h2T") for _ in range(2)]
            rh2T = [Res() for _ in range(2)]
            wr = st.sb([128, 16, NE], F32)
            r_wr = Res()
            st.op("sp", _dma(wr[:], w_router[l].rearrange("(kc p) e -> p kc e", p=128)), writes=[r_wr], dma="wr")
            brb = st.sb([128, NE], F32)
            r_brb = Res()
            load_bc(st, brb[:], b_router[l:l + 1, :], r_brb, "brb")
            pl = [st.ps([128, NE], F32) for _ in range(2)]
            rpl = [Res() for _ in range(2)]
            lg = [st.sb([128, NE], F32, "lg") for _ in range(2)]
            rlg = [Res() for _ in range(2)]
            sm = [st.sb([128, 4], F32, "sm") for _ in range(2)]
            rsm = [Res() for _ in range(2)]
            r_aff = Res()
        for i in range(NT):
            s = i % 2
            r = 0 if i < 16 else 1
            x_, ss_, t_, h_ = xt[s], ss[s], tt[s], hb[s]
            st.op("sp", _dma(x_[:], Xsrc[i * 128:(i + 1) * 128, :]), writes=[rx[s]], dma=f"x{s}")
            st.op("act", lambda e, x_=x_, ss_=ss_: e.activation(junk[:], x_[:], AF.Square, accum_out=ss_[:, 0:1]),
                  reads=[rx[s]], writes=[r_junk, rss[s]])
            st.op("act", lambda e, ss_=ss_: e.activation(ss_[:, 1:2], ss_[:, 0:1], AF.Sqrt, bias=EPS, scale=1.0 / D),
                  reads=[rss[s]], writes=[rss[s]])
            st.op("dve", lambda e, ss_=ss_: e.reciprocal(ss_[:, 2:3], ss_[:, 1:2]), reads=[rss[s]], writes=[rss[s]])
            st.op("dve", lambda e, x_=x_, ss_=ss_, t_=t_, r=r: e.scalar_tensor_tensor(t_[:], x_[:], ss_[:, 2:3], gm[:, r, :], ALU.mult, ALU.mult),
                  reads=[rx[s], rss[s], r_gm], writes=[rtt[s]])
            if which == 1:
                st.op("pool", lambda e, t_=t_, h_=h_, r=r: e.tensor_tensor(h_[:], t_[:], sh[:, r, :], ALU.add),
                      reads=[rtt[s], r_sh], writes=[rhb[s]])
                for half in range(2):
                    pp = pst[2 * s + half]
                    rp = rpst[2 * s + half]

                    def tr(e, pp=pp, h_=h_, half=half):
                        for j in range(8):
                            kc = half * 8 + j
                            ins = e.transpose(pp[:, j, :], h_[:, kc * 128:(kc + 1) * 128], ident_b[:])
                        return ins

                    st.op("pe", tr, reads=[rhb[s]], writes=[rp])
                    eng = "act" if half == 0 else "dve"
                    if eng == "act":
                        st.op("act", lambda e, pp=pp, half=half, i=i: e.copy(hT[:, half * 8:(half + 1) * 8, i * 128:(i + 1) * 128], pp[:]),
                              reads=[rp], writes=[r_hT])
                    else:
                        st.op("dve", lambda e, pp=pp, half=half, i=i: e.tensor_copy(hT[:, half * 8:(half + 1) * 8, i * 128:(i + 1) * 128], pp[:]),
                              reads=[rp], writes=[r_hT])
            else:
                st.op("pool", lambda e, t_=t_, r=r: e.tensor_tensor(t_[:], t_[:], sh[:, r, :], ALU.add),
                      reads=[rtt[s], r_sh], writes=[rtt[s]])
                st.op("pool", lambda e, t_=t_, h_=h_: e.tensor_copy(h_[:], t_[:]), reads=[rtt[s]], writes=[rhb[s]])
                st.op("sp", _dma(H2[i * 128:(i + 1) * 128, :], h_[:]), reads=[rhb[s]], dma=f"h{s}")
                hT_ = h2T[s]
                for q in range(4):
                    pp = pst[q]

                    def tr(e, pp=pp, t_=t_, q=q):
                        for j in range(4):
                            kc = q * 4 + j
                            ins = e.transpose(pp[:, j, :], t_[:, kc * 128:(kc + 1) * 128], ident_f[:])
                        return ins

                    st.op("pe", tr, reads=[rtt[s]], writes=[rpst[q]])
                    if q % 2 == 0:
                        st.op("act", lambda e, pp=pp, q=q, hT_=hT_: e.copy(hT_[:, q * 4:(q + 1) * 4, :], pp[:]),
                              reads=[rpst[q]], writes=[rh2T[s]])
                    else:
                        st.op("dve", lambda e, pp=pp, q=q, hT_=hT_: e.tensor_copy(hT_[:, q * 4:(q + 1) * 4, :], pp[:]),
                              reads=[rpst[q]], writes=[rh2T[s]])
                pl_ = pl[s]

                def mmr(e, pl_=pl_, hT_=hT_):
                    for kc in range(16):
                        ins = e.matmul(pl_[:], hT_[:, kc, :], wr[:, kc, :], start=(kc == 0), stop=(kc == 15))
                    return ins

                st.op("pe", mmr, reads=[rh2T[s], r_wr], writes=[rpl[s]])
                lg_, sm_ = lg[s], sm[s]
                st.op("dve", lambda e, lg_=lg_, pl_=pl_: e.tensor_tensor(lg_[:], pl_[:], brb[:], ALU.add),
                      reads=[rpl[s], r_brb], writes=[rlg[s]])
                st.op("dve", lambda e, lg_=lg_, sm_=sm_: e.tensor_reduce(sm_[:, 0:1], lg_[:], AX.X, ALU.max, negate=True),
                      reads=[rlg[s]], writes=[rsm[s]])
                st.op("act", lambda e, lg_=lg_, sm_=sm_: e.activation(lg_[:], lg_[:], AF.Exp, bias=sm_[:, 0:1], accum_out=sm_[:, 1:2]),
                      reads=[rlg[s], rsm[s]], writes=[rlg[s], rsm[s]])
                st.op("dve", lambda e, sm_=sm_: e.reciprocal(sm_[:, 2:3], sm_[:, 1:2]), reads=[rsm[s]], writes=[rsm[s]])
                st.op("dve", lambda e, lg_=lg_, sm_=sm_, i=i: e.tensor_scalar(AFF[:, i, :], lg_[:], sm_[:, 2:3], None, ALU.mult),
                      reads=[rlg[s], rsm[s]], writes=[r_aff])
        if which == 2 and "DBG_AFF" in dbg:
            st.op("sp", _dma(DBG_AFF, AFF[:]), reads=[r_aff], dma="dbg")
        if which == 1 and "DBG_HT" in dbg:
            st.op("sp", _dma(DBG_HT, hT[:]), reads=[r_hT], dma="dbg")
        st.run()

    def stage_proj(l, hT, r_hT):
        st = Stage(nc, f"pj{l}")
        ws = [st.sb([128, 16, 512], BF16, "w") for _ in range(3)]
        rw = [Res() for _ in range(3)]
        NPS = 6
        pss = [st.ps([128, 512]) for _ in range(NPS)]
        rps = [Res() for _ in range(NPS)]
        NO = 4
        os_ = [st.sb([128, 512], BF16, "o") for _ in range(NO)]
        ros = [Res() for _ in range(NO)]
        NG = getattr(build, 'proj_ng', DPROJ // 512)
        cntr = [0]

        def ld(g):
            st.op("pool", _dma(ws[g % 3][:], w_in[l, :, g * 512:(g + 1) * 512].rearrange("(kc p) c -> p kc c", p=128)),
                  writes=[rw[g % 3]], dma=f"w{g % 3}")

        def evac_store(p, rp, n_part, n_free, dst):
            k = cntr[0]
            cntr[0] += 1
            o = os_[k % NO]
            ro = ros[k % NO]
            if k % 2 == 0:
                st.op("act", lambda e: e.copy(o[:n_part, :n_free], p[:n_part, :n_free]), reads=[rp], writes=[ro])
            else:
                st.op("dve", lambda e: e.tensor_copy(o[:n_part, :n_free], p[:n_part, :n_free]), reads=[rp], writes=[ro])
            st.op("sp", _dma(dst, o[:n_part, :n_free]), reads=[ro], dma=f"o{k % NO}")

        ld(0)
        ld(1)
        pk = 0
        for g in range(NG):
            if g + 2 < NG:
                ld(g + 2)
            w = ws[g % 3]
            tm = g in (4, 5, 14, 15)
            if not tm:
                for (t0, n) in TGS:
                    for cb in range(4):
                        p = pss[pk % NPS]
                        rp = rps[pk % NPS]
                        pk += 1

                        def mm(e, w=w, p=p, cb=cb, t0=t0, n=n):
                            for kc in range(16):
                                ins = e.matmul(p[:, :n], w[:, kc, cb * 128:(cb + 1) * 128], hT[:, kc, t0:t0 + n],
                                               start=(kc == 0), stop=(kc == 15))
                            return ins

                        st.op("pe", mm, reads=[rw[g % 3], r_hT], writes=[rp])
                        row0 = g * 512 + cb * 128
                        evac_store(p, rp, 128, n, PROJ[row0:row0 + 128, t0:t0 + n])
            else:
                dst_t = V_tm if g in (4, 5) else VS_tm
                c0 = (g - 4) * 512 if g in (4, 5) else (g - 14) * 512
                for i in range(NT):
                    p = pss[pk % NPS]
                    rp = rps[pk % NPS]
                    pk += 1

                    def mm(e, w=w, p=p, i=i):
                        for kc in range(16):
                            ins = e.matmul(p[:], hT[:, kc, i * 128:(i + 1) * 128], w[:, kc, :],
                                           start=(kc == 0), stop=(kc == 15))
                        return ins

                    st.op("pe", mm, reads=[rw[g % 3], r_hT], writes=[rp])
                    evac_store(p, rp, 128, 512, dst_t[i * 128:(i + 1) * 128, c0:c0 + 512])
        st.run()

    def stage_rope(l):
        st = Stage(nc, f"rp{l}")
        st.lag = 2
        pmf = st.sb([128, 128], F32)
        pmb = st.sb([128, 128], BF16)
        cs = st.sb([128, SEQ], F32)
        sn = st.sb([128, SEQ], F32)
        r_pmf, r_pmb, r_cs, r_sn = Res(), Res(), Res(), Res()
        st.op("sp", _dma(pmf[:], pm_d), writes=[r_pmf], dma="pm")
        st.op("sp", _dma(cs[:], cos_d), writes=[r_cs], dma="cs")
        st.op("sp", _dma(sn[:], sin_d), writes=[r_sn], dma="sn")
        st.op("dve", lambda e: e.tensor_copy(pmb[:], pmf[:]), reads=[r_pmf], writes=[r_pmb])
        RD = 4
        qb = [st.sb([128, 512], BF16, "q") for _ in range(RD)]
        rq = [Res() for _ in range(RD)]
        pq = [st.ps([128, 512]) for _ in range(RD)]
        rpq = [Res() for _ in range(RD)]
        t1 = [st.sb([128, 512], F32, "t1") for _ in range(RD)]
        rt1 = [Res() for _ in range(RD)]
        t2 = [st.sb([128, 512], F32, "t2") for _ in range(RD)]
        rt2 = [Res() for _ in range(RD)]
        ob = [st.sb([128, 512], BF16, "ob") for _ in range(RD)]
        rob = [Res() for _ in range(RD)]
        k = 0
        for blk in range(16):
            for tg in range(4):
                s = k % RD
                k += 1
                t0 = tg * 512
                src = PROJ[blk * 128:(blk + 1) * 128, t0:t0 + 512]
                q_, p_, a_, b_, o_ = qb[s], pq[s], t1[s], t2[s], ob[s]
                st.op("sp", _dma(q_[:], src), writes=[rq[s]], dma=f"q{s}")
                st.op("pe", lambda e, q_=q_, p_=p_: e.matmul(p_[:], pmb[:], q_[:], start=True, stop=True),
                      reads=[rq[s], r_pmb], writes=[rpq[s]])
                st.op("dve", lambda e, q_=q_, a_=a_, t0=t0: e.tensor_tensor(a_[:], q_[:], cs[:, t0:t0 + 512], ALU.mult),
                      reads=[rq[s], r_cs], writes=[rt1[s]])
                st.op("dve", lambda e, p_=p_, b_=b_, t0=t0: e.tensor_tensor(b_[:], p_[:], sn[:, t0:t0 + 512], ALU.mult),
                      reads=[rpq[s], r_sn], writes=[rt2[s]])
                st.op("pool", lambda e, a_=a_, b_=b_, o_=o_: e.tensor_tensor(o_[:], a_[:], b_[:], ALU.add),
                      reads=[rt1[s], rt2[s]], writes=[rob[s]])
                st.op("sp", _dma(src, o_[:]), reads=[rob[s]], dma=f"o{s}")
        st.run()

    def stage_attn(l):
        st = Stage(nc, f"at{l}")
        st.lag = 0
        scale = 128 ** -0.5
        qT = [st.sb([128, T], BF16, "q") for _ in range(2)]
        kT = [st.sb([128, T], BF16, "k") for _ in range(2)]
        Vh = [st.sb([128, NT, 128], BF16, "v") for _ in range(2)]
        bt = [st.sb([128, 5, 896], F32, "b") for _ in range(2)]
        oT = [st.sb([128, T], BF16, "o") for _ in range(2)]
        rq = [Res() for _ in range(2)]
        rk = [Res() for _ in range(2)]
        rv = [Res() for _ in range(2)]
        rb = [Res() for _ in range(2)]
        ro = [Res() for _ in range(2)]
        psS = [st.ps([128, 1024]) for _ in range(2)]
        rpS = [Res() for _ in range(2)]
        psT = [st.ps([128, 7, 128], BF16) for _ in range(2)]
        rpT = [Res() for _ in range(2)]
        psO = [st.ps([128, 2, 128]) for _ in range(2)]
        rpO = [Res() for _ in range(2)]
        AD = 4
        sbS = [st.sb([128, 896], F32, "s") for _ in range(AD)]
        rsS = [Res() for _ in range(AD)]
        mx = [st.sb([128, 2], F32, "mx") for _ in range(AD)]
        rmx = [Res() for _ in range(AD)]
        pb = [st.sb([128, 896], BF16, "p") for _ in range(AD)]
        rpb_ = [Res() for _ in range(AD)]
        pT = [st.sb([128, 7, 128], BF16, "pT") for _ in range(AD)]
        rpTs = [Res() for _ in range(AD)]
        ri = [st.sb([128, 128], F32, "ri") for _ in range(AD)]
        rri = [Res() for _ in range(AD)]
        units = [(h, qt) for h in range(8) for qt in range(NT)]
        NU = len(units)

        def geom(qt):
            if qt < 16:
                tb = min(max(qt - 2, 0), 11)
                cls = {0: 0, 1: 1, 14: 3, 15: 4}.get(qt, 2)
                segs = [(0, tb * 128, 512), (512, tb * 128 + 512, 128), (640, SEQ, 256)]
                vts = [tb + j for j in range(5)] + [16, 17]
                return segs, vts, 896, cls
            return [(0, SEQ, 256)], [16, 17], 256, None

        def stA(u):
            h, qt = units[u]
            hs = h % 2
            q_, k_, v_, b_ = qT[hs], kT[hs], Vh[hs], bt[hs]
            if qt == 0:
                st.op("sp", _dma(q_[:], PROJ[h * 128:(h + 1) * 128, :]), writes=[rq[hs]], dma=f"q{hs}")
                st.op("sp", _dma(k_[:], PROJ[DB + h * 128:DB + (h + 1) * 128, :]), writes=[rk[hs]], dma=f"k{hs}")
                st.op("sp", _dma(v_[:], V_tm[:, h * 128:(h + 1) * 128].rearrange("(i p) d -> p i d", p=128)),
                      writes=[rv[hs]], dma=f"v{hs}")
                st.op("sp", _dma(b_[:], rpb_tab[l, h]), writes=[rb[hs]], dma=f"b{hs}")
            s, sB = u % 2, u % AD
            S_, sb_, mx_, p_ = psS[s], sbS[sB], mx[sB], pb[sB]
            segs, vts, nk, cls = geom(qt)

            def mmS(e, S_=S_, q_=q_, k_=k_, qt=qt, segs=segs):
                for (c0, k0, n) in segs:
                    ins = e.matmul(S_[:, c0:c0 + n], q_[:, qt * 128:(qt + 1) * 128], k_[:, k0:k0 + n], start=True, stop=True)
                return ins

            st.op("pe", mmS, reads=[rq[hs], rk[hs]], writes=[rpS[s]])
            if cls is not None:
                st.op("dve", lambda e, S_=S_, sb_=sb_, b_=b_, cls=cls: e.scalar_tensor_tensor(sb_[:, :896], S_[:, :896], scale, b_[:, cls, :], ALU.mult, ALU.add),
                      reads=[rpS[s], rb[hs]], writes=[rsS[sB]])
            else:
                st.op("dve", lambda e, S_=S_, sb_=sb_: e.tensor_scalar(sb_[:, :256], S_[:, :256], scale, None, ALU.mult),
                      reads=[rpS[s]], writes=[rsS[sB]])
            st.op("dve", lambda e, sb_=sb_, mx_=mx_, nk=nk: e.tensor_reduce(mx_[:, 0:1], sb_[:, :nk], AX.X, ALU.max, negate=True),
                  reads=[rsS[sB]], writes=[rmx[sB]])
            st.op("act", lambda e, sb_=sb_, mx_=mx_, p_=p_, nk=nk: e.activation(p_[:, :nk], sb_[:, :nk], AF.Exp, bias=mx_[:, 0:1]),
                  reads=[rsS[sB], rmx[sB]], writes=[rpb_[sB]])

        def stB(u):
            h, qt = units[u]
            s, sB = u % 2, u % AD
            Ts_, p_, pT_ = psT[s], pb[sB], pT[sB]
            segs, vts, nk, cls = geom(qt)
            nj = nk // 128

            def trp(e, Ts_=Ts_, p_=p_, nj=nj):
                for j in range(nj):
                    ins = e.transpose(Ts_[:, j, :], p_[:, j * 128:(j + 1) * 128], ident_b[:])
                return ins

            st.op("pe", trp, reads=[rpb_[sB]], writes=[rpT[s]])
            st.op("act", lambda e, Ts_=Ts_, pT_=pT_, nj=nj: e.copy(pT_[:, :nj, :], Ts_[:, :nj, :]),
                  reads=[rpT[s]], writes=[rpTs[sB]])

        def stC(u):
            h, qt = units[u]
            hs = h % 2
            v_, o_ = Vh[hs], oT[hs]
            s, sB = u % 2, u % AD
            O_, pT_, ri_ = psO[s], pT[sB], ri[sB]
            segs, vts, nk, cls = geom(qt)

            def mmO(e, O_=O_, v_=v_, pT_=pT_, vts=vts):
                n = len(vts)
                for j, vt in enumerate(vts):
                    e.matmul(O_[:, 0, :], v_[:, vt, :], pT_[:, j, :], start=(j == 0), stop=(j == n - 1))
                for j in range(n):
                    ins = e.matmul(O_[:, 1, :], ones_b[:], pT_[:, j, :], start=(j == 0), stop=(j == n - 1))
                return ins

            st.op("pe", mmO, reads=[rv[hs], rpTs[sB]], writes=[rpO[s]])
            st.op("dve", lambda e, O_=O_, ri_=ri_: e.reciprocal(ri_[:], O_[:, 1, :]), reads=[rpO[s]], writes=[rri[sB]])
            st.op("dve", lambda e, O_=O_, ri_=ri_, o_=o_, qt=qt: e.tensor_tensor(o_[:, qt * 128:(qt + 1) * 128], O_[:, 0, :], ri_[:], ALU.mult),
                  reads=[rpO[s], rri[sB]], writes=[ro[hs]])
            if qt == NT - 1:
                st.op("sp", _dma(BR[0, h * 128:(h + 1) * 128, :], o_[:]), reads=[ro[hs]], dma=f"o{hs}")

        for t in range(NU + 2):
            if t < NU:
                stA(t)
            if 0 <= t - 1 < NU:
                stB(t - 1)
            if 0 <= t - 2 < NU:
                stC(t - 2)
        st.run()

    def stage_conv(l):
        st = Stage(nc, f"cv{l}")
        st.lag = 3
        cw = st.sb([128, 8, 3], F32)
        r_cw = Res()
        st.op("sp", _dma(cw[:], conv_wT[l]), writes=[r_cw], dma="cw")
        xc = [st.sb([128, T], BF16, "xc") for _ in range(2)]
        bg = [st.sb([128, T], BF16, "bg") for _ in range(2)]
        cg = [st.sb([128, T], BF16, "cg") for _ in range(2)]
        z = [st.sb([128, T], F32, "z") for _ in range(2)]
        y = [st.sb([128, T], F32, "y") for _ in range(2)]
        o = [st.sb([128, T], BF16, "o") for _ in range(2)]
        rxc, rbg, rcg, rz, ry, ro = ([Res() for _ in range(2)] for _ in range(6))
        for cb in range(8):
            s = cb % 2
            xc_, bg_, cg_, z_, y_, o_ = xc[s], bg[s], cg[s], z[s], y[s], o[s]
            st.op("sp", _dma(xc_[:], PROJ[3072 + cb * 128:3072 + (cb + 1) * 128, :]), writes=[rxc[s]], dma=f"a{s}")
            st.op("sp", _dma(bg_[:], PROJ[4096 + cb * 128:4096 + (cb + 1) * 128, :]), writes=[rbg[s]], dma=f"b{s}")
            st.op("sp", _dma(cg_[:], PROJ[5120 + cb * 128:5120 + (cb + 1) * 128, :]), writes=[rcg[s]], dma=f"c{s}")
            st.op("pool", lambda e, z_=z_, cg_=cg_, xc_=xc_: e.tensor_tensor(z_[:], cg_[:], xc_[:], ALU.mult),
                  reads=[rxc[s], rcg[s]], writes=[rz[s]])
            st.op("dve", lambda e, y_=y_, z_=z_, cb=cb: e.tensor_scalar(y_[:], z_[:], cw[:, cb, 1:2], None, ALU.mult),
                  reads=[rz[s], r_cw], writes=[ry[s]])
            for (a, b) in ((0, SEQ), (SEQ, T)):
                st.op("dve", lambda e, y_=y_, z_=z_, cb=cb, a=a, b=b: e.scalar_tensor_tensor(y_[:, a + 1:b], z_[:, a:b - 1], cw[:, cb, 0:1], y_[:, a + 1:b], ALU.mult, ALU.add),
                      reads=[rz[s], ry[s], r_cw], writes=[ry[s]])
                st.op("dve", lambda e, y_=y_, z_=z_, cb=cb, a=a, b=b: e.scalar_tensor_tensor(y_[:, a:b - 1], z_[:, a + 1:b], cw[:, cb, 2:3], y_[:, a:b - 1], ALU.mult, ALU.add),
                      reads=[rz[s], ry[s], r_cw], writes=[ry[s]])
            st.op("pool", lambda e, o_=o_, bg_=bg_, y_=y_: e.tensor_tensor(o_[:], bg_[:], y_[:], ALU.mult),
                  reads=[rbg[s], ry[s]], writes=[ro[s]])
            st.op("sp", _dma(BR[1, cb * 128:(cb + 1) * 128, :], o_[:]), reads=[ro[s]], dma=f"o{s}")
        st.run()

    def gelu_ops(st, eng2, x_ap, tmp_ap, out_ap, rx, rtmp, rout):
        st.op(eng2, lambda e: e.tensor_tensor(tmp_ap, x_ap, x_ap, ALU.mult), reads=[rx], writes=[rtmp])
        st.op("dve", lambda e: e.tensor_scalar(tmp_ap, tmp_ap, 0.044715, 1.0, ALU.mult, ALU.add), reads=[rtmp], writes=[rtmp])
        st.op(eng2, lambda e: e.tensor_tensor(tmp_ap, tmp_ap, x_ap, ALU.mult), reads=[rtmp, rx], writes=[rtmp])
        st.op("act", lambda e: e.activation(tmp_ap, tmp_ap, AF.Sigmoid, scale=1.5957691216057308), reads=[rtmp], writes=[rtmp])
        st.op("dve", lambda e: e.tensor_tensor(out_ap, tmp_ap, x_ap, ALU.mult), reads=[rtmp, rx], writes=[rout])

    def stage_gmlp(l):
        st = Stage(nc, f"gm{l}")
        st.lag = 0
        lng = st.sb([128, DB], F32)
        bsb = st.sb([128, DB], F32)
        wsf = st.sb([128, DB], F32)
        wsb = st.sb([128, 8, 128], BF16)
        r_lng, r_bsb, r_wsf, r_wsb = Res(), Res(), Res(), Res()
        load_bc(st, lng[:], ln_g[l:l + 1, :], r_lng, "lng")
        load_bc(st, bsb[:], b_sp[l:l + 1, :], r_bsb, "bsb")
        st.op("sp", _dma(wsf[:], wsT[l].rearrange("q g p -> q (g p)")), writes=[r_wsf], dma="ws")
        st.op("dve", lambda e: e.tensor_copy(wsb[:].rearrange("q g p -> q (g p)"), wsf[:]), reads=[r_wsf], writes=[r_wsb])
        GD = 3

        def mk(shape, dt, nm):
            return [st.sb(shape, dt, nm) for _ in range(GD)], [Res() for _ in range(GD)]

        vb, rvb = mk([128, DB], BF16, "v")
        ub, rub = mk([128, DB], BF16, "u")
        tvL, r_tvL = mk([128, DB], F32, "tv")
        gvL, r_gvL = mk([128, DB], F32, "gv")
        tuL, r_tuL = mk([128, DB], F32, "tu")
        guL, r_guL = mk([128, DB], F32, "gu")
        sttL, r_sttL = mk([128, 2, 6], F32, "stt")
        mvL, r_mvL = mk([128, 4], F32, "mv")
        vn, rvn = mk([128, DB], BF16, "vn")
        soL, r_soL = mk([128, DB], F32, "so")
        ob, rob = mk([128, DB], BF16, "o")
        pss = [st.ps([128, 4, 128]) for _ in range(4)]
        rps = [Res() for _ in range(4)]

        def ph1(i):
            s = i % GD
            v_, u_ = vb[s], ub[s]
            st.op("sp", _dma(v_[:], VS_tm[i * 128:(i + 1) * 128, :]), writes=[rvb[s]], dma=f"v{s}")
            st.op("sp", _dma(u_[:].rearrange("p (b t) -> p b t", b=8),
                             PROJ[6144:7168, i * 128:(i + 1) * 128].rearrange("(b p) t -> p b t", p=128)),
                  writes=[rub[s]], dma=f"u{s}")
            gelu_ops(st, "pool", v_[:], tvL[s][:], gvL[s][:], rvb[s], r_tvL[s], r_gvL[s])
            gelu_ops(st, "pool", u_[:], tuL[s][:], guL[s][:], rub[s], r_tuL[s], r_guL[s])

        def ph2(i):
            s = i % GD
            gv, stt, mv, vn_ = gvL[s], sttL[s], mvL[s], vn[s]
            r_gv, r_stt, r_mv = r_gvL[s], r_sttL[s], r_mvL[s]
            for hh in range(2):
                st.op("dve", lambda e, hh=hh, stt=stt, gv=gv: e.bn_stats(stt[:, hh, :], gv[:, hh * 512:(hh + 1) * 512]), reads=[r_gv], writes=[r_stt])
            st.op("dve", lambda e, mv=mv, stt=stt: e.bn_aggr(mv[:, 0:2], stt[:].rearrange("p a b -> p (a b)")), reads=[r_stt], writes=[r_mv])
            st.op("act", lambda e, mv=mv: e.activation(mv[:, 2:3], mv[:, 1:2], AF.Sqrt, bias=EPS, scale=1.0), reads=[r_mv], writes=[r_mv])
            st.op("dve", lambda e, mv=mv: e.reciprocal(mv[:, 3:4], mv[:, 2:3]), reads=[r_mv], writes=[r_mv])
            st.op("dve", lambda e, gv=gv, mv=mv: e.tensor_scalar(gv[:], gv[:], mv[:, 0:1], mv[:, 3:4], ALU.subtract, ALU.mult), reads=[r_gv, r_mv], writes=[r_gv])
            st.op("pool", lambda e, vn_=vn_, gv=gv: e.tensor_tensor(vn_[:], gv[:], lng[:], ALU.mult), reads=[r_gv, r_lng], writes=[rvn[s]])
            for half in range(2):
                pp = pss[2 * (i % 2) + half]

                def mm(e, pp=pp, half=half, vn_=vn_):
                    for j in range(4):
                        gi = half * 4 + j
                        ins = e.matmul(pp[:, j, :], vn_[:, gi * 128:(gi + 1) * 128], wsb[:, gi, :], start=True, stop=True)
                    return ins

                st.op("pe", mm, reads=[rvn[s], r_wsb], writes=[rps[2 * (i % 2) + half]])

        def ph3(i):
            s = i % GD
            so, gu, o_ = soL[s], guL[s], ob[s]
            for half in range(2):
                pp = pss[2 * (i % 2) + half]
                st.op("dve", lambda e, pp=pp, half=half, so=so: e.tensor_tensor(so[:, half * 512:(half + 1) * 512], pp[:].rearrange("p a b -> p (a b)"), bsb[:, half * 512:(half + 1) * 512], ALU.add),
                      reads=[rps[2 * (i % 2) + half], r_bsb], writes=[r_soL[s]])
            st.op("pool", lambda e, o_=o_, so=so, gu=gu: e.tensor_tensor(o_[:], so[:], gu[:], ALU.mult), reads=[r_soL[s], r_guL[s]], writes=[rob[s]])
            st.op("sp", _dma(BR[2, :, i * 128:(i + 1) * 128].rearrange("(b p) t -> p b t", p=128),
                             o_[:].rearrange("p (b t) -> p b t", b=8)), reads=[rob[s]], dma=f"o{s}")

        for t in range(NT + 2):
            if t < NT:
                ph1(t)
            if 0 <= t - 1 < NT:
                ph2(t - 1)
            if 0 <= t - 2 < NT:
                ph3(t - 2)
        st.run()

    def stage_merge1(l):
        st = Stage(nc, f"m1{l}")
        st.lag = 2
        wb = [st.sb([128, 3, 8, 512], BF16, "w") for _ in range(2)]
        rwb = [Res() for _ in range(2)]
        br = [st.sb([128, 3, 8, 512], BF16, "br") for _ in range(2)]
        rbr = [Res() for _ in range(2)]
        MD = 4
        gl = [st.sb([128, 3, 512], BF16, "gl") for _ in range(MD)]
        rgl = [Res() for _ in range(MD)]
        sg = [st.sb([128, 3, 512], F32, "sg") for _ in range(MD)]
        rsg = [Res() for _ in range(MD)]
        pss = [st.ps([128, 512]) for _ in range(6)]
        rps = [Res() for _ in range(6)]
        ta = [st.sb([128, 512], F32, "ta") for _ in range(MD)]
        tb_ = [st.sb([128, 512], F32, "tb") for _ in range(MD)]
        rta, rtb = [Res() for _ in range(MD)], [Res() for _ in range(MD)]
        mb = [st.sb([128, 512], BF16, "mb") for _ in range(MD)]
        rmb = [Res() for _ in range(MD)]
        GLv = PROJ[8192:DPROJ, :].rearrange("(i r) t -> r i t", i=3)
        kk = 0
        kb = 0
        for dg in range(4):
            ws_ = dg % 2
            for i in range(3):
                st.op("pool", _dma(wb[ws_][:, i], w_branch[l, i, :, dg * 512:(dg + 1) * 512].rearrange("(kc p) d -> p kc d", p=128)),
                      writes=[rwb[ws_]], dma=f"w{ws_}{i}")
            for (t0, n) in TGS:
                bs = kb % 2
                kb += 1
                for i in range(3):
                    st.op("sp", _dma(br[bs][:, i, :, :n], BR[i, :, t0:t0 + n].rearrange("(kc p) t -> p kc t", p=128)),
                          writes=[rbr[bs]], dma=f"b{bs}{i}")
                for db in range(4):
                    s = kk % 2
                    sB = kk % MD
                    kk += 1
                    r0 = dg * 512 + db * 128
                    gl_, sg_, ta_, tb2, mb_ = gl[sB], sg[sB], ta[sB], tb_[sB], mb[sB]
                    st.op("sp", _dma(gl_[:, :, :n], GLv[r0:r0 + 128, :, t0:t0 + n]), writes=[rgl[sB]], dma=f"g{sB}")
                    st.op("act", lambda e, gl_=gl_, sg_=sg_, n=n: e.activation(sg_[:, :, :n], gl_[:, :, :n], AF.Sigmoid),
                          reads=[rgl[sB]], writes=[rsg[sB]])
                    for i in range(3):
                        p = pss[3 * s + i]

                        def mm(e, p=p, i=i, ws_=ws_, bs=bs, db=db, n=n):
                            for kc in range(8):
                                ins = e.matmul(p[:, :n], wb[ws_][:, i, kc, db * 128:(db + 1) * 128], br[bs][:, i, kc, :n],
                                               start=(kc == 0), stop=(kc == 7))
                            return ins

                        st.op("pe", mm, reads=[rwb[ws_], rbr[bs]], writes=[rps[3 * s + i]])
                    p0, p1, p2 = pss[3 * s], pss[3 * s + 1], pss[3 * s + 2]
                    st.op("dve", lambda e, ta_=ta_, p0=p0, sg_=sg_, n=n: e.tensor_tensor(ta_[:, :n], p0[:, :n], sg_[:, 0, :n], ALU.mult),
                          reads=[rps[3 * s], rsg[sB]], writes=[rta[sB]])
                    st.op("dve", lambda e, tb2=tb2, p1=p1, sg_=sg_, n=n: e.tensor_tensor(tb2[:, :n], p1[:, :n], sg_[:, 1, :n], ALU.mult),
                          reads=[rps[3 * s + 1], rsg[sB]], writes=[rtb[sB]])
                    st.op("pool", lambda e, ta_=ta_, tb2=tb2, n=n: e.tensor_tensor(ta_[:, :n], ta_[:, :n], tb2[:, :n], ALU.add),
                          reads=[rta[sB], rtb[sB]], writes=[rta[sB]])
                    st.op("dve", lambda e, tb2=tb2, p2=p2, sg_=sg_, n=n: e.tensor_tensor(tb2[:, :n], p2[:, :n], sg_[:, 2, :n], ALU.mult),
                          reads=[rps[3 * s + 2], rsg[sB]], writes=[rtb[sB]])
                    st.op("pool", lambda e, ta_=ta_, tb2=tb2, mb_=mb_, n=n: e.tensor_tensor(mb_[:, :n], ta_[:, :n], tb2[:, :n], ALU.add),
                          reads=[rta[sB], rtb[sB]], writes=[rmb[sB]])
                    st.op("sp", _dma(MERGED[r0:r0 + 128, t0:t0 + n], mb_[:, :n]), reads=[rmb[sB]], dma=f"o{sB}")
        st.run()

    def stage_merge2(l, Xsrc):
        st = Stage(nc, f"m2{l}")
        st.lag = 2
        wo = [st.sb([128, 16, 512], BF16, "w") for _ in range(2)]
        rwo = [Res() for _ in range(2)]
        g1 = st.sb([128, 2, D], F32)
        r_g1 = Res()
        for r in range(2):
            load_bc(st, g1[:, r, :], MOD[r:r + 1, 2 * D:3 * D], r_g1, f"g{r}")
        mT = [st.sb([128, 16, 512], BF16, "m") for _ in range(2)]
        rmT = [Res() for _ in range(2)]
        xt = [st.sb([128, 512], F32, "x") for _ in range(4)]
        rxt = [Res() for _ in range(4)]
        xo = [st.sb([128, 512], F32, "xo") for _ in range(4)]
        rxo = [Res() for _ in range(4)]
        tt = [st.sb([128, 512], F32, "t") for _ in range(4)]
        rtt = [Res() for _ in range(4)]
        pss = [st.ps([128, 512]) for _ in range(4)]
        rps = [Res() for _ in range(4)]
        pk = 0
        mk = 0
        for cg in range(getattr(build, 'm2_cg', 4)):
            wsl = cg % 2
            w_ = wo[wsl]
            st.op("pool", _dma(w_[:], w_out[l, :, cg * 512:(cg + 1) * 512].rearrange("(kc p) d -> p kc d", p=128)),
                  writes=[rwo[wsl]], dma=f"w{wsl}")
            for gi, (t0, n) in enumerate(TGS[:getattr(build, 'm2_tg', 5)]):
                ms = mk % 2
                mk += 1
                m_ = mT[ms]
                st.op("sp", _dma(m_[:, :, :n], MERGED[:, t0:t0 + n].rearrange("(kc p) t -> p kc t", p=128)),
                      writes=[rmT[ms]], dma=f"m{ms}")
                for j in range(n // 128):
                    i = t0 // 128 + j
                    r = 0 if i < 16 else 1
                    s = pk % 4
                    p = pss[pk % 4]
                    rp = rps[pk % 4]
                    pk += 1
                    x_, xo_, t_ = xt[s], xo[s], tt[s]
                    st.op("sp", _dma(x_[:], Xsrc[i * 128:(i + 1) * 128, cg * 512:(cg + 1) * 512]), writes=[rxt[s]], dma=f"x{s}")

                    def mm(e, p=p, m_=m_, w_=w_, j=j):
                        for kc in range(16):
                            ins = e.matmul(p[:], m_[:, kc, j * 128:(j + 1) * 128], w_[:, kc, :],
                                           start=(kc == 0), stop=(kc == 15))
                        return ins

                    st.op("pe", mm, reads=[rmT[ms], rwo[wsl]], writes=[rp])
                    st.op("dve", lambda e, p=p, t_=t_, r=r, cg=cg: e.tensor_tensor(t_[:], p[:], g1[:, r, cg * 512:(cg + 1) * 512], ALU.mult),
                          reads=[rp, r_g1], writes=[rtt[s]])
                    st.op("pool", lambda e, t_=t_, x_=x_, xo_=xo_: e.tensor_tensor(xo_[:], t_[:], x_[:], ALU.add),
                          reads=[rtt[s], rxt[s]], writes=[rxo[s]])
                    st.op("sp", _dma(XA[i * 128:(i + 1) * 128, cg * 512:(cg + 1) * 512], xo_[:]), reads=[rxo[s]], dma=f"o{s}")
        st.run()

    def stage_route(l):
        st = Stage(nc, f"rt{l}")
        affT = st.sb([16, T], F32)
        r_affT = Res()
        pst = [st.ps([16, 512]) for _ in range(2)]
        rpst = [Res() for _ in range(2)]
        r_aff = Res()
        for q in range(5):
            p = pst[q % 2]
            ntile = 4 if q < 4 else 2

            def tr(e, p=p, q=q, ntile=ntile):
                for j in range(ntile):
                    ins = e.transpose(p[:, j * 128:(j + 1) * 128], AFF[:, q * 4 + j, :], ident_f[:])
                return ins

            st.op("pe", tr, reads=[r_aff], writes=[rpst[q % 2]])
            st.op("act", lambda e, p=p, q=q, ntile=ntile: e.copy(affT[:, q * 512:q * 512 + ntile * 128], p[:, :ntile * 128]),
                  reads=[rpst[q % 2]], writes=[r_affT])
        work = st.sb([16, SEQ], F32)
        workc = st.sb([16, CTX], F32)
        m8 = st.sb([16, 8], F32)
        m8c = st.sb([16, 8], F32)
        r_work, r_workc, r_m8, r_m8c = Res(), Res(), Res(), Res()
        st.op("dve", lambda e: e.tensor_copy(work[:], affT[:, :SEQ]), reads=[r_affT], writes=[r_work])
        st.op("pool", lambda e: e.tensor_copy(workc[:], affT[:, SEQ:]), reads=[r_affT], writes=[r_workc])
        for it in range(CAP // 8):
            st.op("dve", lambda e: e.max(m8[:], work[:]), reads=[r_work], writes=[r_m8])
            if it < CAP // 8 - 1:
                st.op("dve", lambda e: e.match_replace(work[:], m8[:], work[:], -1.0), reads=[r_work, r_m8], writes=[r_work])
        for it in range(CAPC // 8):
            st.op("dve", lambda e: e.max(m8c[:], workc[:]), reads=[r_workc], writes=[r_m8c])
            if it < CAPC // 8 - 1:
                st.op("dve", lambda e: e.match_replace(workc[:], m8c[:], workc[:], -1.0), reads=[r_workc, r_m8c], writes=[r_workc])
        maskT = st.sb([16, T], F32)
        gateT = st.sb([16, T], F32)
        posT = st.sb([16, T], F32)
        onesr = st.sb([16, SEQ], F32)
        r_mask, r_gate, r_pos, r_ones = Res(), Res(), Res(), Res()
        st.op("pool", lambda e: e.memset(onesr[:], 1.0), writes=[r_ones])
        st.op("dve", lambda e: e.tensor_scalar(maskT[:, :SEQ], affT[:, :SEQ], m8[:, 7:8], None, ALU.is_ge), reads=[r_affT, r_m8], writes=[r_mask])
        st.op("dve", lambda e: e.tensor_scalar(maskT[:, SEQ:], affT[:, SEQ:], m8c[:, 7:8], None, ALU.is_ge), reads=[r_affT, r_m8c], writes=[r_mask])
        st.op("dve", lambda e: e.tensor_tensor(gateT[:], maskT[:], affT[:], ALU.mult), reads=[r_mask, r_affT], writes=[r_gate])
        st.op("dve", lambda e: e.tensor_tensor_scan(posT[:, :SEQ], onesr[:, :SEQ], maskT[:, :SEQ], 0.0, ALU.mult, ALU.add),
              reads=[r_mask, r_ones], writes=[r_pos])
        st.op("dve", lambda e: e.tensor_tensor_scan(posT[:, SEQ:], onesr[:, :CTX], maskT[:, SEQ:], 0.0, ALU.mult, ALU.add),
              reads=[r_mask, r_ones], writes=[r_pos])
        st.op("dve", lambda e: e.tensor_tensor(posT[:], posT[:], maskT[:], ALU.subtract), reads=[r_pos, r_mask], writes=[r_pos])
        ptm = [st.ps([128, 2, NE]) for _ in range(2)]
        rptm = [Res() for _ in range(2)]
        r_P, r_M = Res(), Res()
        for i in range(NT):
            s = i % 2
            p = ptm[s]

            def tr2(e, p=p, i=i):
                e.transpose(p[:, 0, :], posT[:, i * 128:(i + 1) * 128], ident_f[:16, :16])
                return e.transpose(p[:, 1, :], maskT[:, i * 128:(i + 1) * 128], ident_f[:16, :16])

            st.op("pe", tr2, reads=[r_pos, r_mask], writes=[rptm[s]])
            st.op("act", lambda e, p=p, i=i: e.copy(POS[:, i, :], p[:, 0, :]), reads=[rptm[s]], writes=[r_P])
            st.op("dve", lambda e, p=p, i=i: e.tensor_copy(MSK[:, i, :], p[:, 1, :]), reads=[rptm[s]], writes=[r_M])
        sel = st.sb([16, 16, 128], F32)
        iop = st.sb([128, 2], F32)
        r_sel, r_iop = Res(), Res()
        st.op("sp", _dma(sel[:], sel_d), writes=[r_sel], dma="sel")
        selb = st.sb([16, 16, 128], BF16)
        posb = st.sb([16, T], BF16)
        gateb = st.sb([16, T], BF16)
        r_selb, r_posb, r_gateb = Res(), Res(), Res()
        st.op("act", lambda e: e.copy(selb[:].rearrange("a b c -> a (b c)"), sel[:].rearrange("a b c -> a (b c)")), reads=[r_sel], writes=[r_selb])
        st.op("act", lambda e: e.copy(posb[:], posT[:]), reads=[r_pos], writes=[r_posb])
        st.op("act", lambda e: e.copy(gateb[:], gateT[:]), reads=[r_gate], writes=[r_gateb])
        st.op("sp", _dma(iop[:], iop_d), writes=[r_iop], dma="iop")
        pbp = [st.ps([128, 512]) for _ in range(2)]
        pbg = [st.ps([128, 512]) for _ in range(2)]
        rpbp, rpbg = [Res() for _ in range(2)], [Res() for _ in range(2)]
        gbs = [st.sb([128, 512], F32, "gb") for _ in range(2)]
        rgbs = [Res() for _ in range(2)]
        gto = [st.sb([128, 2, 512], BF16, "gt") for _ in range(2)]
        rgto = [Res() for _ in range(2)]
        k = 0
        for ex in range(NE):
            for (t0, n) in TGS:
                s = k % 2
                k += 1
                pp, pg, gb_, go_ = pbp[s], pbg[s], gbs[s], gto[s]
                st.op("pe", lambda e, pp=pp, ex=ex, t0=t0, n=n: e.matmul(pp[:, :n], selb[:, ex, :], posb[:, t0:t0 + n], start=True, stop=True),
                      reads=[r_selb, r_posb], writes=[rpbp[s]])
                st.op("pe", lambda e, pg=pg, ex=ex, t0=t0, n=n: e.matmul(pg[:, :n], selb[:, ex, :], gateb[:, t0:t0 + n], start=True, stop=True),
                      reads=[r_selb, r_gateb], writes=[rpbg[s]])
                st.op("act", lambda e, pg=pg, gb_=gb_, n=n: e.copy(gb_[:, :n], pg[:, :n]), reads=[rpbg[s]], writes=[rgbs[s]])
                for sti in range(2):
                    st.op("dve", lambda e, pp=pp, gb_=gb_, go_=go_, sti=sti, n=n: e.scalar_tensor_tensor(go_[:, sti, :n], pp[:, :n], iop[:, sti:sti + 1], gb_[:, :n], ALU.is_equal, ALU.mult),
                          reads=[rpbp[s], rgbs[s], r_iop], writes=[rgto[s]])
                for sti in range(2):
                    st.op("sp", _dma(GT[t0 // 128:(t0 + n) // 128, :, ex, sti, :].rearrange("i p t -> p i t"),
                                     go_[:, sti, :n].rearrange("p (i t) -> p i t", t=128)), reads=[rgto[s]], dma=f"o{s}{sti}")
        st.run()

    def stage_gather(l):
        st = Stage(nc, f"ga{l}")
        h2 = st.sb([128, NT, D], BF16)
        r_h2 = Res()
        for q in range(6):
            st.op("sp", _dma(h2[:, q * 3:(q + 1) * 3, :], H2[q * 384:(q + 1) * 384, :].rearrange("(i p) d -> p i d", p=128)),
                  writes=[r_h2], dma=f"h{q}")
        io = st.sb([128, 256], F32)
        r_io = Res()
        st.op("sp", _dma(io[:], iota_d), writes=[r_io], dma="io")
        S = [st.sb([128, 16, 256], BF16, "S") for _ in range(3)]
        Sc = [st.sb([128, 2, 32], BF16, "Sc") for _ in range(3)]
        rS = [Res() for _ in range(3)]
        xs = [st.sb([128, 16, NS], BF16, "xs") for _ in range(3)]
        rxs = [Res() for _ in range(3)]
        pss = [st.ps([128, 512]) for _ in range(4)]
        rps = [Res() for _ in range(4)]
        r_P, r_M = Res(), Res()
        pk = 0
        for ex in range(NE):
            s = ex % 3
            S_, Sc_, xs_ = S[s], Sc[s], xs[s]
            for tt in range(16):
                eng = "pool" if tt % 4 == 3 else "dve"
                st.op(eng, lambda e, S_=S_, tt=tt, ex=ex: e.tensor_scalar(S_[:, tt, :], io[:], POS[:, tt, ex:ex + 1], MSK[:, tt, ex:ex + 1], ALU.is_equal, ALU.mult),
                      reads=[r_io, r_P, r_M], writes=[rS[s]])
            for j in range(2):
                st.op("dve", lambda e, Sc_=Sc_, j=j, ex=ex: e.tensor_scalar(Sc_[:, j, :], io[:, :32], POS[:, 16 + j, ex:ex + 1], MSK[:, 16 + j, ex:ex + 1], ALU.is_equal, ALU.mult),
                      reads=[r_io, r_P, r_M], writes=[rS[s]])
            for fb in range(16):
                p = pss[pk % 4]
                rp = rps[pk % 4]
                pk += 1

                def mm(e, p=p, S_=S_, Sc_=Sc_, fb=fb):
                    for tt in range(16):
                        e.matmul(p[:, :256], h2[:, tt, fb * 128:(fb + 1) * 128], S_[:, tt, :], start=(tt == 0), stop=(tt == 15))
                    for j in range(2):
                        ins = e.matmul(p[:, 256:NS], h2[:, 16 + j, fb * 128:(fb + 1) * 128], Sc_[:, j, :], start=(j == 0), stop=(j == 1))
                    return ins

                st.op("pe", mm, reads=[r_h2, rS[s]], writes=[rp])
                if fb % 2 == 0:
                    st.op("act", lambda e, p=p, xs_=xs_, fb=fb: e.copy(xs_[:, fb, :], p[:, :NS]), reads=[rp], writes=[rxs[s]])
                else:
                    st.op("dve", lambda e, p=p, xs_=xs_, fb=fb: e.tensor_copy(xs_[:, fb, :], p[:, :NS]), reads=[rp], writes=[rxs[s]])
            st.op("sp", _dma(XS[ex].rearrange("(kc p) s -> p kc s", p=128), xs_[:]), reads=[rxs[s]], dma=f"o{s}")
        st.run()

    def stage_experts(l):
        st = Stage(nc, f"ex{l}")
        st.lag = 1
        NW = 4
        ws = [st.sb([128, 16, 512], BF16, "w") for _ in range(NW)]
        rw = [Res() for _ in range(NW)]
        xs = [st.sb([128, 16, NS], BF16, "xs") for _ in range(2)]
        rxs = [Res() for _ in range(2)]
        aT = [st.sb([128, 16, NS], BF16, "aT") for _ in range(2)]
        raT = [Res() for _ in range(2)]
        yb = [st.sb([128, 3, D], BF16, "yb") for _ in range(2)]
        ryb = [Res() for _ in range(2)]
        sg = [st.sb([128, NS], F32, "sg") for _ in range(2)]
        rsg = [Res() for _ in range(2)]
        pss = [st.ps([128, 512]) for _ in range(8)]
        rps = [Res() for _ in range(8)]
        pieces = []
        for ex in range(NE):
            for fg in range(4):
                pieces.append((w_eg, ex, fg))
                pieces.append((w_eu, ex, fg))
            for dg in range(4):
                pieces.append((w_ed, ex, dg))
        NP = len(pieces)

        def ld(pi):
            wt, ex, cg = pieces[pi]
            st.op("pool", _dma(ws[pi % NW][:], wt[l, ex, :, cg * 512:(cg + 1) * 512].rearrange("(kc p) c -> p kc c", p=128)),
                  writes=[rw[pi % NW]], dma=f"w{pi % NW}")

        for pi in range(NW):
            ld(pi)
        pi = 0
        pk = 0
        sk = 0
        for ex in range(NE):
            s = ex % 2
            xs_, aT_, yb_ = xs[s], aT[s], yb[s]
            st.op("sp", _dma(xs_[:], XS[ex].rearrange("(kc p) s -> p kc s", p=128)), writes=[rxs[s]], dma=f"x{s}")
            for fg in range(4):
                pi += 2
                wg_, wu_ = ws[(pi - 2) % NW], ws[(pi - 1) % NW]
                rwg, rwu = rw[(pi - 2) % NW], rw[(pi - 1) % NW]
                for fb in range(4):
                    pg, rpg = pss[pk % 8], rps[pk % 8]
                    pu, rpu = pss[(pk + 1) % 8], rps[(pk + 1) % 8]
                    pk += 2
                    sg_, rsg_ = sg[sk % 2], rsg[sk % 2]
                    sk += 1

                    def mm(e, p=pg, w=wg_, fb=fb, xs_=xs_):
                        for kc in range(16):
                            ins = e.matmul(p[:, :NS], w[:, kc, fb * 128:(fb + 1) * 128], xs_[:, kc, :], start=(kc == 0), stop=(kc == 15))
                        return ins

                    def mm2(e, p=pu, w=wu_, fb=fb, xs_=xs_):
                        for kc in range(16):
                            ins = e.matmul(p[:, :NS], w[:, kc, fb * 128:(fb + 1) * 128], xs_[:, kc, :], start=(kc == 0), stop=(kc == 15))
                        return ins

                    st.op("pe", mm, reads=[rwg, rxs[s]], writes=[rpg])
                    st.op("pe", mm2, reads=[rwu, rxs[s]], writes=[rpu])
                    st.op("act", lambda e, pg=pg, sg_=sg_: e.activation(sg_[:], pg[:, :NS], AF.Silu), reads=[rpg], writes=[rsg_])
                    st.op("dve", lambda e, pu=pu, sg_=sg_, aT_=aT_, f=fg * 4 + fb: e.tensor_tensor(aT_[:, f, :], sg_[:], pu[:, :NS], ALU.mult),
                          reads=[rpu, rsg_], writes=[raT[s]])
                for nxt in (pi - 2 + NW, pi - 1 + NW):
                    if nxt < NP:
                        ld(nxt)
            for dg in range(4):
                pi += 1
                wd_, rwd = ws[(pi - 1) % NW], rw[(pi - 1) % NW]
                for sti, (s0, m) in enumerate(((0, 128), (128, 128), (256, 32))):
                    p, rp = pss[pk % 8], rps[pk % 8]
                    pk += 1

                    def mm3(e, p=p, w=wd_, s0=s0, m=m, aT_=aT_):
                        for fc in range(16):
                            ins = e.matmul(p[:m, :], aT_[:, fc, s0:s0 + m], w[:, fc, :], start=(fc == 0), stop=(fc == 15))
                        return ins

                    st.op("pe", mm3, reads=[rwd, raT[s]], writes=[rp])
                    if sti % 2 == 0:
                        st.op("act", lambda e, p=p, yb_=yb_, sti=sti, m=m, dg=dg: e.copy(yb_[:m, sti, dg * 512:(dg + 1) * 512], p[:m, :]),
                              reads=[rp], writes=[ryb[s]])
                    else:
                        st.op("dve", lambda e, p=p, yb_=yb_, sti=sti, m=m, dg=dg: e.tensor_copy(yb_[:m, sti, dg * 512:(dg + 1) * 512], p[:m, :]),
                              reads=[rp], writes=[ryb[s]])
                if pi - 1 + NW < NP:
                    ld(pi - 1 + NW)
            st.op("sp", _dma(Y[ex].rearrange("(s p) d -> p s d", p=128), yb_[:, 0:2, :]), reads=[ryb[s]], dma=f"y{s}")
            st.op("sp", _dma(YC[ex], yb_[:32, 2, :]), reads=[ryb[s]], dma=f"c{s}")
        st.run()

    def stage_combine(l, Xdst):
        st = Stage(nc, f"cb{l}")
        st.lag = 4
        g2 = st.sb([128, 2, D], F32)
        r_g2 = Res()
        for r in range(2):
            load_bc(st, g2[:, r, :], MOD[r:r + 1, 5 * D:6 * D], r_g2, f"g{r}")
        Yf = st.sb([128, NE, 2, 512], BF16)
        YCf = st.sb([32, NE, 512], BF16)
        r_Yf, r_YCf = Res(), Res()
        CD = 4
        gtt = [st.sb([128, NE, 2, 128], BF16, "gt") for _ in range(CD)]
        rgt = [Res() for _ in range(CD)]
        xa = [st.sb([128, 512], F32, "xa") for _ in range(CD)]
        rxa = [Res() for _ in range(CD)]
        tt = [st.sb([128, 512], F32, "t") for _ in range(CD)]
        rtt = [Res() for _ in range(CD)]
        xo = [st.sb([128, 512], F32, "xo") for _ in range(CD)]
        rxo = [Res() for _ in range(CD)]
        pss = [st.ps([128, 512]) for _ in range(CD)]
        rps = [Res() for _ in range(CD)]
        k = 0
        for fg in range(4):
            c0 = fg * 512
            for q in range(4):
                st.op("sp", _dma(Yf[:, q * 4:(q + 1) * 4], Y[q * 4:(q + 1) * 4, :, c0:c0 + 512].rearrange("e (s p) d -> p e s d", p=128)),
                      writes=[r_Yf], dma=f"y{q}")
            st.op("sp", _dma(YCf[:], YC[:, :, c0:c0 + 512].rearrange("e p d -> p e d")), writes=[r_YCf], dma="yc")
            for i in range(NT):
                s = k % CD
                k += 1
                r = 0 if i < 16 else 1
                g_, xa_, t_, xo_, p = gtt[s], xa[s], tt[s], xo[s], pss[s]
                st.op("sp", _dma(g_[:], GT[i]), writes=[rgt[s]], dma=f"g{s}")
                st.op("sp", _dma(xa_[:], XA[i * 128:(i + 1) * 128, c0:c0 + 512]), writes=[rxa[s]], dma=f"x{s}")
                if i < 16:
                    def mm(e, p=p, g_=g_):
                        for j in range(32):
                            ex, sti = j // 2, j % 2
                            ins = e.matmul(p[:], g_[:, ex, sti, :], Yf[:, ex, sti, :], start=(j == 0), stop=(j == 31))
                        return ins
                    st.op("pe", mm, reads=[rgt[s], r_Yf], writes=[rps[s]])
                else:
                    def mmc(e, p=p, g_=g_):
                        for ex in range(NE):
                            ins = e.matmul(p[:], g_[:32, ex, 0, :], YCf[:, ex, :], start=(ex == 0), stop=(ex == NE - 1))
                        return ins
                    st.op("pe", mmc, reads=[rgt[s], r_YCf], writes=[rps[s]])
                st.op("dve", lambda e, p=p, t_=t_, r=r, c0=c0: e.tensor_tensor(t_[:], p[:], g2[:, r, c0:c0 + 512], ALU.mult),
                      reads=[rps[s], r_g2], writes=[rtt[s]])
                st.op("pool", lambda e, t_=t_, xa_=xa_, xo_=xo_: e.tensor_tensor(xo_[:], t_[:], xa_[:], ALU.add),
                      reads=[rtt[s], rxa[s]], writes=[rxo[s]])
                st.op("sp", _dma(Xdst[i * 128:(i + 1) * 128, c0:c0 + 512], xo_[:]), reads=[rxo[s]], dma=f"o{s}")
        st.run()

    def stage_final(Xsrc):
        st = Stage(nc, "fin")
        st.lag = 1
        gb = st.sb([128, D], F32)
        r_gb = Res()
        load_bc(st, gb[:], final_g[0:1, :], r_gb, "g")
        xt = [st.sb([128, D], F32, "x") for _ in range(2)]
        rx = [Res() for _ in range(2)]
        junk = st.sb([128, D], BF16)
        r_junk = Res()
        ss = [st.sb([128, 4], F32, "ss") for _ in range(2)]
        rss = [Res() for _ in range(2)]
        ot = [st.sb([128, D], F32, "o") for _ in range(2)]
        rot = [Res() for _ in range(2)]
        for i in range(16):
            s = i % 2
            x_, ss_, o_ = xt[s], ss[s], ot[s]
            st.op("sp", _dma(x_[:], Xsrc[i * 128:(i + 1) * 128, :]), writes=[rx[s]], dma=f"x{s}")
            st.op("act", lambda e, x_=x_, ss_=ss_: e.activation(junk[:], x_[:], AF.Square, accum_out=ss_[:, 0:1]),
                  reads=[rx[s]], writes=[r_junk, rss[s]])
            st.op("act", lambda e, ss_=ss_: e.activation(ss_[:, 1:2], ss_[:, 0:1], AF.Sqrt, bias=EPS, scale=1.0 / D),
                  reads=[rss[s]], writes=[rss[s]])
            st.op("dve", lambda e, ss_=ss_: e.reciprocal(ss_[:, 2:3], ss_[:, 1:2]), reads=[rss[s]], writes=[rss[s]])
            st.op("dve", lambda e, x_=x_, ss_=ss_, o_=o_: e.scalar_tensor_tensor(o_[:], x_[:], ss_[:, 2:3], gb[:], ALU.mult, ALU.mult),
                  reads=[rx[s], rss[s], r_gb], writes=[rot[s]])
            st.op("sp", _dma(out[i * 128:(i + 1) * 128, :], o_[:]), reads=[rot[s]], dma=f"o{s}")
        st.run()

    stop = getattr(build, "stop_after", None)

    def done(tag):
        return stop is not None and tag == stop

    Xcur = xin
    finished = False
    for l in range(nlayers):
        stage_mod(l)
        if done(f"mod{l}"):
            finished = True
            break
        with nc.sbuf_tensor(f"hT{l}", [128, 16, T], BF16) as hT:
            r_hT = Res()
            stage_norm(l, 1, Xcur, hT, r_hT)
            if done(f"n1{l}"):
                finished = True
                break
            stage_proj(l, hT, r_hT)
        if done(f"pj{l}"):
            finished = True
            break
        stage_rope(l)
        if done(f"rp{l}"):
            finished = True
            break
        stage_attn(l)
        stage_conv(l)
        stage_gmlp(l)
        if done(f"br{l}"):
            finished = True
            break
        stage_merge1(l)
        if done(f"m1{l}"):
            finished = True
            break
        stage_merge2(l, Xcur)
        if done(f"m2{l}"):
            finished = True
            break
        stage_norm(l, 2, XA, None, None)
        stage_route(l)
        if done(f"rt{l}"):
            finished = True
            break
        stage_gather(l)
        stage_experts(l)
        stage_combine(l, XB)
        if done(f"cb{l}"):
            finished = True
            break
        Xcur = XB
    if not finished:
        stage_final(XB)
    top.close()
    return nc


def _consts():
    ident = np.eye(128, dtype=np.float32)
    pm = np.zeros((128, 128), np.float32)
    for f in range(128):
        if (f % 64) < 32:
            pm[f + 32, f] = -1.0
        else:
            pm[f - 32, f] = 1.0
    pos = np.arange(SEQ)
    rc = np.stack([pos // 64, pos % 64], axis=-1).astype(np.float32)
    inv = (np.float32(10000.0) ** (-np.arange(0, 64, 2, dtype=np.float32) / np.float32(64))).astype(np.float32)
    ang = rc[:, :, None] * inv
    cos = np.zeros((128, SEQ), np.float32)
    sin = np.zeros((128, SEQ), np.float32)
    for ax in range(2):
        for half in range(2):
            f0 = ax * 64 + half * 32
            cos[f0:f0 + 32, :] = np.cos(ang[:, ax, :]).T
            sin[f0:f0 + 32, :] = np.sin(ang[:, ax, :]).T
    selT = np.zeros((16, 16, 128), np.float32)
    for e in range(16):
        selT[e, e, :] = 1.0
    iota_s = np.broadcast_to(np.arange(256, dtype=np.float32)[None, :], (128, 256)).copy()
    iop = np.stack([np.arange(128), np.arange(128) + 128], axis=1).astype(np.float32)
    return dict(ident=ident, pm=pm, cos=cos, sin=sin, selT=selT, iota_s=iota_s, iop=iop)


def _rpb_index():
    gs = [0, 1, 5, 14, 15]
    dr = np.zeros((5, 128, 640), np.int64)
    dc = np.zeros((5, 128, 640), np.int64)
    ok = np.zeros((5, 128, 640), bool)
    ql = np.arange(128)
    kl = np.arange(640)
    for ci, g in enumerate(gs):
        tb = min(max(g - 2, 0), 11)
        r = 2 * g + ql // 64
        qc = ql % 64
        rs = np.clip(r - 4, 0, 24)
        ws = np.clip(qc - 8, 0, 48)
        kr = 2 * tb + kl // 64
        kc = kl % 64
        rowok = (kr[None, :] >= rs[:, None]) & (kr[None, :] < rs[:, None] + 8)
        colok = (kc[None, :] >= ws[:, None]) & (kc[None, :] < ws[:, None] + 16)
        ok[ci] = rowok & colok
        dr[ci] = np.clip(kr[None, :] - r[:, None] + 7, 0, 14)
        dc[ci] = np.clip(kc[None, :] - qc[:, None] + 15, 0, 30)
    return dr, dc, ok


def _host_inputs(inputs):
    f = lambda a: np.ascontiguousarray(np.asarray(a, dtype=np.float32))
    x, c, ctx, c_ctx = f(inputs["x"]), f(inputs["c"]), f(inputs["ctx"]), f(inputs["c_ctx"])
    shared = {k: f(inputs[k]) for k in ["w_mod", "b_mod", "norm1_g", "w_in", "gmlp_ln_g", "w_branch", "w_out", "norm2_g",
                                        "w_router", "b_router", "w_e_gate", "w_e_up", "w_e_down"]}
    shared["final_g"] = f(inputs["final_g"]).reshape(1, D)
    shared["b_spatial"] = f(inputs["b_spatial"]).reshape(L, DB)
    shared["wsT"] = np.ascontiguousarray(f(inputs["w_spatial"]).transpose(0, 3, 1, 2))
    shared["conv_wT"] = np.ascontiguousarray(f(inputs["conv_w"]).reshape(L, 3, 8, 128).transpose(0, 3, 2, 1))
    rpb = f(inputs["na_rpb"])
    dr, dc, ok = _rpb_index()
    tab = np.zeros((L, 8, 5, 128, 896), np.float32)
    gathered = rpb[:, :, dr, dc]
    tab[..., :640] = np.where(ok[None, None], gathered, np.float32(NEG))
    shared["rpb_tab"] = np.ascontiguousarray(tab.transpose(0, 1, 3, 2, 4))
    shared.update(_consts())
    in_maps = []
    for b in range(x.shape[0]):
        m = dict(shared)
        m["xin"] = np.ascontiguousarray(np.concatenate([x[b], ctx[b]], axis=0))
        c2 = np.stack([c[b], c_ctx], axis=0)
        m["c2T"] = np.ascontiguousarray(c2.reshape(2, 16, 128).transpose(2, 1, 0))
        in_maps.append(m)
    return in_maps


def kernel(**inputs):
    in_maps = _host_inputs(inputs)
    nc = bass.Bass("TRN2", target_bir_lowering=False)
    build(nc)
    res = run_bass_kernel_spmd(nc, in_maps, core_ids=list(range(len(in_maps))))
    outs = [np.asarray(r["out"], dtype=np.float32) for r in res.results]
    return np.stack(outs, axis=0)
```

```python
import numpy as np
from contextlib import ExitStack
from collections import defaultdict
import concourse.bass as bass
import concourse.mybir as mybir
from concourse.bass_utils import run_bass_kernel_spmd

F32 = mybir.dt.float32
BF16 = mybir.dt.bfloat16
AF = mybir.ActivationFunctionType
ALU = mybir.AluOpType
AX = mybir.AxisListType

D = 2048
L = 2
SEQ = 2048
CTX = 256
T = SEQ + CTX
NT = T // 128
DB = 1024
DPROJ = 14336
NE = 16
CAP = 256
CAPC = 32
NS = CAP + CAPC
EPS = 1e-6
NEG = -30000.0
TGS = [(0, 512), (512, 512), (1024, 512), (1536, 512), (2048, 256)]


class Res:
    __slots__ = ("name", "last_w", "readers")

    def __init__(self, name=""):
        self.name = name
        self.last_w = None
        self.readers = {}


class Op:
    __slots__ = ("eng", "fn", "deps", "needed", "token", "dma", "has_reads")


BLK = {"pe": "tensor", "act": "scalar", "dve": "vector", "pool": "gpsimd", "sp": "sync"}


class Stage:
    def __init__(self, nc, name):
        self.nc = nc
        self.name = name
        self.ops = []
        self.es = ExitStack()
        self._n = 0
        self.touched = []
        self.lag = 0

    def sb(self, shape, dtype, nm="t"):
        self._n += 1
        return self.es.enter_context(self.nc.sbuf_tensor(f"{self.name}_{nm}{self._n}", list(shape), dtype))

    def ps(self, shape, dtype=F32, nm="p"):
        self._n += 1
        return self.es.enter_context(self.nc.psum_tensor(f"{self.name}_{nm}{self._n}", list(shape), dtype))

    def op(self, eng, fn, reads=(), writes=(), dma=None):
        o = Op()
        o.eng = eng
        o.fn = fn
        o.dma = dma
        o.needed = False
        o.token = None
        o.has_reads = len(reads) > 0
        deps = {}
        self.touched.extend(reads)
        self.touched.extend(writes)
        for r in reads:
            if r.last_w is not None:
                deps[id(r.last_w)] = r.last_w
        for w in writes:
            if w.last_w is not None:
                deps[id(w.last_w)] = w.last_w
            for rd in w.readers.values():
                deps[id(rd)] = rd
        key = eng if dma is None else ("dma", dma)
        for r in reads:
            r.readers[key] = o
        for w in writes:
            w.last_w = o
            w.readers = {}
        o.deps = [d for d in deps.values()
                  if d is not o and not (d.eng == "pe" and eng == "pe" and d.dma is None and dma is None)]
        for d in o.deps:
            d.needed = True
        self.ops.append(o)
        return o

    def run(self):
        nc = self.nc
        cnt = defaultdict(int)
        for o in self.ops:
            if o.dma is not None:
                k = ("dma", o.dma)
                cnt[k] += 16
                o.token = (k, cnt[k])
            elif o.needed:
                k = ("eng", o.eng)
                cnt[k] += 1
                o.token = (k, cnt[k])
        sems = {}
        handles = []
        for i, k in enumerate(cnt):
            sems[k] = nc.alloc_semaphore(f"{self.name}_s{i}")
            handles.append(sems[k])
        with nc.Block() as blk:
            for eng, bname in BLK.items():
                ops_e = [o for o in self.ops if o.eng == eng]
                if not ops_e:
                    continue
                if eng == "sp" and self.lag > 0:
                    outl, pend = [], []
                    for o in ops_e:
                        if o.dma is not None and o.has_reads:
                            pend.append([o, 0])
                        else:
                            outl.append(o)
                            for pp_ in pend:
                                pp_[1] += 1
                            while pend and pend[0][1] >= self.lag:
                                outl.append(pend.pop(0)[0])
                    outl.extend(pp_[0] for pp_ in pend)
                    ops_e = outl

                def body(e, ops_e=ops_e):
                    waited = {}
                    fin = {}
                    for o in ops_e:
                        need = {}
                        for d in o.deps:
                            k, v = d.token
                            if need.get(k, 0) < v:
                                need[k] = v
                        for k, v in need.items():
                            if waited.get(k, 0) < v:
                                e.wait_ge(sems[k], v)
                                waited[k] = v
                        ins = o.fn(e)
                        if o.token is not None:
                            ins.then_inc(sems[o.token[0]], 16 if o.dma is not None else 1)
                            if o.dma is not None:
                                fin[o.token[0]] = o.token[1]
                    for k, v in fin.items():
                        if waited.get(k, 0) < v:
                            e.wait_ge(sems[k], v)

                getattr(blk, bname)(body)
        for r in self.touched:
            r.last_w = None
            r.readers = {}
        self.es.close()
        nc.all_engine_barrier()
        nc.clear_and_free_semaphores(handles)
        nc.all_engine_barrier()


def _dma(out, in_):
    return lambda e: e.dma_start(out=out, in_=in_)


def build(nc, dbg=None, nlayers=L, Lw=L, NEw=NE):
    dbg = dbg or set()

    def din(name, shape, dt=F32):
        return nc.dram_tensor(name, list(shape), dt, kind="ExternalInput").ap()

    def dscr(name, shape, dt):
        kind = "ExternalOutput" if name in dbg else "Internal"
        return nc.dram_tensor(name, list(shape), dt, kind=kind).ap()

    xin = din("xin", [T, D])
    c2T = din("c2T", [128, 16, 2])
    w_mod = din("w_mod", [Lw, D, 6 * D])
    b_mod = din("b_mod", [Lw, 6 * D])
    norm1_g = din("norm1_g", [Lw, D])
    w_in = din("w_in", [Lw, D, DPROJ])
    rpb_tab = din("rpb_tab", [Lw, 8, 128, 5, 896])
    conv_wT = din("conv_wT", [Lw, 128, 8, 3])
    ln_g = din("gmlp_ln_g", [Lw, DB])
    wsT = din("wsT", [Lw, 128, 8, 128])
    b_sp = din("b_spatial", [Lw, DB])
    w_branch = din("w_branch", [Lw, 3, DB, D])
    w_out = din("w_out", [Lw, D, D])
    norm2_g = din("norm2_g", [Lw, D])
    w_router = din("w_router", [Lw, D, NE])
    b_router = din("b_router", [Lw, NE])
    w_eg = din("w_e_gate", [Lw, NEw, D, D])
    w_eu = din("w_e_up", [Lw, NEw, D, D])
    w_ed = din("w_e_down", [Lw, NEw, D, D])
    final_g = din("final_g", [1, D])
    ident_d = din("ident", [128, 128])
    pm_d = din("pm", [128, 128])
    cos_d = din("cos", [128, SEQ])
    sin_d = din("sin", [128, SEQ])
    sel_d = din("selT", [16, 16, 128])
    iota_d = din("iota_s", [128, 256])
    iop_d = din("iop", [128, 2])
    out = nc.dram_tensor("out", [SEQ, D], F32, kind="ExternalOutput").ap()

    MOD = dscr("MOD", [2, 6 * D], F32)
    PROJ = dscr("PROJ", [DPROJ, T], BF16)
    V_tm = dscr("V_tm", [T, DB], BF16)
    VS_tm = dscr("VS_tm", [T, DB], BF16)
    BR = dscr("BR", [3, DB, T], BF16)
    MERGED = dscr("MERGED", [D, T], BF16)
    XA = dscr("XA", [T, D], F32)
    XB = dscr("XB", [T, D], F32)
    H2 = dscr("H2", [T, D], BF16)
    GT = dscr("GT", [NT, 128, NE, 2, 128], BF16)
    XS = dscr("XS", [NE, D, NS], BF16)
    Y = dscr("Y", [NE, CAP, D], BF16)
    YC = dscr("YC", [NE, CAPC, D], BF16)
    DBG_AFF = dscr("DBG_AFF", [128, NT, NE], F32)
    DBG_HT = dscr("DBG_HT", [128, 16, T], BF16)

    top = ExitStack()

    def tsb(name, shape, dt):
        return top.enter_context(nc.sbuf_tensor(name, list(shape), dt))

    ident_f = tsb("ident_f", [128, 128], F32)
    ident_b = tsb("ident_b", [128, 128], BF16)
    ones_b = tsb("ones_b", [128, 128], BF16)
    AFF = tsb("AFF", [128, NT, NE], F32)
    POS = tsb("POS", [128, NT, NE], F32)
    MSK = tsb("MSK", [128, NT, NE], F32)

    st = Stage(nc, "c0")
    r_if, r_ib, r_ob = Res(), Res(), Res()
    st.op("sp", _dma(ident_f[:], ident_d), writes=[r_if], dma="a")
    st.op("dve", lambda e: e.tensor_copy(ident_b[:], ident_f[:]), reads=[r_if], writes=[r_ib])
    st.op("dve", lambda e: e.memset(ones_b[:], 1.0), writes=[r_ob])
    st.run()

    def stage_mod(l):
        st = Stage(nc, f"mod{l}")
        cT = st.sb([128, 16, 2], F32)
        sc = st.sb([128, 16, 2], BF16)
        b2 = st.sb([2, 6 * D], F32)
        msb = st.sb([2, 6 * D], F32)
        ws = [st.sb([128, 16, 512], BF16, "w") for _ in range(3)]
        rw = [Res() for _ in range(3)]
        pss = [st.ps([2, 512]) for _ in range(2)]
        rps = [Res() for _ in range(2)]
        r_c, r_sc, r_b2, r_m = Res(), Res(), Res(), Res()
        st.op("sp", _dma(cT[:], c2T), writes=[r_c], dma="c")
        st.op("sp", _dma(b2[:], b_mod[l:l + 1, :].partition_broadcast(2)), writes=[r_b2], dma="b")
        st.op("act", lambda e: e.activation(sc[:], cT[:], AF.Silu), reads=[r_c], writes=[r_sc])
        NG = 24

        def ld(g):
            st.op("pool", _dma(ws[g % 3][:], w_mod[l, :, g * 512:(g + 1) * 512].rearrange("(kc p) c -> p kc c", p=128)),
                  writes=[rw[g % 3]], dma=f"w{g % 3}")

        ld(0)
        ld(1)
        for g in range(NG):
            if g + 2 < NG:
                ld(g + 2)
            w = ws[g % 3]
            p = pss[g % 2]

            def mm(e, w=w, p=p):
                for kc in range(16):
                    ins = e.matmul(p[:], sc[:, kc, :], w[:, kc, :], start=(kc == 0), stop=(kc == 15))
                return ins

            st.op("pe", mm, reads=[rw[g % 3], r_sc], writes=[rps[g % 2]])
            st.op("dve", lambda e, p=p, g=g: e.tensor_tensor(msb[:, g * 512:(g + 1) * 512], p[:], b2[:, g * 512:(g + 1) * 512], ALU.add),
                  reads=[rps[g % 2], r_b2], writes=[r_m])
        st.op("sp", _dma(MOD, msb[:]), reads=[r_m], dma="o")
        st.run()

    def load_bc(st, dst_ap, src_row_ap, res, key):
        st.op("sp", _dma(dst_ap, src_row_ap.partition_broadcast(128)), writes=[res], dma=key)

    def stage_norm(l, which, Xsrc, hT, r_hT):
        st = Stage(nc, f"n{which}_{l}")
        st.lag = 1
        gsrc = norm1_g if which == 1 else norm2_g
        jo = 0 if which == 1 else 3
        gm = st.sb([128, 2, D], F32)
        sh = st.sb([128, 2, D], F32)
        gb = st.sb([128, D], F32)
        r_gm, r_sh, r_gb = Res(), Res(), Res()
        load_bc(st, gb[:], gsrc[l:l + 1, :], r_gb, "g")
        for r in range(2):
            load_bc(st, gm[:, r, :], MOD[r:r + 1, (jo + 1) * D:(jo + 2) * D], r_gm, f"gm{r}")
            load_bc(st, sh[:, r, :], MOD[r:r + 1, jo * D:(jo + 1) * D], r_sh, f"sh{r}")
            st.op("dve", lambda e, r=r: e.scalar_tensor_tensor(gm[:, r, :], gm[:, r, :], 1.0, gb[:], ALU.add, ALU.mult),
                  reads=[r_gm, r_gb], writes=[r_gm])
        xt = [st.sb([128, D], F32, "x") for _ in range(2)]
        rx = [Res() for _ in range(2)]
        junk = st.sb([128, D], BF16)
        r_junk = Res()
        ss = [st.sb([128, 4], F32, "ss") for _ in range(2)]
        rss = [Res() for _ in range(2)]
        tt = [st.sb([128, D], F32, "tt") for _ in range(2)]
        rtt = [Res() for _ in range(2)]
        if which == 1:
            hb = [st.sb([128, D], BF16, "hb") for _ in range(2)]
            rhb = [Res() for _ in range(2)]
            pst = [st.ps([128, 8, 128], BF16) for _ in range(4)]
            rpst = [Res() for _ in range(4)]
        else:
            hb = [st.sb([128, D], BF16, "hb") for _ in range(2)]
            rhb = [Res() for _ in range(2)]
            pst = [st.ps([128, 4, 128], F32) for _ in range(4)]
            rpst = [Res() for _ in range(4)]
            h2T = [st.sb([128, 16, 128], F32, "h2T") for _ in range(2)]
            rh2T = [Res() for _ in range(2)]
            wr = st.sb([128, 16, NE], F32)
            r_wr = Res()
            st.op("sp", _dma(wr[:], w_router[l].rearrange("(kc p) e -> p kc e", p=128)), writes=[r_wr], dma="wr")
            brb = st.sb([128, NE], F32)
            r_brb = Res()
            load_bc(st, brb[:], b_router[l:l + 1, :], r_brb, "brb")
            pl = [st.ps([128, NE], F32) for _ in range(2)]
            rpl = [Res() for _ in range(2)]
            lg = [st.sb([128, NE], F32, "lg") for _ in range(2)]
            rlg = [Res() for _ in range(2)]
            sm = [st.sb([128, 4], F32, "sm") for _ in range(2)]
            rsm = [Res() for _ in range(2)]
            r_aff = Res()
        for i in range(NT):
            s = i % 2
            r = 0 if i < 16 else 1
            x_, ss_, t_, h_ = xt[s], ss[s], tt[s], hb[s]
            st.op("sp", _dma(x_[:], Xsrc[i * 128:(i + 1) * 128, :]), writes=[rx[s]], dma=f"x{s}")
            st.op("act", lambda e, x_=x_, ss_=ss_: e.activation(junk[:], x_[:], AF.Square, accum_out=ss_[:, 0:1]),
                  reads=[rx[s]], writes=[r_junk, rss[s]])
            st.op("act", lambda e, ss_=ss_: e.activation(ss_[:, 1:2], ss_[:, 0:1], AF.Sqrt, bias=EPS, scale=1.0 / D),
                  reads=[rss[s]], writes=[rss[s]])
            st.op("dve", lambda e, ss_=ss_: e.reciprocal(ss_[:, 2:3], ss_[:, 1:2]), reads=[rss[s]], writes=[rss[s]])
            st.op("dve", lambda e, x_=x_, ss_=ss_, t_=t_, r=r: e.scalar_tensor_tensor(t_[:], x_[:], ss_[:, 2:3], gm[:, r, :], ALU.mult, ALU.mult),
                  reads=[rx[s], rss[s], r_gm], writes=[rtt[s]])
            if which == 1:
                st.op("pool", lambda e, t_=t_, h_=h_, r=r: e.tensor_tensor(h_[:], t_[:], sh[:, r, :], ALU.add),
                      reads=[rtt[s], r_sh], writes=[rhb[s]])
                for half in range(2):
                    pp = pst[2 * s + half]
                    rp = rpst[2 * s + half]

                    def tr(e, pp=pp, h_=h_, half=half):
                        for j in range(8):
                            kc = half * 8 + j
                            ins = e.transpose(pp[:, j, :], h_[:, kc * 128:(kc + 1) * 128], ident_b[:])
                        return ins

                    st.op("pe", tr, reads=[rhb[s]], writes=[rp])
                    eng = "act" if half == 0 else "dve"
                    if eng == "act":
                        st.op("act", lambda e, pp=pp, half=half, i=i: e.copy(hT[:, half * 8:(half + 1) * 8, i * 128:(i + 1) * 128], pp[:]),
                              reads=[rp], writes=[r_hT])
                    else:
                        st.op("dve", lambda e, pp=pp, half=half, i=i: e.tensor_copy(hT[:, half * 8:(half + 1) * 8, i * 128:(i + 1) * 128], pp[:]),
                              reads=[rp], writes=[r_hT])
            else:
                st.op("pool", lambda e, t_=t_, r=r: e.tensor_tensor(t_[:], t_[:], sh[:, r, :], ALU.add),
                      reads=[rtt[s], r_sh], writes=[rtt[s]])
                st.op("pool", lambda e, t_=t_, h_=h_: e.tensor_copy(h_[:], t_[:]), reads=[rtt[s]], writes=[rhb[s]])
                st.op("sp", _dma(H2[i * 128:(i + 1) * 128, :], h_[:]), reads=[rhb[s]], dma=f"h{s}")
                hT_ = h2T[s]
                for q in range(4):
                    pp = pst[q]

                    def tr(e, pp=pp, t_=t_, q=q):
                        for j in range(4):
                            kc = q * 4 + j
                            ins = e.transpose(pp[:, j, :], t_[:, kc * 128:(kc + 1) * 128], ident_f[:])
                        return ins

                    st.op("pe", tr, reads=[rtt[s]], writes=[rpst[q]])
                    if q % 2 == 0:
                        st.op("act", lambda e, pp=pp, q=q, hT_=hT_: e.copy(hT_[:, q * 4:(q + 1) * 4, :], pp[:]),
                              reads=[rpst[q]], writes=[rh2T[s]])
                    else:
                        st.op("dve", lambda e, pp=pp, q=q, hT_=hT_: e.tensor_copy(hT_[:, q * 4:(q + 1) * 4, :], pp[:]),
                              reads=[rpst[q]], writes=[rh2T[s]])
                pl_ = pl[s]

                def mmr(e, pl_=pl_, hT_=hT_):
                    for kc in range(16):
                        ins = e.matmul(pl_[:], hT_[:, kc, :], wr[:, kc, :], start=(kc == 0), stop=(kc == 15))
                    return ins

                st.op("pe", mmr, reads=[rh2T[s], r_wr], writes=[rpl[s]])
                lg_, sm_ = lg[s], sm[s]
                st.op("dve", lambda e, lg_=lg_, pl_=pl_: e.tensor_tensor(lg_[:], pl_[:], brb[:], ALU.add),
                      reads=[rpl[s], r_brb], writes=[rlg[s]])
                st.op("dve", lambda e, lg_=lg_, sm_=sm_: e.tensor_reduce(sm_[:, 0:1], lg_[:], AX.X, ALU.max, negate=True),
                      reads=[rlg[s]], writes=[rsm[s]])
                st.op("act", lambda e, lg_=lg_, sm_=sm_: e.activation(lg_[:], lg_[:], AF.Exp, bias=sm_[:, 0:1], accum_out=sm_[:, 1:2]),
                      reads=[rlg[s], rsm[s]], writes=[rlg[s], rsm[s]])
                st.op("dve", lambda e, sm_=sm_: e.reciprocal(sm_[:, 2:3], sm_[:, 1:2]), reads=[rsm[s]], writes=[rsm[s]])
                st.op("dve", lambda e, lg_=lg_, sm_=sm_, i=i: e.tensor_scalar(AFF[:, i, :], lg_[:], sm_[:, 2:3], None, ALU.mult),
                      reads=[rlg[s], rsm[s]], writes=[r_aff])
        if which == 2 and "DBG_AFF" in dbg:
            st.op("sp", _dma(DBG_AFF, AFF[:]), reads=[r_aff], dma="dbg")
        if which == 1 and "DBG_HT" in dbg:
            st.op("sp", _dma(DBG_HT, hT[:]), reads=[r_hT], dma="dbg")
        st.run()

    def stage_proj(l, hT, r_hT):
        st = Stage(nc, f"pj{l}")
        ws = [st.sb([128, 16, 512], BF16, "w") for _ in range(3)]
        rw = [Res() for _ in range(3)]
        NPS = 6
        pss = [st.ps([128, 512]) for _ in range(NPS)]
        rps = [Res() for _ in range(NPS)]
        NO = 4
        os_ = [st.sb([128, 512], BF16, "o") for _ in range(NO)]
        ros = [Res() for _ in range(NO)]
        NG = getattr(build, 'proj_ng', DPROJ // 512)
        cntr = [0]

        def ld(g):
            st.op("pool", _dma(ws[g % 3][:], w_in[l, :, g * 512:(g + 1) * 512].rearrange("(kc p) c -> p kc c", p=128)),
                  writes=[rw[g % 3]], dma=f"w{g % 3}")

        def evac_store(p, rp, n_part, n_free, dst):
            k = cntr[0]
            cntr[0] += 1
            o = os_[k % NO]
            ro = ros[k % NO]
            if k % 2 == 0:
                st.op("act", lambda e: e.copy(o[:n_part, :n_free], p[:n_part, :n_free]), reads=[rp], writes=[ro])
            else:
                st.op("dve", lambda e: e.tensor_copy(o[:n_part, :n_free], p[:n_part, :n_free]), reads=[rp], writes=[ro])
            st.op("sp", _dma(dst, o[:n_part, :n_free]), reads=[ro], dma=f"o{k % NO}")

        ld(0)
        ld(1)
        pk = 0
        for g in range(NG):
            if g + 2 < NG:
                ld(g + 2)
            w = ws[g % 3]
            tm = g in (4, 5, 14, 15)
            if not tm:
                for (t0, n) in TGS:
                    for cb in range(4):
                        p = pss[pk % NPS]
                        rp = rps[pk % NPS]
                        pk += 1

                        def mm(e, w=w, p=p, cb=cb, t0=t0, n=n):
                            for kc in range(16):
                                ins = e.matmul(p[:, :n], w[:, kc, cb * 128:(cb + 1) * 128], hT[:, kc, t0:t0 + n],
                                               start=(kc == 0), stop=(kc == 15))
                            return ins

                        st.op("pe", mm, reads=[rw[g % 3], r_hT], writes=[rp])
                        row0 = g * 512 + cb * 128
                        evac_store(p, rp, 128, n, PROJ[row0:row0 + 128, t0:t0 + n])
            else:
                dst_t = V_tm if g in (4, 5) else VS_tm
                c0 = (g - 4) * 512 if g in (4, 5) else (g - 14) * 512
                for i in range(NT):
                    p = pss[pk % NPS]
                    rp = rps[pk % NPS]
                    pk += 1

                    def mm(e, w=w, p=p, i=i):
                        for kc in range(16):
                            ins = e.matmul(p[:], hT[:, kc, i * 128:(i + 1) * 128], w[:, kc, :],
                                           start=(kc == 0), stop=(kc == 15))
                        return ins

                    st.op("pe", mm, reads=[rw[g % 3], r_hT], writes=[rp])
                    evac_store(p, rp, 128, 512, dst_t[i * 128:(i + 1) * 128, c0:c0 + 512])
        st.run()

    def stage_rope(l):
        st = Stage(nc, f"rp{l}")
        st.lag = 2
        pmf = st.sb([128, 128], F32)
        pmb = st.sb([128, 128], BF16)
        cs = st.sb([128, SEQ], F32)
        sn = st.sb([128, SEQ], F32)
        r_pmf, r_pmb, r_cs, r_sn = Res(), Res(), Res(), Res()
        st.op("sp", _dma(pmf[:], pm_d), writes=[r_pmf], dma="pm")
        st.op("sp", _dma(cs[:], cos_d), writes=[r_cs], dma="cs")
        st.op("sp", _dma(sn[:], sin_d), writes=[r_sn], dma="sn")
        st.op("dve", lambda e: e.tensor_copy(pmb[:], pmf[:]), reads=[r_pmf], writes=[r_pmb])
        RD = 4
        qb = [st.sb([128, 512], BF16, "q") for _ in range(RD)]
        rq = [Res() for _ in range(RD)]
        pq = [st.ps([128, 512]) for _ in range(RD)]
        rpq = [Res() for _ in range(RD)]
        t1 = [st.sb([128, 512], F32, "t1") for _ in range(RD)]
        rt1 = [Res() for _ in range(RD)]
        t2 = [st.sb([128, 512], F32, "t2") for _ in range(RD)]
        rt2 = [Res() for _ in range(RD)]
        ob = [st.sb([128, 512], BF16, "ob") for _ in range(RD)]
        rob = [Res() for _ in range(RD)]
        k = 0
        for blk in range(16):
            for tg in range(4):
                s = k % RD
                k += 1
                t0 = tg * 512
                src = PROJ[blk * 128:(blk + 1) * 128, t0:t0 + 512]
                q_, p_, a_, b_, o_ = qb[s], pq[s], t1[s], t2[s], ob[s]
                st.op("sp", _dma(q_[:], src), writes=[rq[s]], dma=f"q{s}")
                st.op("pe", lambda e, q_=q_, p_=p_: e.matmul(p_[:], pmb[:], q_[:], start=True, stop=True),
                      reads=[rq[s], r_pmb], writes=[rpq[s]])
                st.op("dve", lambda e, q_=q_, a_=a_, t0=t0: e.tensor_tensor(a_[:], q_[:], cs[:, t0:t0 + 512], ALU.mult),
                      reads=[rq[s], r_cs], writes=[rt1[s]])
                st.op("dve", lambda e, p_=p_, b_=b_, t0=t0: e.tensor_tensor(b_[:], p_[:], sn[:, t0:t0 + 512], ALU.mult),
                      reads=[rpq[s], r_sn], writes=[rt2[s]])
                st.op("pool", lambda e, a_=a_, b_=b_, o_=o_: e.tensor_tensor(o_[:], a_[:], b_[:], ALU.add),
                      reads=[rt1[s], rt2[s]], writes=[rob[s]])
                st.op("sp", _dma(src, o_[:]), reads=[rob[s]], dma=f"o{s}")
        st.run()

    def stage_attn(l):
        st = Stage(nc, f"at{l}")
        st.lag = 0
        scale = 128 ** -0.5
        qT = [st.sb([128, T], BF16, "q") for _ in range(2)]
        kT = [st.sb([128, T], BF16, "k") for _ in range(2)]
        Vh = [st.sb([128, NT, 128], BF16, "v") for _ in range(2)]
        bt = [st.sb([128, 5, 896], F32, "b") for _ in range(2)]
        oT = [st.sb([128, T], BF16, "o") for _ in range(2)]
        rq = [Res() for _ in range(2)]
        rk = [Res() for _ in range(2)]
        rv = [Res() for _ in range(2)]
        rb = [Res() for _ in range(2)]
        ro = [Res() for _ in range(2)]
        psS = [st.ps([128, 1024]) for _ in range(2)]
        rpS = [Res() for _ in range(2)]
        psT = [st.ps([128, 7, 128], BF16) for _ in range(2)]
        rpT = [Res() for _ in range(2)]
        psO = [st.ps([128, 2, 128]) for _ in range(2)]
        rpO = [Res() for _ in range(2)]
        AD = 4
        sbS = [st.sb([128, 896], F32, "s") for _ in range(AD)]
        rsS = [Res() for _ in range(AD)]
        mx = [st.sb([128, 2], F32, "mx") for _ in range(AD)]
        rmx = [Res() for _ in range(AD)]
        pb = [st.sb([128, 896], BF16, "p") for _ in range(AD)]
        rpb_ = [Res() for _ in range(AD)]
        pT = [st.sb([128, 7, 128], BF16, "pT") for _ in range(AD)]
        rpTs = [Res() for _ in range(AD)]
        ri = [st.sb([128, 128], F32, "ri") for _ in range(AD)]
        rri = [Res() for _ in range(AD)]
        units = [(h, qt) for h in range(8) for qt in range(NT)]
        NU = len(units)

        def geom(qt):
            if qt < 16:
                tb = min(max(qt - 2, 0), 11)
                cls = {0: 0, 1: 1, 14: 3, 15: 4}.get(qt, 2)
                segs = [(0, tb * 128, 512), (512, tb * 128 + 512, 128), (640, SEQ, 256)]
                vts = [tb + j for j in range(5)] + [16, 17]
                return segs, vts, 896, cls
            return [(0, SEQ, 256)], [16, 17], 256, None

        def stA(u):
            h, qt = units[u]
            hs = h % 2
            q_, k_, v_, b_ = qT[hs], kT[hs], Vh[hs], bt[hs]
            if qt == 0:
                st.op("sp", _dma(q_[:], PROJ[h * 128:(h + 1) * 128, :]), writes=[rq[hs]], dma=f"q{hs}")
                st.op("sp", _dma(k_[:], PROJ[DB + h * 128:DB + (h + 1) * 128, :]), writes=[rk[hs]], dma=f"k{hs}")
                st.op("sp", _dma(v_[:], V_tm[:, h * 128:(h + 1) * 128].rearrange("(i p) d -> p i d", p=128)),
                      writes=[rv[hs]], dma=f"v{hs}")
                st.op("sp", _dma(b_[:], rpb_tab[l, h]), writes=[rb[hs]], dma=f"b{hs}")
            s, sB = u % 2, u % AD
            S_, sb_, mx_, p_ = psS[s], sbS[sB], mx[sB], pb[sB]
            segs, vts, nk, cls = geom(qt)

            def mmS(e, S_=S_, q_=q_, k_=k_, qt=qt, segs=segs):
                for (c0, k0, n) in segs:
                    ins = e.matmul(S_[:, c0:c0 + n], q_[:, qt * 128:(qt + 1) * 128], k_[:, k0:k0 + n], start=True, stop=True)
                return ins

            st.op("pe", mmS, reads=[rq[hs], rk[hs]], writes=[rpS[s]])
            if cls is not None:
                st.op("dve", lambda e, S_=S_, sb_=sb_, b_=b_, cls=cls: e.scalar_tensor_tensor(sb_[:, :896], S_[:, :896], scale, b_[:, cls, :], ALU.mult, ALU.add),
                      reads=[rpS[s], rb[hs]], writes=[rsS[sB]])
            else:
                st.op("dve", lambda e, S_=S_, sb_=sb_: e.tensor_scalar(sb_[:, :256], S_[:, :256], scale, None, ALU.mult),
                      reads=[rpS[s]], writes=[rsS[sB]])
            st.op("dve", lambda e, sb_=sb_, mx_=mx_, nk=nk: e.tensor_reduce(mx_[:, 0:1], sb_[:, :nk], AX.X, ALU.max, negate=True),
                  reads=[rsS[sB]], writes=[rmx[sB]])
            st.op("act", lambda e, sb_=sb_, mx_=mx_, p_=p_, nk=nk: e.activation(p_[:, :nk], sb_[:, :nk], AF.Exp, bias=mx_[:, 0:1]),
                  reads=[rsS[sB], rmx[sB]], writes=[rpb_[sB]])

        def stB(u):
            h, qt = units[u]
            s, sB = u % 2, u % AD
            Ts_, p_, pT_ = psT[s], pb[sB], pT[sB]
            segs, vts, nk, cls = geom(qt)
            nj = nk // 128

            def trp(e, Ts_=Ts_, p_=p_, nj=nj):
                for j in range(nj):
                    ins = e.transpose(Ts_[:, j, :], p_[:, j * 128:(j + 1) * 128], ident_b[:])
                return ins

            st.op("pe", trp, reads=[rpb_[sB]], writes=[rpT[s]])
            st.op("act", lambda e, Ts_=Ts_, pT_=pT_, nj=nj: e.copy(pT_[:, :nj, :], Ts_[:, :nj, :]),
                  reads=[rpT[s]], writes=[rpTs[sB]])

        def stC(u):
            h, qt = units[u]
            hs = h % 2
            v_, o_ = Vh[hs], oT[hs]
            s, sB = u % 2, u % AD
            O_, pT_, ri_ = psO[s], pT[sB], ri[sB]
            segs, vts, nk, cls = geom(qt)

            def mmO(e, O_=O_, v_=v_, pT_=pT_, vts=vts):
                n = len(vts)
                for j, vt in enumerate(vts):
                    e.matmul(O_[:, 0, :], v_[:, vt, :], pT_[:, j, :], start=(j == 0), stop=(j == n - 1))
                for j in range(n):
                    ins = e.matmul(O_[:, 1, :], ones_b[:], pT_[:, j, :], start=(j == 0), stop=(j == n - 1))
                return ins

            st.op("pe", mmO, reads=[rv[hs], rpTs[sB]], writes=[rpO[s]])
            st.op("dve", lambda e, O_=O_, ri_=ri_: e.reciprocal(ri_[:], O_[:, 1, :]), reads=[rpO[s]], writes=[rri[sB]])
            st.op("dve", lambda e, O_=O_, ri_=ri_, o_=o_, qt=qt: e.tensor_tensor(o_[:, qt * 128:(qt + 1) * 128], O_[:, 0, :], ri_[:], ALU.mult),
                  reads=[rpO[s], rri[sB]], writes=[ro[hs]])
            if qt == NT - 1:
                st.op("sp", _dma(BR[0, h * 128:(h + 1) * 128, :], o_[:]), reads=[ro[hs]], dma=f"o{hs}")

        for t in range(NU + 2):
            if t < NU:
                stA(t)
            if 0 <= t - 1 < NU:
                stB(t - 1)
            if 0 <= t - 2 < NU:
                stC(t - 2)
        st.run()

    def stage_conv(l):
        st = Stage(nc, f"cv{l}")
        st.lag = 3
        cw = st.sb([128, 8, 3], F32)
        r_cw = Res()
        st.op("sp", _dma(cw[:], conv_wT[l]), writes=[r_cw], dma="cw")
        xc = [st.sb([128, T], BF16, "xc") for _ in range(2)]
        bg = [st.sb([128, T], BF16, "bg") for _ in range(2)]
        cg = [st.sb([128, T], BF16, "cg") for _ in range(2)]
        z = [st.sb([128, T], F32, "z") for _ in range(2)]
        y = [st.sb([128, T], F32, "y") for _ in range(2)]
        o = [st.sb([128, T], BF16, "o") for _ in range(2)]
        rxc, rbg, rcg, rz, ry, ro = ([Res() for _ in range(2)] for _ in range(6))
        for cb in range(8):
            s = cb % 2
            xc_, bg_, cg_, z_, y_, o_ = xc[s], bg[s], cg[s], z[s], y[s], o[s]
            st.op("sp", _dma(xc_[:], PROJ[3072 + cb * 128:3072 + (cb + 1) * 128, :]), writes=[rxc[s]], dma=f"a{s}")
            st.op("sp", _dma(bg_[:], PROJ[4096 + cb * 128:4096 + (cb + 1) * 128, :]), writes=[rbg[s]], dma=f"b{s}")
            st.op("sp", _dma(cg_[:], PROJ[5120 + cb * 128:5120 + (cb + 1) * 128, :]), writes=[rcg[s]], dma=f"c{s}")
            st.op("pool", lambda e, z_=z_, cg_=cg_, xc_=xc_: e.tensor_tensor(z_[:], cg_[:], xc_[:], ALU.mult),
                  reads=[rxc[s], rcg[s]], writes=[rz[s]])
            st.op("dve", lambda e, y_=y_, z_=z_, cb=cb: e.tensor_scalar(y_[:], z_[:], cw[:, cb, 1:2], None, ALU.mult),
                  reads=[rz[s], r_cw], writes=[ry[s]])
            for (a, b) in ((0, SEQ), (SEQ, T)):
                st.op("dve", lambda e, y_=y_, z_=z_, cb=cb, a=a, b=b: e.scalar_tensor_tensor(y_[:, a + 1:b], z_[:, a:b - 1], cw[:, cb, 0:1], y_[:, a + 1:b], ALU.mult, ALU.add),
                      reads=[rz[s], ry[s], r_cw], writes=[ry[s]])
                st.op("dve", lambda e, y_=y_, z_=z_, cb=cb, a=a, b=b: e.scalar_tensor_tensor(y_[:, a:b - 1], z_[:, a + 1:b], cw[:, cb, 2:3], y_[:, a:b - 1], ALU.mult, ALU.add),
                      reads=[rz[s], ry[s], r_cw], writes=[ry[s]])
            st.op("pool", lambda e, o_=o_, bg_=bg_, y_=y_: e.tensor_tensor(o_[:], bg_[:], y_[:], ALU.mult),
                  reads=[rbg[s], ry[s]], writes=[ro[s]])
            st.op("sp", _dma(BR[1, cb * 128:(cb + 1) * 128, :], o_[:]), reads=[ro[s]], dma=f"o{s}")
        st.run()

    def gelu_ops(st, eng2, x_ap, tmp_ap, out_ap, rx, rtmp, rout):
        st.op(eng2, lambda e: e.tensor_tensor(tmp_ap, x_ap, x_ap, ALU.mult), reads=[rx], writes=[rtmp])
        st.op("dve", lambda e: e.tensor_scalar(tmp_ap, tmp_ap, 0.044715, 1.0, ALU.mult, ALU.add), reads=[rtmp], writes=[rtmp])
        st.op(eng2, lambda e: e.tensor_tensor(tmp_ap, tmp_ap, x_ap, ALU.mult), reads=[rtmp, rx], writes=[rtmp])
        st.op("act", lambda e: e.activation(tmp_ap, tmp_ap, AF.Sigmoid, scale=1.5957691216057308), reads=[rtmp], writes=[rtmp])
        st.op("dve", lambda e: e.tensor_tensor(out_ap, tmp_ap, x_ap, ALU.mult), reads=[rtmp, rx], writes=[rout])

    def stage_gmlp(l):
        st = Stage(nc, f"gm{l}")
        st.lag = 0
        lng = st.sb([128, DB], F32)
        bsb = st.sb([128, DB], F32)
        wsf = st.sb([128, DB], F32)
        wsb = st.sb([128, 8, 128], BF16)
        r_lng, r_bsb, r_wsf, r_wsb = Res(), Res(), Res(), Res()
        load_bc(st, lng[:], ln_g[l:l + 1, :], r_lng, "lng")
        load_bc(st, bsb[:], b_sp[l:l + 1, :], r_bsb, "bsb")
        st.op("sp", _dma(wsf[:], wsT[l].rearrange("q g p -> q (g p)")), writes=[r_wsf], dma="ws")
        st.op("dve", lambda e: e.tensor_copy(wsb[:].rearrange("q g p -> q (g p)"), wsf[:]), reads=[r_wsf], writes=[r_wsb])
        GD = 3

        def mk(shape, dt, nm):
            return [st.sb(shape, dt, nm) for _ in range(GD)], [Res() for _ in range(GD)]

        vb, rvb = mk([128, DB], BF16, "v")
        ub, rub = mk([128, DB], BF16, "u")
        tvL, r_tvL = mk([128, DB], F32, "tv")
        gvL, r_gvL = mk([128, DB], F32, "gv")
        tuL, r_tuL = mk([128, DB], F32, "tu")
        guL, r_guL = mk([128, DB], F32, "gu")
        sttL, r_sttL = mk([128, 2, 6], F32, "stt")
        mvL, r_mvL = mk([128, 4], F32, "mv")
        vn, rvn = mk([128, DB], BF16, "vn")
        soL, r_soL = mk([128, DB], F32, "so")
        ob, rob = mk([128, DB], BF16, "o")
        pss = [st.ps([128, 4, 128]) for _ in range(4)]
        rps = [Res() for _ in range(4)]

        def ph1(i):
            s = i % GD
            v_, u_ = vb[s], ub[s]
            st.op("sp", _dma(v_[:], VS_tm[i * 128:(i + 1) * 128, :]), writes=[rvb[s]], dma=f"v{s}")
            st.op("sp", _dma(u_[:].rearrange("p (b t) -> p b t", b=8),
                             PROJ[6144:7168, i * 128:(i + 1) * 128].rearrange("(b p) t -> p b t", p=128)),
                  writes=[rub[s]], dma=f"u{s}")
            gelu_ops(st, "pool", v_[:], tvL[s][:], gvL[s][:], rvb[s], r_tvL[s], r_gvL[s])
            gelu_ops(st, "pool", u_[:], tuL[s][:], guL[s][:], rub[s], r_tuL[s], r_guL[s])

        def ph2(i):
            s = i % GD
            gv, stt, mv, vn_ = gvL[s], sttL[s], mvL[s], vn[s]
            r_gv, r_stt, r_mv = r_gvL[s], r_sttL[s], r_mvL[s]
            for hh in range(2):
                st.op("dve", lambda e, hh=hh, stt=stt, gv=gv: e.bn_stats(stt[:, hh, :], gv[:, hh * 512:(hh + 1) * 512]), reads=[r_gv], writes=[r_stt])
            st.op("dve", lambda e, mv=mv, stt=stt: e.bn_aggr(mv[:, 0:2], stt[:].rearrange("p a b -> p (a b)")), reads=[r_stt], writes=[r_mv])
            st.op("act", lambda e, mv=mv: e.activation(mv[:, 2:3], mv[:, 1:2], AF.Sqrt, bias=EPS, scale=1.0), reads=[r_mv], writes=[r_mv])
            st.op("dve", lambda e, mv=mv: e.reciprocal(mv[:, 3:4], mv[:, 2:3]), reads=[r_mv], writes=[r_mv])
            st.op("dve", lambda e, gv=gv, mv=mv: e.tensor_scalar(gv[:], gv[:], mv[:, 0:1], mv[:, 3:4], ALU.subtract, ALU.mult), reads=[r_gv, r_mv], writes=[r_gv])
            st.op("pool", lambda e, vn_=vn_, gv=gv: e.tensor_tensor(vn_[:], gv[:], lng[:], ALU.mult), reads=[r_gv, r_lng], writes=[rvn[s]])
            for half in range(2):
                pp = pss[2 * (i % 2) + half]

                def mm(e, pp=pp, half=half, vn_=vn_):
                    for j in range(4):
                        gi = half * 4 + j
                        ins = e.matmul(pp[:, j, :], vn_[:, gi * 128:(gi + 1) * 128], wsb[:, gi, :], start=True, stop=True)
                    return ins

                st.op("pe", mm, reads=[rvn[s], r_wsb], writes=[rps[2 * (i % 2) + half]])

        def ph3(i):
            s = i % GD
            so, gu, o_ = soL[s], guL[s], ob[s]
            for half in range(2):
                pp = pss[2 * (i % 2) + half]
                st.op("dve", lambda e, pp=pp, half=half, so=so: e.tensor_tensor(so[:, half * 512:(half + 1) * 512], pp[:].rearrange("p a b -> p (a b)"), bsb[:, half * 512:(half + 1) * 512], ALU.add),
                      reads=[rps[2 * (i % 2) + half], r_bsb], writes=[r_soL[s]])
            st.op("pool", lambda e, o_=o_, so=so, gu=gu: e.tensor_tensor(o_[:], so[:], gu[:], ALU.mult), reads=[r_soL[s], r_guL[s]], writes=[rob[s]])
            st.op("sp", _dma(BR[2, :, i * 128:(i + 1) * 128].rearrange("(b p) t -> p b t", p=128),
                             o_[:].rearrange("p (b t) -> p b t", b=8)), reads=[rob[s]], dma=f"o{s}")

        for t in range(NT + 2):
            if t < NT:
                ph1(t)
            if 0 <= t - 1 < NT:
                ph2(t - 1)
            if 0 <= t - 2 < NT:
                ph3(t - 2)
        st.run()

    def stage_merge1(l):
        st = Stage(nc, f"m1{l}")
        st.lag = 2
        wb = [st.sb([128, 3, 8, 512], BF16, "w") for _ in range(2)]
        rwb = [Res() for _ in range(2)]
        br = [st.sb([128, 3, 8, 512], BF16, "br") for _ in range(2)]
        rbr = [Res() for _ in range(2)]
        MD = 4
        gl = [st.sb([128, 3, 512], BF16, "gl") for _ in range(MD)]
        rgl = [Res() for _ in range(MD)]
        sg = [st.sb([128, 3, 512], F32, "sg") for _ in range(MD)]
        rsg = [Res() for _ in range(MD)]
        pss = [st.ps([128, 512]) for _ in range(6)]
        rps = [Res() for _ in range(6)]
        ta = [st.sb([128, 512], F32, "ta") for _ in range(MD)]
        tb_ = [st.sb([128, 512], F32, "tb") for _ in range(MD)]
        rta, rtb = [Res() for _ in range(MD)], [Res() for _ in range(MD)]
        mb = [st.sb([128, 512], BF16, "mb") for _ in range(MD)]
        rmb = [Res() for _ in range(MD)]
        GLv = PROJ[8192:DPROJ, :].rearrange("(i r) t -> r i t", i=3)
        kk = 0
        kb = 0
        def ldw(dg):
            for i in range(3):
                st.op("pool", _dma(wb[dg % 2][:, i], w_branch[l, i, :, dg * 512:(dg + 1) * 512].rearrange("(kc p) d -> p kc d", p=128)),
                      writes=[rwb[dg % 2]], dma=f"w{dg % 2}{i}")

        ldw(0)
        for dg in range(4):
            ws_ = dg % 2
            if dg + 1 < 4:
                ldw(dg + 1)
            for (t0, n) in TGS:
                bs = kb % 2
                kb += 1
                for i in range(3):
                    st.op("sp", _dma(br[bs][:, i, :, :n], BR[i, :, t0:t0 + n].rearrange("(kc p) t -> p kc t", p=128)),
                          writes=[rbr[bs]], dma=f"b{bs}{i}")
                for db in range(4):
                    s = kk % 2
                    sB = kk % MD
                    kk += 1
                    r0 = dg * 512 + db * 128
                    gl_, sg_, ta_, tb2, mb_ = gl[sB], sg[sB], ta[sB], tb_[sB], mb[sB]
                    st.op("sp", _dma(gl_[:, :, :n], GLv[r0:r0 + 128, :, t0:t0 + n]), writes=[rgl[sB]], dma=f"g{sB}")
                    st.op("act", lambda e, gl_=gl_, sg_=sg_, n=n: e.activation(sg_[:, :, :n], gl_[:, :, :n], AF.Sigmoid),
                          reads=[rgl[sB]], writes=[rsg[sB]])
                    for i in range(3):
                        p = pss[3 * s + i]

                        def mm(e, p=p, i=i, ws_=ws_, bs=bs, db=db, n=n):
                            for kc in range(8):
                                ins = e.matmul(p[:, :n], wb[ws_][:, i, kc, db * 128:(db + 1) * 128], br[bs][:, i, kc, :n],
                                               start=(kc == 0), stop=(kc == 7))
                            return ins

                        st.op("pe", mm, reads=[rwb[ws_], rbr[bs]], writes=[rps[3 * s + i]])
                    p0, p1, p2 = pss[3 * s], pss[3 * s + 1], pss[3 * s + 2]
                    st.op("dve", lambda e, ta_=ta_, p0=p0, sg_=sg_, n=n: e.tensor_tensor(ta_[:, :n], p0[:, :n], sg_[:, 0, :n], ALU.mult),
                          reads=[rps[3 * s], rsg[sB]], writes=[rta[sB]])
                    st.op("dve", lambda e, tb2=tb2, p1=p1, sg_=sg_, n=n: e.tensor_tensor(tb2[:, :n], p1[:, :n], sg_[:, 1, :n], ALU.mult),
                          reads=[rps[3 * s + 1], rsg[sB]], writes=[rtb[sB]])
                    st.op("pool", lambda e, ta_=ta_, tb2=tb2, n=n: e.tensor_tensor(ta_[:, :n], ta_[:, :n], tb2[:, :n], ALU.add),
                          reads=[rta[sB], rtb[sB]], writes=[rta[sB]])
                    st.op("dve", lambda e, tb2=tb2, p2=p2, sg_=sg_, n=n: e.tensor_tensor(tb2[:, :n], p2[:, :n], sg_[:, 2, :n], ALU.mult),
                          reads=[rps[3 * s + 2], rsg[sB]], writes=[rtb[sB]])
                    st.op("pool", lambda e, ta_=ta_, tb2=tb2, mb_=mb_, n=n: e.tensor_tensor(mb_[:, :n], ta_[:, :n], tb2[:, :n], ALU.add),
                          reads=[rta[sB], rtb[sB]], writes=[rmb[sB]])
                    st.op("sp", _dma(MERGED[r0:r0 + 128, t0:t0 + n], mb_[:, :n]), reads=[rmb[sB]], dma=f"o{sB}")
        st.run()

    def stage_merge2(l, Xsrc):
        st = Stage(nc, f"m2{l}")
        st.lag = 2
        wo = [st.sb([128, 16, 512], BF16, "w") for _ in range(2)]
        rwo = [Res() for _ in range(2)]
        g1 = st.sb([128, 2, D], F32)
        r_g1 = Res()
        for r in range(2):
            load_bc(st, g1[:, r, :], MOD[r:r + 1, 2 * D:3 * D], r_g1, f"g{r}")
        mT = [st.sb([128, 16, 512], BF16, "m") for _ in range(2)]
        rmT = [Res() for _ in range(2)]
        xt = [st.sb([128, 512], F32, "x") for _ in range(4)]
        rxt = [Res() for _ in range(4)]
        xo = [st.sb([128, 512], F32, "xo") for _ in range(4)]
        rxo = [Res() for _ in range(4)]
        tt = [st.sb([128, 512], F32, "t") for _ in range(4)]
        rtt = [Res() for _ in range(4)]
        pss = [st.ps([128, 512]) for _ in range(4)]
        rps = [Res() for _ in range(4)]
        pk = 0
        mk = 0
        def ldwo(cg):
            st.op("pool", _dma(wo[cg % 2][:], w_out[l, :, cg * 512:(cg + 1) * 512].rearrange("(kc p) d -> p kc d", p=128)),
                  writes=[rwo[cg % 2]], dma=f"w{cg % 2}")

        ldwo(0)
        for cg in range(4):
            wsl = cg % 2
            w_ = wo[wsl]
            if cg + 1 < 4:
                ldwo(cg + 1)
            for gi, (t0, n) in enumerate(TGS):
                ms = mk % 2
                mk += 1
                m_ = mT[ms]
                st.op("sp", _dma(m_[:, :, :n], MERGED[:, t0:t0 + n].rearrange("(kc p) t -> p kc t", p=128)),
                      writes=[rmT[ms]], dma=f"m{ms}")
                for j in range(n // 128):
                    i = t0 // 128 + j
                    r = 0 if i < 16 else 1
                    s = pk % 4
                    p = pss[pk % 4]
                    rp = rps[pk % 4]
                    pk += 1
                    x_, xo_, t_ = xt[s], xo[s], tt[s]
                    st.op("sp", _dma(x_[:], Xsrc[i * 128:(i + 1) * 128, cg * 512:(cg + 1) * 512]), writes=[rxt[s]], dma=f"x{s}")

                    def mm(e, p=p, m_=m_, w_=w_, j=j):
                        for kc in range(16):
                            ins = e.matmul(p[:], m_[:, kc, j * 128:(j + 1) * 128], w_[:, kc, :],
                                           start=(kc == 0), stop=(kc == 15))
                        return ins

                    st.op("pe", mm, reads=[rmT[ms], rwo[wsl]], writes=[rp])
                    st.op("dve", lambda e, p=p, t_=t_, r=r, cg=cg: e.tensor_tensor(t_[:], p[:], g1[:, r, cg * 512:(cg + 1) * 512], ALU.mult),
                          reads=[rp, r_g1], writes=[rtt[s]])
                    st.op("pool", lambda e, t_=t_, x_=x_, xo_=xo_: e.tensor_tensor(xo_[:], t_[:], x_[:], ALU.add),
                          reads=[rtt[s], rxt[s]], writes=[rxo[s]])
                    st.op("sp", _dma(XA[i * 128:(i + 1) * 128, cg * 512:(cg + 1) * 512], xo_[:]), reads=[rxo[s]], dma=f"o{s}")
        st.run()

    def stage_route(l):
        st = Stage(nc, f"rt{l}")
        affT = st.sb([16, T], F32)
        r_affT = Res()
        pst = [st.ps([16, 512]) for _ in range(2)]
        rpst = [Res() for _ in range(2)]
        r_aff = Res()
        for q in range(5):
            p = pst[q % 2]
            ntile = 4 if q < 4 else 2

            def tr(e, p=p, q=q, ntile=ntile):
                for j in range(ntile):
                    ins = e.transpose(p[:, j * 128:(j + 1) * 128], AFF[:, q * 4 + j, :], ident_f[:])
                return ins

            st.op("pe", tr, reads=[r_aff], writes=[rpst[q % 2]])
            st.op("act", lambda e, p=p, q=q, ntile=ntile: e.copy(affT[:, q * 512:q * 512 + ntile * 128], p[:, :ntile * 128]),
                  reads=[rpst[q % 2]], writes=[r_affT])
        work = st.sb([16, SEQ], F32)
        workc = st.sb([16, CTX], F32)
        m8 = st.sb([16, 8], F32)
        m8c = st.sb([16, 8], F32)
        r_work, r_workc, r_m8, r_m8c = Res(), Res(), Res(), Res()
        st.op("dve", lambda e: e.tensor_copy(work[:], affT[:, :SEQ]), reads=[r_affT], writes=[r_work])
        st.op("pool", lambda e: e.tensor_copy(workc[:], affT[:, SEQ:]), reads=[r_affT], writes=[r_workc])
        for it in range(CAP // 8):
            st.op("dve", lambda e: e.max(m8[:], work[:]), reads=[r_work], writes=[r_m8])
            if it < CAP // 8 - 1:
                st.op("dve", lambda e: e.match_replace(work[:], m8[:], work[:], -1.0), reads=[r_work, r_m8], writes=[r_work])
        for it in range(CAPC // 8):
            st.op("dve", lambda e: e.max(m8c[:], workc[:]), reads=[r_workc], writes=[r_m8c])
            if it < CAPC // 8 - 1:
                st.op("dve", lambda e: e.match_replace(workc[:], m8c[:], workc[:], -1.0), reads=[r_workc, r_m8c], writes=[r_workc])
        maskT = st.sb([16, T], F32)
        gateT = st.sb([16, T], F32)
        posT = st.sb([16, T], F32)
        onesr = st.sb([16, SEQ], F32)
        r_mask, r_gate, r_pos, r_ones = Res(), Res(), Res(), Res()
        st.op("pool", lambda e: e.memset(onesr[:], 1.0), writes=[r_ones])
        st.op("dve", lambda e: e.tensor_scalar(maskT[:, :SEQ], affT[:, :SEQ], m8[:, 7:8], None, ALU.is_ge), reads=[r_affT, r_m8], writes=[r_mask])
        st.op("dve", lambda e: e.tensor_scalar(maskT[:, SEQ:], affT[:, SEQ:], m8c[:, 7:8], None, ALU.is_ge), reads=[r_affT, r_m8c], writes=[r_mask])
        st.op("dve", lambda e: e.tensor_tensor(gateT[:], maskT[:], affT[:], ALU.mult), reads=[r_mask, r_affT], writes=[r_gate])
        st.op("dve", lambda e: e.tensor_tensor_scan(posT[:, :SEQ], onesr[:, :SEQ], maskT[:, :SEQ], 0.0, ALU.mult, ALU.add),
              reads=[r_mask, r_ones], writes=[r_pos])
        st.op("dve", lambda e: e.tensor_tensor_scan(posT[:, SEQ:], onesr[:, :CTX], maskT[:, SEQ:], 0.0, ALU.mult, ALU.add),
              reads=[r_mask, r_ones], writes=[r_pos])
        st.op("dve", lambda e: e.tensor_tensor(posT[:], posT[:], maskT[:], ALU.subtract), reads=[r_pos, r_mask], writes=[r_pos])
        ptm = [st.ps([128, 2, NE]) for _ in range(2)]
        rptm = [Res() for _ in range(2)]
        r_P, r_M = Res(), Res()
        for i in range(NT):
            s = i % 2
            p = ptm[s]

            def tr2(e, p=p, i=i):
                e.transpose(p[:, 0, :], posT[:, i * 128:(i + 1) * 128], ident_f[:16, :16])
                return e.transpose(p[:, 1, :], maskT[:, i * 128:(i + 1) * 128], ident_f[:16, :16])

            st.op("pe", tr2, reads=[r_pos, r_mask], writes=[rptm[s]])
            st.op("act", lambda e, p=p, i=i: e.copy(POS[:, i, :], p[:, 0, :]), reads=[rptm[s]], writes=[r_P])
            st.op("dve", lambda e, p=p, i=i: e.tensor_copy(MSK[:, i, :], p[:, 1, :]), reads=[rptm[s]], writes=[r_M])
        sel = st.sb([16, 16, 128], F32)
        iop = st.sb([128, 2], F32)
        r_sel, r_iop = Res(), Res()
        st.op("sp", _dma(sel[:], sel_d), writes=[r_sel], dma="sel")
        selb = st.sb([16, 16, 128], BF16)
        posb = st.sb([16, T], BF16)
        gateb = st.sb([16, T], BF16)
        r_selb, r_posb, r_gateb = Res(), Res(), Res()
        st.op("act", lambda e: e.copy(selb[:].rearrange("a b c -> a (b c)"), sel[:].rearrange("a b c -> a (b c)")), reads=[r_sel], writes=[r_selb])
        st.op("act", lambda e: e.copy(posb[:], posT[:]), reads=[r_pos], writes=[r_posb])
        st.op("act", lambda e: e.copy(gateb[:], gateT[:]), reads=[r_gate], writes=[r_gateb])
        st.op("sp", _dma(iop[:], iop_d), writes=[r_iop], dma="iop")
        pbp = [st.ps([128, 512]) for _ in range(2)]
        pbg = [st.ps([128, 512]) for _ in range(2)]
        rpbp, rpbg = [Res() for _ in range(2)], [Res() for _ in range(2)]
        gbs = [st.sb([128, 512], F32, "gb") for _ in range(2)]
        rgbs = [Res() for _ in range(2)]
        gto = [st.sb([128, 2, 512], BF16, "gt") for _ in range(2)]
        rgto = [Res() for _ in range(2)]
        k = 0
        for ex in range(NE):
            for (t0, n) in TGS:
                s = k % 2
                k += 1
                pp, pg, gb_, go_ = pbp[s], pbg[s], gbs[s], gto[s]
                st.op("pe", lambda e, pp=pp, ex=ex, t0=t0, n=n: e.matmul(pp[:, :n], selb[:, ex, :], posb[:, t0:t0 + n], start=True, stop=True),
                      reads=[r_selb, r_posb], writes=[rpbp[s]])
                st.op("pe", lambda e, pg=pg, ex=ex, t0=t0, n=n: e.matmul(pg[:, :n], selb[:, ex, :], gateb[:, t0:t0 + n], start=True, stop=True),
                      reads=[r_selb, r_gateb], writes=[rpbg[s]])
                st.op("act", lambda e, pg=pg, gb_=gb_, n=n: e.copy(gb_[:, :n], pg[:, :n]), reads=[rpbg[s]], writes=[rgbs[s]])
                for sti in range(2):
                    st.op("dve", lambda e, pp=pp, gb_=gb_, go_=go_, sti=sti, n=n: e.scalar_tensor_tensor(go_[:, sti, :n], pp[:, :n], iop[:, sti:sti + 1], gb_[:, :n], ALU.is_equal, ALU.mult),
                          reads=[rpbp[s], rgbs[s], r_iop], writes=[rgto[s]])
                for sti in range(2):
                    st.op("sp", _dma(GT[t0 // 128:(t0 + n) // 128, :, ex, sti, :].rearrange("i p t -> p i t"),
                                     go_[:, sti, :n].rearrange("p (i t) -> p i t", t=128)), reads=[rgto[s]], dma=f"o{s}{sti}")
        st.run()

    def stage_gather(l):
        st = Stage(nc, f"ga{l}")
        h2 = st.sb([128, NT, D], BF16)
        r_h2 = Res()
        for q in range(6):
            st.op("sp", _dma(h2[:, q * 3:(q + 1) * 3, :], H2[q * 384:(q + 1) * 384, :].rearrange("(i p) d -> p i d", p=128)),
                  writes=[r_h2], dma=f"h{q}")
        io = st.sb([128, 256], F32)
        r_io = Res()
        st.op("sp", _dma(io[:], iota_d), writes=[r_io], dma="io")
        S = [st.sb([128, 16, 256], BF16, "S") for _ in range(3)]
        Sc = [st.sb([128, 2, 32], BF16, "Sc") for _ in range(3)]
        rS = [Res() for _ in range(3)]
        xs = [st.sb([128, 16, NS], BF16, "xs") for _ in range(3)]
        rxs = [Res() for _ in range(3)]
        pss = [st.ps([128, 512]) for _ in range(4)]
        rps = [Res() for _ in range(4)]
        r_P, r_M = Res(), Res()
        pk = 0
        for ex in range(NE):
            s = ex % 3
            S_, Sc_, xs_ = S[s], Sc[s], xs[s]
            for tt in range(16):
                eng = "pool" if tt % 4 == 3 else "dve"
                st.op(eng, lambda e, S_=S_, tt=tt, ex=ex: e.tensor_scalar(S_[:, tt, :], io[:], POS[:, tt, ex:ex + 1], MSK[:, tt, ex:ex + 1], ALU.is_equal, ALU.mult),
                      reads=[r_io, r_P, r_M], writes=[rS[s]])
            for j in range(2):
                st.op("dve", lambda e, Sc_=Sc_, j=j, ex=ex: e.tensor_scalar(Sc_[:, j, :], io[:, :32], POS[:, 16 + j, ex:ex + 1], MSK[:, 16 + j, ex:ex + 1], ALU.is_equal, ALU.mult),
                      reads=[r_io, r_P, r_M], writes=[rS[s]])
            for fb in range(16):
                p = pss[pk % 4]
                rp = rps[pk % 4]
                pk += 1

                def mm(e, p=p, S_=S_, Sc_=Sc_, fb=fb):
                    for tt in range(16):
                        e.matmul(p[:, :256], h2[:, tt, fb * 128:(fb + 1) * 128], S_[:, tt, :], start=(tt == 0), stop=(tt == 15))
                    for j in range(2):
                        ins = e.matmul(p[:, 256:NS], h2[:, 16 + j, fb * 128:(fb + 1) * 128], Sc_[:, j, :], start=(j == 0), stop=(j == 1))
                    return ins

                st.op("pe", mm, reads=[r_h2, rS[s]], writes=[rp])
                if fb % 2 == 0:
                    st.op("act", lambda e, p=p, xs_=xs_, fb=fb: e.copy(xs_[:, fb, :], p[:, :NS]), reads=[rp], writes=[rxs[s]])
                else:
                    st.op("dve", lambda e, p=p, xs_=xs_, fb=fb: e.tensor_copy(xs_[:, fb, :], p[:, :NS]), reads=[rp], writes=[rxs[s]])
            st.op("sp", _dma(XS[ex].rearrange("(kc p) s -> p kc s", p=128), xs_[:]), reads=[rxs[s]], dma=f"o{s}")
        st.run()

    def stage_experts(l):
        st = Stage(nc, f"ex{l}")
        st.lag = 1
        NW = 4
        ws = [st.sb([128, 16, 512], BF16, "w") for _ in range(NW)]
        rw = [Res() for _ in range(NW)]
        xs = [st.sb([128, 16, NS], BF16, "xs") for _ in range(2)]
        rxs = [Res() for _ in range(2)]
        aT = [st.sb([128, 16, NS], BF16, "aT") for _ in range(2)]
        raT = [Res() for _ in range(2)]
        yb = [st.sb([128, 3, D], BF16, "yb") for _ in range(2)]
        ryb = [Res() for _ in range(2)]
        sg = [st.sb([128, NS], F32, "sg") for _ in range(2)]
        rsg = [Res() for _ in range(2)]
        pss = [st.ps([128, 512]) for _ in range(8)]
        rps = [Res() for _ in range(8)]
        pieces = []
        for ex in range(NE):
            for fg in range(4):
                pieces.append((w_eg, ex, fg))
                pieces.append((w_eu, ex, fg))
            for dg in range(4):
                pieces.append((w_ed, ex, dg))
        NP = len(pieces)

        def ld(pi):
            wt, ex, cg = pieces[pi]
            st.op("pool", _dma(ws[pi % NW][:], wt[l, ex, :, cg * 512:(cg + 1) * 512].rearrange("(kc p) c -> p kc c", p=128)),
                  writes=[rw[pi % NW]], dma=f"w{pi % NW}")

        for pi in range(NW):
            ld(pi)
        pi = 0
        pk = 0
        sk = 0
        for ex in range(NE):
            s = ex % 2
            xs_, aT_, yb_ = xs[s], aT[s], yb[s]
            st.op("sp", _dma(xs_[:], XS[ex].rearrange("(kc p) s -> p kc s", p=128)), writes=[rxs[s]], dma=f"x{s}")
            for fg in range(4):
                pi += 2
                wg_, wu_ = ws[(pi - 2) % NW], ws[(pi - 1) % NW]
                rwg, rwu = rw[(pi - 2) % NW], rw[(pi - 1) % NW]
                for fb in range(4):
                    pg, rpg = pss[pk % 8], rps[pk % 8]
                    pu, rpu = pss[(pk + 1) % 8], rps[(pk + 1) % 8]
                    pk += 2
                    sg_, rsg_ = sg[sk % 2], rsg[sk % 2]
                    sk += 1

                    def mm(e, p=pg, w=wg_, fb=fb, xs_=xs_):
                        for kc in range(16):
                            ins = e.matmul(p[:, :NS], w[:, kc, fb * 128:(fb + 1) * 128], xs_[:, kc, :], start=(kc == 0), stop=(kc == 15))
                        return ins

                    def mm2(e, p=pu, w=wu_, fb=fb, xs_=xs_):
                        for kc in range(16):
                            ins = e.matmul(p[:, :NS], w[:, kc, fb * 128:(fb + 1) * 128], xs_[:, kc, :], start=(kc == 0), stop=(kc == 15))
                        return ins

                    st.op("pe", mm, reads=[rwg, rxs[s]], writes=[rpg])
                    st.op("pe", mm2, reads=[rwu, rxs[s]], writes=[rpu])
                    st.op("act", lambda e, pg=pg, sg_=sg_: e.activation(sg_[:], pg[:, :NS], AF.Silu), reads=[rpg], writes=[rsg_])
                    st.op("dve", lambda e, pu=pu, sg_=sg_, aT_=aT_, f=fg * 4 + fb: e.tensor_tensor(aT_[:, f, :], sg_[:], pu[:, :NS], ALU.mult),
                          reads=[rpu, rsg_], writes=[raT[s]])
                for nxt in (pi - 2 + NW, pi - 1 + NW):
                    if nxt < NP:
                        ld(nxt)
            for dg in range(4):
                pi += 1
                wd_, rwd = ws[(pi - 1) % NW], rw[(pi - 1) % NW]
                for sti, (s0, m) in enumerate(((0, 128), (128, 128), (256, 32))):
                    p, rp = pss[pk % 8], rps[pk % 8]
                    pk += 1

                    def mm3(e, p=p, w=wd_, s0=s0, m=m, aT_=aT_):
                        for fc in range(16):
                            ins = e.matmul(p[:m, :], aT_[:, fc, s0:s0 + m], w[:, fc, :], start=(fc == 0), stop=(fc == 15))
                        return ins

                    st.op("pe", mm3, reads=[rwd, raT[s]], writes=[rp])
                    if sti % 2 == 0:
                        st.op("act", lambda e, p=p, yb_=yb_, sti=sti, m=m, dg=dg: e.copy(yb_[:m, sti, dg * 512:(dg + 1) * 512], p[:m, :]),
                              reads=[rp], writes=[ryb[s]])
                    else:
                        st.op("dve", lambda e, p=p, yb_=yb_, sti=sti, m=m, dg=dg: e.tensor_copy(yb_[:m, sti, dg * 512:(dg + 1) * 512], p[:m, :]),
                              reads=[rp], writes=[ryb[s]])
                if pi - 1 + NW < NP:
                    ld(pi - 1 + NW)
            st.op("sp", _dma(Y[ex].rearrange("(s p) d -> p s d", p=128), yb_[:, 0:2, :]), reads=[ryb[s]], dma=f"y{s}")
            st.op("sp", _dma(YC[ex], yb_[:32, 2, :]), reads=[ryb[s]], dma=f"c{s}")
        st.run()

    def stage_combine(l, Xdst):
        st = Stage(nc, f"cb{l}")
        st.lag = 4
        g2 = st.sb([128, 2, D], F32)
        r_g2 = Res()
        for r in range(2):
            load_bc(st, g2[:, r, :], MOD[r:r + 1, 5 * D:6 * D], r_g2, f"g{r}")
        YfL = [st.sb([128, NE, 2, 512], BF16, "Yf") for _ in range(2)]
        YCfL = [st.sb([32, NE, 512], BF16, "YCf") for _ in range(2)]
        r_YfL, r_YCfL = [Res() for _ in range(2)], [Res() for _ in range(2)]

        def ldy(fg):
            b_ = fg % 2
            for q in range(4):
                st.op("sp", _dma(YfL[b_][:, q * 4:(q + 1) * 4], Y[q * 4:(q + 1) * 4, :, fg * 512:(fg + 1) * 512].rearrange("e (s p) d -> p e s d", p=128)),
                      writes=[r_YfL[b_]], dma=f"y{b_}{q}")
            st.op("sp", _dma(YCfL[b_][:], YC[:, :, fg * 512:(fg + 1) * 512].rearrange("e p d -> p e d")), writes=[r_YCfL[b_]], dma=f"yc{b_}")

        ldy(0)
        CD = 4
        gtt = [st.sb([128, NE, 2, 128], BF16, "gt") for _ in range(CD)]
        rgt = [Res() for _ in range(CD)]
        xa = [st.sb([128, 512], F32, "xa") for _ in range(CD)]
        rxa = [Res() for _ in range(CD)]
        tt = [st.sb([128, 512], F32, "t") for _ in range(CD)]
        rtt = [Res() for _ in range(CD)]
        xo = [st.sb([128, 512], F32, "xo") for _ in range(CD)]
        rxo = [Res() for _ in range(CD)]
        pss = [st.ps([128, 512]) for _ in range(CD)]
        rps = [Res() for _ in range(CD)]
        k = 0
        for fg in range(4):
            c0 = fg * 512
            Yf, YCf, r_Yf, r_YCf = YfL[fg % 2], YCfL[fg % 2], r_YfL[fg % 2], r_YCfL[fg % 2]
            if fg + 1 < 4:
                ldy(fg + 1)
            for i in range(NT):
                s = k % CD
                k += 1
                r = 0 if i < 16 else 1
                g_, xa_, t_, xo_, p = gtt[s], xa[s], tt[s], xo[s], pss[s]
                st.op("sp", _dma(g_[:], GT[i]), writes=[rgt[s]], dma=f"g{s}")
                st.op("sp", _dma(xa_[:], XA[i * 128:(i + 1) * 128, c0:c0 + 512]), writes=[rxa[s]], dma=f"x{s}")
                if i < 16:
                    def mm(e, p=p, g_=g_, Yf=Yf):
                        for j in range(32):
                            ex, sti = j // 2, j % 2
                            ins = e.matmul(p[:], g_[:, ex, sti, :], Yf[:, ex, sti, :], start=(j == 0), stop=(j == 31))
                        return ins
                    st.op("pe", mm, reads=[rgt[s], r_Yf], writes=[rps[s]])
                else:
                    def mmc(e, p=p, g_=g_, YCf=YCf):
                        for ex in range(NE):
                            ins = e.matmul(p[:], g_[:32, ex, 0, :], YCf[:, ex, :], start=(ex == 0), stop=(ex == NE - 1))
                        return ins
                    st.op("pe", mmc, reads=[rgt[s], r_YCf], writes=[rps[s]])
                st.op("dve", lambda e, p=p, t_=t_, r=r, c0=c0: e.tensor_tensor(t_[:], p[:], g2[:, r, c0:c0 + 512], ALU.mult),
                      reads=[rps[s], r_g2], writes=[rtt[s]])
                st.op("pool", lambda e, t_=t_, xa_=xa_, xo_=xo_: e.tensor_tensor(xo_[:], t_[:], xa_[:], ALU.add),
                      reads=[rtt[s], rxa[s]], writes=[rxo[s]])
                st.op("sp", _dma(Xdst[i * 128:(i + 1) * 128, c0:c0 + 512], xo_[:]), reads=[rxo[s]], dma=f"o{s}")
        st.run()

    def stage_final(Xsrc):
        st = Stage(nc, "fin")
        st.lag = 1
        gb = st.sb([128, D], F32)
        r_gb = Res()
        load_bc(st, gb[:], final_g[0:1, :], r_gb, "g")
        xt = [st.sb([128, D], F32, "x") for _ in range(2)]
        rx = [Res() for _ in range(2)]
        junk = st.sb([128, D], BF16)
        r_junk = Res()
        ss = [st.sb([128, 4], F32, "ss") for _ in range(2)]
        rss = [Res() for _ in range(2)]
        ot = [st.sb([128, D], F32, "o") for _ in range(2)]
        rot = [Res() for _ in range(2)]
        for i in range(16):
            s = i % 2
            x_, ss_, o_ = xt[s], ss[s], ot[s]
            st.op("sp", _dma(x_[:], Xsrc[i * 128:(i + 1) * 128, :]), writes=[rx[s]], dma=f"x{s}")
            st.op("act", lambda e, x_=x_, ss_=ss_: e.activation(junk[:], x_[:], AF.Square, accum_out=ss_[:, 0:1]),
                  reads=[rx[s]], writes=[r_junk, rss[s]])
            st.op("act", lambda e, ss_=ss_: e.activation(ss_[:, 1:2], ss_[:, 0:1], AF.Sqrt, bias=EPS, scale=1.0 / D),
                  reads=[rss[s]], writes=[rss[s]])
            st.op("dve", lambda e, ss_=ss_: e.reciprocal(ss_[:, 2:3], ss_[:, 1:2]), reads=[rss[s]], writes=[rss[s]])
            st.op("dve", lambda e, x_=x_, ss_=ss_, o_=o_: e.scalar_tensor_tensor(o_[:], x_[:], ss_[:, 2:3], gb[:], ALU.mult, ALU.mult),
                  reads=[rx[s], rss[s], r_gb], writes=[rot[s]])
            st.op("sp", _dma(out[i * 128:(i + 1) * 128, :], o_[:]), reads=[rot[s]], dma=f"o{s}")
        st.run()

    stop = getattr(build, "stop_after", None)

    def done(tag):
        return stop is not None and tag == stop

    Xcur = xin
    finished = False
    for l in range(nlayers):
        stage_mod(l)
        if done(f"mod{l}"):
            finished = True
            break
        with nc.sbuf_tensor(f"hT{l}", [128, 16, T], BF16) as hT:
            r_hT = Res()
            stage_norm(l, 1, Xcur, hT, r_hT)
            if done(f"n1{l}"):
                finished = True
                break
            stage_proj(l, hT, r_hT)
        if done(f"pj{l}"):
            finished = True
            break
        stage_rope(l)
        if done(f"rp{l}"):
            finished = True
            break
        stage_attn(l)
        stage_conv(l)
        stage_gmlp(l)
        if done(f"br{l}"):
            finished = True
            break
        stage_merge1(l)
        if done(f"m1{l}"):
            finished = True
            break
        stage_merge2(l, Xcur)
        if done(f"m2{l}"):
            finished = True
            break
        stage_norm(l, 2, XA, None, None)
        stage_route(l)
        if done(f"rt{l}"):
            finished = True
            break
        stage_gather(l)
        stage_experts(l)
        stage_combine(l, XB)
        if done(f"cb{l}"):
            finished = True
            break
        Xcur = XB
    if not finished:
        stage_final(XB)
    top.close()
    return nc


def _consts():
    ident = np.eye(128, dtype=np.float32)
    pm = np.zeros((128, 128), np.float32)
    for f in range(128):
        if (f % 64) < 32:
            pm[f + 32, f] = -1.0
        else:
            pm[f - 32, f] = 1.0
    pos = np.arange(SEQ)
    rc = np.stack([pos // 64, pos % 64], axis=-1).astype(np.float32)
    inv = (np.float32(10000.0) ** (-np.arange(0, 64, 2, dtype=np.float32) / np.float32(64))).astype(np.float32)
    ang = rc[:, :, None] * inv
    cos = np.zeros((128, SEQ), np.float32)
    sin = np.zeros((128, SEQ), np.float32)
    for ax in range(2):
        for half in range(2):
            f0 = ax * 64 + half * 32
            cos[f0:f0 + 32, :] = np.cos(ang[:, ax, :]).T
            sin[f0:f0 + 32, :] = np.sin(ang[:, ax, :]).T
    selT = np.zeros((16, 16, 128), np.float32)
    for e in range(16):
        selT[e, e, :] = 1.0
    iota_s = np.broadcast_to(np.arange(256, dtype=np.float32)[None, :], (128, 256)).copy()
    iop = np.stack([np.arange(128), np.arange(128) + 128], axis=1).astype(np.float32)
    return dict(ident=ident, pm=pm, cos=cos, sin=sin, selT=selT, iota_s=iota_s, iop=iop)


def _rpb_index():
    gs = [0, 1, 5, 14, 15]
    dr = np.zeros((5, 128, 640), np.int64)
    dc = np.zeros((5, 128, 640), np.int64)
    ok = np.zeros((5, 128, 640), bool)
    ql = np.arange(128)
    kl = np.arange(640)
    for ci, g in enumerate(gs):
        tb = min(max(g - 2, 0), 11)
        r = 2 * g + ql // 64
        qc = ql % 64
        rs = np.clip(r - 4, 0, 24)
        ws = np.clip(qc - 8, 0, 48)
        kr = 2 * tb + kl // 64
        kc = kl % 64
        rowok = (kr[None, :] >= rs[:, None]) & (kr[None, :] < rs[:, None] + 8)
        colok = (kc[None, :] >= ws[:, None]) & (kc[None, :] < ws[:, None] + 16)
        ok[ci] = rowok & colok
        dr[ci] = np.clip(kr[None, :] - r[:, None] + 7, 0, 14)
        dc[ci] = np.clip(kc[None, :] - qc[:, None] + 15, 0, 30)
    return dr, dc, ok


def _host_inputs(inputs):
    f = lambda a: np.ascontiguousarray(np.asarray(a, dtype=np.float32))
    x, c, ctx, c_ctx = f(inputs["x"]), f(inputs["c"]), f(inputs["ctx"]), f(inputs["c_ctx"])
    shared = {k: f(inputs[k]) for k in ["w_mod", "b_mod", "norm1_g", "w_in", "gmlp_ln_g", "w_branch", "w_out", "norm2_g",
                                        "w_router", "b_router", "w_e_gate", "w_e_up", "w_e_down"]}
    shared["final_g"] = f(inputs["final_g"]).reshape(1, D)
    shared["b_spatial"] = f(inputs["b_spatial"]).reshape(L, DB)
    shared["wsT"] = np.ascontiguousarray(f(inputs["w_spatial"]).transpose(0, 3, 1, 2))
    shared["conv_wT"] = np.ascontiguousarray(f(inputs["conv_w"]).reshape(L, 3, 8, 128).transpose(0, 3, 2, 1))
    rpb = f(inputs["na_rpb"])
    dr, dc, ok = _rpb_index()
    tab = np.zeros((L, 8, 5, 128, 896), np.float32)
    gathered = rpb[:, :, dr, dc]
    tab[..., :640] = np.where(ok[None, None], gathered, np.float32(NEG))
    shared["rpb_tab"] = np.ascontiguousarray(tab.transpose(0, 1, 3, 2, 4))
    shared.update(_consts())
    in_maps = []
    for b in range(x.shape[0]):
        m = dict(shared)
        m["xin"] = np.ascontiguousarray(np.concatenate([x[b], ctx[b]], axis=0))
        c2 = np.stack([c[b], c_ctx], axis=0)
        m["c2T"] = np.ascontiguousarray(c2.reshape(2, 16, 128).transpose(2, 1, 0))
        in_maps.append(m)
    return in_maps


def kernel(**inputs):
    in_maps = _host_inputs(inputs)
    nc = bass.Bass("TRN2", target_bir_lowering=False)
    build(nc)
    res = run_bass_kernel_spmd(nc, in_maps, core_ids=list(range(len(in_maps))))
    outs = [np.asarray(r["out"], dtype=np.float32) for r in res.results]
    return np.stack(outs, axis=0)
```

```python
import numpy as np
from contextlib import ExitStack
from collections import defaultdict
import concourse.bass as bass
import concourse.mybir as mybir
from concourse.bass_utils import run_bass_kernel_spmd

F32 = mybir.dt.float32
BF16 = mybir.dt.bfloat16
AF = mybir.ActivationFunctionType
ALU = mybir.AluOpType
AX = mybir.AxisListType

D = 2048
L = 2
SEQ = 2048
CTX = 256
T = SEQ + CTX
NT = T // 128
DB = 1024
DPROJ = 14336
NE = 16
CAP = 256
CAPC = 32
NS = CAP + CAPC
EPS = 1e-6
NEG = -30000.0
TGS = [(0, 512), (512, 512), (1024, 512), (1536, 512), (2048, 256)]


class Res:
    __slots__ = ("name", "last_w", "readers")

    def __init__(self, name=""):
        self.name = name
        self.last_w = None
        self.readers = {}


class Op:
    __slots__ = ("eng", "fn", "deps", "needed", "token", "dma", "has_reads")


BLK = {"pe": "tensor", "act": "scalar", "dve": "vector", "pool": "gpsimd", "sp": "sync"}


class Stage:
    def __init__(self, nc, name):
        self.nc = nc
        self.name = name
        self.ops = []
        self.es = ExitStack()
        self._n = 0
        self.touched = []
        self.lag = 0

    def sb(self, shape, dtype, nm="t"):
        self._n += 1
        return self.es.enter_context(self.nc.sbuf_tensor(f"{self.name}_{nm}{self._n}", list(shape), dtype))

    def ps(self, shape, dtype=F32, nm="p"):
        self._n += 1
        return self.es.enter_context(self.nc.psum_tensor(f"{self.name}_{nm}{self._n}", list(shape), dtype))

    def op(self, eng, fn, reads=(), writes=(), dma=None):
        o = Op()
        o.eng = eng
        o.fn = fn
        o.dma = dma
        o.needed = False
        o.token = None
        o.has_reads = len(reads) > 0
        deps = {}
        self.touched.extend(reads)
        self.touched.extend(writes)
        for r in reads:
            if r.last_w is not None:
                deps[id(r.last_w)] = r.last_w
        for w in writes:
            if w.last_w is not None:
                deps[id(w.last_w)] = w.last_w
            for rd in w.readers.values():
                deps[id(rd)] = rd
        key = eng if dma is None else ("dma", dma)
        for r in reads:
            r.readers[key] = o
        for w in writes:
            w.last_w = o
            w.readers = {}
        o.deps = [d for d in deps.values()
                  if d is not o and not (d.eng == "pe" and eng == "pe" and d.dma is None and dma is None)]
        for d in o.deps:
            d.needed = True
        self.ops.append(o)
        return o

    def run(self):
        nc = self.nc
        cnt = defaultdict(int)
        for o in self.ops:
            if o.dma is not None:
                k = ("dma", o.dma)
                cnt[k] += 16
                o.token = (k, cnt[k])
            elif o.needed:
                k = ("eng", o.eng)
                cnt[k] += 1
                o.token = (k, cnt[k])
        sems = {}
        handles = []
        for i, k in enumerate(cnt):
            sems[k] = nc.alloc_semaphore(f"{self.name}_s{i}")
            handles.append(sems[k])
        with nc.Block() as blk:
            for eng, bname in BLK.items():
                ops_e = [o for o in self.ops if o.eng == eng]
                if not ops_e:
                    continue
                if eng == "sp" and self.lag > 0:
                    outl, pend = [], []
                    for o in ops_e:
                        if o.dma is not None and o.has_reads:
                            pend.append([o, 0])
                        else:
                            outl.append(o)
                            for pp_ in pend:
                                pp_[1] += 1
                            while pend and pend[0][1] >= self.lag:
                                outl.append(pend.pop(0)[0])
                    outl.extend(pp_[0] for pp_ in pend)
                    ops_e = outl

                def body(e, ops_e=ops_e):
                    waited = {}
                    fin = {}
                    for o in ops_e:
                        need = {}
                        for d in o.deps:
                            k, v = d.token
                            if need.get(k, 0) < v:
                                need[k] = v
                        for k, v in need.items():
                            if waited.get(k, 0) < v:
                                e.wait_ge(sems[k], v)
                                waited[k] = v
                        ins = o.fn(e)
                        if o.token is not None:
                            ins.then_inc(sems[o.token[0]], 16 if o.dma is not None else 1)
                            if o.dma is not None:
                                fin[o.token[0]] = o.token[1]
                    for k, v in fin.items():
                        if waited.get(k, 0) < v:
                            e.wait_ge(sems[k], v)

                getattr(blk, bname)(body)
        for r in self.touched:
            r.last_w = None
            r.readers = {}
        self.es.close()
        nc.all_engine_barrier()
        nc.clear_and_free_semaphores(handles)
        nc.all_engine_barrier()


def _dma(out, in_):
    return lambda e: e.dma_start(out=out, in_=in_)


def build(nc, dbg=None, nlayers=L, Lw=L, NEw=NE):
    dbg = dbg or set()

    def din(name, shape, dt=F32):
        return nc.dram_tensor(name, list(shape), dt, kind="ExternalInput").ap()

    def dscr(name, shape, dt):
        kind = "ExternalOutput" if name in dbg else "Internal"
        return nc.dram_tensor(name, list(shape), dt, kind=kind).ap()

    xin = din("xin", [T, D])
    c2T = din("c2T", [128, 16, 2])
    w_mod = din("w_mod", [Lw, D, 6 * D])
    b_mod = din("b_mod", [Lw, 6 * D])
    norm1_g = din("norm1_g", [Lw, D])
    w_in = din("w_in", [Lw, D, DPROJ])
    rpb_tab = din("rpb_tab", [Lw, 8, 128, 5, 896])
    conv_wT = din("conv_wT", [Lw, 128, 8, 3])
    ln_g = din("gmlp_ln_g", [Lw, DB])
    wsT = din("wsT", [Lw, 128, 8, 128])
    b_sp = din("b_spatial", [Lw, DB])
    w_branch = din("w_branch", [Lw, 3, DB, D])
    w_out = din("w_out", [Lw, D, D])
    norm2_g = din("norm2_g", [Lw, D])
    w_router = din("w_router", [Lw, D, NE])
    b_router = din("b_router", [Lw, NE])
    w_eg = din("w_e_gate", [Lw, NEw, D, D])
    w_eu = din("w_e_up", [Lw, NEw, D, D])
    w_ed = din("w_e_down", [Lw, NEw, D, D])
    final_g = din("final_g", [1, D])
    ident_d = din("ident", [128, 128])
    pm_d = din("pm", [128, 128])
    cos_d = din("cos", [128, SEQ])
    sin_d = din("sin", [128, SEQ])
    sel_d = din("selT", [16, 16, 128])
    iota_d = din("iota_s", [128, 256])
    iop_d = din("iop", [128, 2])
    out = nc.dram_tensor("out", [SEQ, D], F32, kind="ExternalOutput").ap()

    MOD = dscr("MOD", [2, 6 * D], F32)
    PROJ = dscr("PROJ", [DPROJ, T], BF16)
    V_tm = dscr("V_tm", [T, DB], BF16)
    VS_tm = dscr("VS_tm", [T, DB], BF16)
    BR = dscr("BR", [3, DB, T], BF16)
    MERGED = dscr("MERGED", [D, T], BF16)
    XA = dscr("XA", [T, D], F32)
    XB = dscr("XB", [T, D], F32)
    H2 = dscr("H2", [T, D], BF16)
    GT = dscr("GT", [NT, 128, NE, 2, 128], BF16)
    XS = dscr("XS", [NE, D, NS], BF16)
    Y = dscr("Y", [NE, CAP, D], BF16)
    YC = dscr("YC", [NE, CAPC, D], BF16)
    DBG_AFF = dscr("DBG_AFF", [128, NT, NE], F32)
    DBG_HT = dscr("DBG_HT", [128, 16, T], BF16)

    top = ExitStack()

    def tsb(name, shape, dt):
        return top.enter_context(nc.sbuf_tensor(name, list(shape), dt))

    ident_f = tsb("ident_f", [128, 128], F32)
    ident_b = tsb("ident_b", [128, 128], BF16)
    ones_b = tsb("ones_b", [128, 128], BF16)
    AFF = tsb("AFF", [128, NT, NE], F32)
    POS = tsb("POS", [128, NT, NE], F32)
    MSK = tsb("MSK", [128, NT, NE], F32)

    st = Stage(nc, "c0")
    r_if, r_ib, r_ob = Res(), Res(), Res()
    st.op("sp", _dma(ident_f[:], ident_d), writes=[r_if], dma="a")
    st.op("dve", lambda e: e.tensor_copy(ident_b[:], ident_f[:]), reads=[r_if], writes=[r_ib])
    st.op("dve", lambda e: e.memset(ones_b[:], 1.0), writes=[r_ob])
    st.run()

    def stage_mod(l):
        st = Stage(nc, f"mod{l}")
        cT = st.sb([128, 16, 2], F32)
        sc = st.sb([128, 16, 2], BF16)
        b2 = st.sb([2, 6 * D], F32)
        msb = st.sb([2, 6 * D], F32)
        ws = [st.sb([128, 16, 512], BF16, "w") for _ in range(3)]
        rw = [Res() for _ in range(3)]
        pss = [st.ps([2, 512]) for _ in range(2)]
        rps = [Res() for _ in range(2)]
        r_c, r_sc, r_b2, r_m = Res(), Res(), Res(), Res()
        st.op("sp", _dma(cT[:], c2T), writes=[r_c], dma="c")
        st.op("sp", _dma(b2[:], b_mod[l:l + 1, :].partition_broadcast(2)), writes=[r_b2], dma="b")
        st.op("act", lambda e: e.activation(sc[:], cT[:], AF.Silu), reads=[r_c], writes=[r_sc])
        NG = 24

        def ld(g):
            st.op("pool", _dma(ws[g % 3][:], w_mod[l, :, g * 512:(g + 1) * 512].rearrange("(kc p) c -> p kc c", p=128)),
                  writes=[rw[g % 3]], dma=f"w{g % 3}")

        ld(0)
        ld(1)
        for g in range(NG):
            if g + 2 < NG:
                ld(g + 2)
            w = ws[g % 3]
            p = pss[g % 2]

            def mm(e, w=w, p=p):
                for kc in range(16):
                    ins = e.matmul(p[:], sc[:, kc, :], w[:, kc, :], start=(kc == 0), stop=(kc == 15))
                return ins

            st.op("pe", mm, reads=[rw[g % 3], r_sc], writes=[rps[g % 2]])
            st.op("dve", lambda e, p=p, g=g: e.tensor_tensor(msb[:, g * 512:(g + 1) * 512], p[:], b2[:, g * 512:(g + 1) * 512], ALU.add),
                  reads=[rps[g % 2], r_b2], writes=[r_m])
        st.op("sp", _dma(MOD, msb[:]), reads=[r_m], dma="o")
        st.run()

    def load_bc(st, dst_ap, src_row_ap, res, key):
        st.op("sp", _dma(dst_ap, src_row_ap.partition_broadcast(128)), writes=[res], dma=key)

    def stage_norm(l, which, Xsrc, hT, r_hT):
        st = Stage(nc, f"n{which}_{l}")
        st.lag = 1
        gsrc = norm1_g if which == 1 else norm2_g
        jo = 0 if which == 1 else 3
        gm = st.sb([128, 2, D], F32)
        sh = st.sb([128, 2, D], F32)
        gb = st.sb([128, D], F32)
        r_gm, r_sh, r_gb = Res(), Res(), Res()
        load_bc(st, gb[:], gsrc[l:l + 1, :], r_gb, "g")
        for r in range(2):
            load_bc(st, gm[:, r, :], MOD[r:r + 1, (jo + 1) * D:(jo + 2) * D], r_gm, f"gm{r}")
            load_bc(st, sh[:, r, :], MOD[r:r + 1, jo * D:(jo + 1) * D], r_sh, f"sh{r}")
            st.op("dve", lambda e, r=r: e.scalar_tensor_tensor(gm[:, r, :], gm[:, r, :], 1.0, gb[:], ALU.add, ALU.mult),
                  reads=[r_gm, r_gb], writes=[r_gm])
        xt = [st.sb([128, D], F32, "x") for _ in range(2)]
        rx = [Res() for _ in range(2)]
        junk = st.sb([128, D], BF16)
        r_junk = Res()
        ss = [st.sb([128, 4], F32, "ss") for _ in range(2)]
        rss = [Res() for _ in range(2)]
        tt = [st.sb([128, D], F32, "tt") for _ in range(2)]
        rtt = [Res() for _ in range(2)]
        if which == 1:
            hb = [st.sb([128, D], BF16, "hb") for _ in range(2)]
            rhb = [Res() for _ in range(2)]
            pst = [st.ps([128, 8, 128], BF16) for _ in range(4)]
            rpst = [Res() for _ in range(4)]
        else:
            hb = [st.sb([128, D], BF16, "hb") for _ in range(2)]
            rhb = [Res() for _ in range(2)]
            pst = [st.ps([128, 4, 128], F32) for _ in range(4)]
            rpst = [Res() for _ in range(4)]
            h2T = [st.sb([128, 16, 128], F32, "h2T") for _ in range(2)]
            rh2T = [Res() for _ in range(2)]
            wr = st.sb([128, 16, NE], F32)
            r_wr = Res()
            st.op("sp", _dma(wr[:], w_router[l].rearrange("(kc p) e -> p kc e", p=128)), writes=[r_wr], dma="wr")
            brb = st.sb([128, NE], F32)
            r_brb = Res()
            load_bc(st, brb[:], b_router[l:l + 1, :], r_brb, "brb")
            pl = [st.ps([128, NE], F32) for _ in range(2)]
            rpl = [Res() for _ in range(2)]
            lg = [st.sb([128, NE], F32, "lg") for _ in range(2)]
            rlg = [Res() for _ in range(2)]
            sm = [st.sb([128, 4], F32, "sm") for _ in range(2)]
            rsm = [Res() for _ in range(2)]
            r_aff = Res()
        for i in range(NT):
            s = i % 2
            r = 0 if i < 16 else 1
            x_, ss_, t_, h_ = xt[s], ss[s], tt[s], hb[s]
            st.op("sp", _dma(x_[:], Xsrc[i * 128:(i + 1) * 128, :]), writes=[rx[s]], dma=f"x{s}")
            st.op("act", lambda e, x_=x_, ss_=ss_: e.activation(junk[:], x_[:], AF.Square, accum_out=ss_[:, 0:1]),
                  reads=[rx[s]], writes=[r_junk, rss[s]])
            st.op("act", lambda e, ss_=ss_: e.activation(ss_[:, 1:2], ss_[:, 0:1], AF.Sqrt, bias=EPS, scale=1.0 / D),
                  reads=[rss[s]], writes=[rss[s]])
            st.op("dve", lambda e, ss_=ss_: e.reciprocal(ss_[:, 2:3], ss_[:, 1:2]), reads=[rss[s]], writes=[rss[s]])
            st.op("dve", lambda e, x_=x_, ss_=ss_, t_=t_, r=r: e.scalar_tensor_tensor(t_[:], x_[:], ss_[:, 2:3], gm[:, r, :], ALU.mult, ALU.mult),
                  reads=[rx[s], rss[s], r_gm], writes=[rtt[s]])
            if which == 1:
                st.op("pool", lambda e, t_=t_, h_=h_, r=r: e.tensor_tensor(h_[:], t_[:], sh[:, r, :], ALU.add),
                      reads=[rtt[s], r_sh], writes=[rhb[s]])
                for half in range(2):
                    pp = pst[2 * s + half]
                    rp = rpst[2 * s + half]

                    def tr(e, pp=pp, h_=h_, half=half):
                        for j in range(8):
                            kc = half * 8 + j
                            ins = e.transpose(pp[:, j, :], h_[:, kc * 128:(kc + 1) * 128], ident_b[:])
                        return ins

                    st.op("pe", tr, reads=[rhb[s]], writes=[rp])
                    eng = "act" if half == 0 else "dve"
                    if eng == "act":
                        st.op("act", lambda e, pp=pp, half=half, i=i: e.copy(hT[:, half * 8:(half + 1) * 8, i * 128:(i + 1) * 128], pp[:]),
                              reads=[rp], writes=[r_hT])
                    else:
                        st.op("dve", lambda e, pp=pp, half=half, i=i: e.tensor_copy(hT[:, half * 8:(half + 1) * 8, i * 128:(i + 1) * 128], pp[:]),
                              reads=[rp], writes=[r_hT])
            else:
                st.op("pool", lambda e, t_=t_, r=r: e.tensor_tensor(t_[:], t_[:], sh[:, r, :], ALU.add),
                      reads=[rtt[s], r_sh], writes=[rtt[s]])
                st.op("pool", lambda e, t_=t_, h_=h_: e.tensor_copy(h_[:], t_[:]), reads=[rtt[s]], writes=[rhb[s]])
                st.op("sp", _dma(H2[i * 128:(i + 1) * 128, :], h_[:]), reads=[rhb[s]], dma=f"h{s}")
                hT_ = h2T[s]
                for q in range(4):
                    pp = pst[q]

                    def tr(e, pp=pp, t_=t_, q=q):
                        for j in range(4):
                            kc = q * 4 + j
                            ins = e.transpose(pp[:, j, :], t_[:, kc * 128:(kc + 1) * 128], ident_f[:])
                        return ins

                    st.op("pe", tr, reads=[rtt[s]], writes=[rpst[q]])
                    if q % 2 == 0:
                        st.op("act", lambda e, pp=pp, q=q, hT_=hT_: e.copy(hT_[:, q * 4:(q + 1) * 4, :], pp[:]),
                              reads=[rpst[q]], writes=[rh2T[s]])
                    else:
                        st.op("dve", lambda e, pp=pp, q=q, hT_=hT_: e.tensor_copy(hT_[:, q * 4:(q + 1) * 4, :], pp[:]),
                              reads=[rpst[q]], writes=[rh2T[s]])
                pl_ = pl[s]

                def mmr(e, pl_=pl_, hT_=hT_):
                    for kc in range(16):
                        ins = e.matmul(pl_[:], hT_[:, kc, :], wr[:, kc, :], start=(kc == 0), stop=(kc == 15))
                    return ins

                st.op("pe", mmr, reads=[rh2T[s], r_wr], writes=[rpl[s]])
                lg_, sm_ = lg[s], sm[s]
                st.op("dve", lambda e, lg_=lg_, pl_=pl_: e.tensor_tensor(lg_[:], pl_[:], brb[:], ALU.add),
                      reads=[rpl[s], r_brb], writes=[rlg[s]])
                st.op("dve", lambda e, lg_=lg_, sm_=sm_: e.tensor_reduce(sm_[:, 0:1], lg_[:], AX.X, ALU.max, negate=True),
                      reads=[rlg[s]], writes=[rsm[s]])
                st.op("act", lambda e, lg_=lg_, sm_=sm_: e.activation(lg_[:], lg_[:], AF.Exp, bias=sm_[:, 0:1], accum_out=sm_[:, 1:2]),
                      reads=[rlg[s], rsm[s]], writes=[rlg[s], rsm[s]])
                st.op("dve", lambda e, sm_=sm_: e.reciprocal(sm_[:, 2:3], sm_[:, 1:2]), reads=[rsm[s]], writes=[rsm[s]])
                st.op("dve", lambda e, lg_=lg_, sm_=sm_, i=i: e.tensor_scalar(AFF[:, i, :], lg_[:], sm_[:, 2:3], None, ALU.mult),
                      reads=[rlg[s], rsm[s]], writes=[r_aff])
        if which == 2 and "DBG_AFF" in dbg:
            st.op("sp", _dma(DBG_AFF, AFF[:]), reads=[r_aff], dma="dbg")
        if which == 1 and "DBG_HT" in dbg:
            st.op("sp", _dma(DBG_HT, hT[:]), reads=[r_hT], dma="dbg")
        st.run()

    def stage_proj(l, hT, r_hT):
        st = Stage(nc, f"pj{l}")
        ws = [st.sb([128, 16, 512], BF16, "w") for _ in range(3)]
        rw = [Res() for _ in range(3)]
        NPS = 6
        pss = [st.ps([128, 512]) for _ in range(NPS)]
        rps = [Res() for _ in range(NPS)]
        NO = 4
        os_ = [st.sb([128, 512], BF16, "o") for _ in range(NO)]
        ros = [Res() for _ in range(NO)]
        NG = getattr(build, 'proj_ng', DPROJ // 512)
        cntr = [0]

        def ld(g):
            st.op("pool", _dma(ws[g % 3][:], w_in[l, :, g * 512:(g + 1) * 512].rearrange("(kc p) c -> p kc c", p=128)),
                  writes=[rw[g % 3]], dma=f"w{g % 3}")

        def evac_store(p, rp, n_part, n_free, dst):
            k = cntr[0]
            cntr[0] += 1
            o = os_[k % NO]
            ro = ros[k % NO]
            if k % 2 == 0:
                st.op("act", lambda e: e.copy(o[:n_part, :n_free], p[:n_part, :n_free]), reads=[rp], writes=[ro])
            else:
                st.op("dve", lambda e: e.tensor_copy(o[:n_part, :n_free], p[:n_part, :n_free]), reads=[rp], writes=[ro])
            st.op("sp", _dma(dst, o[:n_part, :n_free]), reads=[ro], dma=f"o{k % NO}")

        ld(0)
        ld(1)
        pk = 0
        for g in range(NG):
            if g + 2 < NG:
                ld(g + 2)
            w = ws[g % 3]
            tm = g in (4, 5, 14, 15)
            if not tm:
                for (t0, n) in TGS:
                    for cb in range(4):
                        p = pss[pk % NPS]
                        rp = rps[pk % NPS]
                        pk += 1

                        def mm(e, w=w, p=p, cb=cb, t0=t0, n=n):
                            for kc in range(16):
                                ins = e.matmul(p[:, :n], w[:, kc, cb * 128:(cb + 1) * 128], hT[:, kc, t0:t0 + n],
                                               start=(kc == 0), stop=(kc == 15))
                            return ins

                        st.op("pe", mm, reads=[rw[g % 3], r_hT], writes=[rp])
                        row0 = g * 512 + cb * 128
                        evac_store(p, rp, 128, n, PROJ[row0:row0 + 128, t0:t0 + n])
            else:
                dst_t = V_tm if g in (4, 5) else VS_tm
                c0 = (g - 4) * 512 if g in (4, 5) else (g - 14) * 512
                for i in range(NT):
                    p = pss[pk % NPS]
                    rp = rps[pk % NPS]
                    pk += 1

                    def mm(e, w=w, p=p, i=i):
                        for kc in range(16):
                            ins = e.matmul(p[:], hT[:, kc, i * 128:(i + 1) * 128], w[:, kc, :],
                                           start=(kc == 0), stop=(kc == 15))
                        return ins

                    st.op("pe", mm, reads=[rw[g % 3], r_hT], writes=[rp])
                    evac_store(p, rp, 128, 512, dst_t[i * 128:(i + 1) * 128, c0:c0 + 512])
        st.run()

    def stage_rope(l):
        st = Stage(nc, f"rp{l}")
        st.lag = 2
        pmf = st.sb([128, 128], F32)
        pmb = st.sb([128, 128], BF16)
        cs = st.sb([128, SEQ], F32)
        sn = st.sb([128, SEQ], F32)
        r_pmf, r_pmb, r_cs, r_sn = Res(), Res(), Res(), Res()
        st.op("sp", _dma(pmf[:], pm_d), writes=[r_pmf], dma="pm")
        st.op("sp", _dma(cs[:], cos_d), writes=[r_cs], dma="cs")
        st.op("sp", _dma(sn[:], sin_d), writes=[r_sn], dma="sn")
        st.op("dve", lambda e: e.tensor_copy(pmb[:], pmf[:]), reads=[r_pmf], writes=[r_pmb])
        RD = 4
        qb = [st.sb([128, 512], BF16, "q") for _ in range(RD)]
        rq = [Res() for _ in range(RD)]
        pq = [st.ps([128, 512]) for _ in range(RD)]
        rpq = [Res() for _ in range(RD)]
        t1 = [st.sb([128, 512], F32, "t1") for _ in range(RD)]
        rt1 = [Res() for _ in range(RD)]
        t2 = [st.sb([128, 512], F32, "t2") for _ in range(RD)]
        rt2 = [Res() for _ in range(RD)]
        ob = [st.sb([128, 512], BF16, "ob") for _ in range(RD)]
        rob = [Res() for _ in range(RD)]
        k = 0
        for blk in range(16):
            for tg in range(4):
                s = k % RD
                k += 1
                t0 = tg * 512
                src = PROJ[blk * 128:(blk + 1) * 128, t0:t0 + 512]
                q_, p_, a_, b_, o_ = qb[s], pq[s], t1[s], t2[s], ob[s]
                st.op("sp", _dma(q_[:], src), writes=[rq[s]], dma=f"q{s}")
                st.op("pe", lambda e, q_=q_, p_=p_: e.matmul(p_[:], pmb[:], q_[:], start=True, stop=True),
                      reads=[rq[s], r_pmb], writes=[rpq[s]])
                st.op("dve", lambda e, q_=q_, a_=a_, t0=t0: e.tensor_tensor(a_[:], q_[:], cs[:, t0:t0 + 512], ALU.mult),
                      reads=[rq[s], r_cs], writes=[rt1[s]])
                st.op("dve", lambda e, p_=p_, b_=b_, t0=t0: e.tensor_tensor(b_[:], p_[:], sn[:, t0:t0 + 512], ALU.mult),
                      reads=[rpq[s], r_sn], writes=[rt2[s]])
                st.op("pool", lambda e, a_=a_, b_=b_, o_=o_: e.tensor_tensor(o_[:], a_[:], b_[:], ALU.add),
                      reads=[rt1[s], rt2[s]], writes=[rob[s]])
                st.op("sp", _dma(src, o_[:]), reads=[rob[s]], dma=f"o{s}")
        st.run()

    def stage_attn(l):
        st = Stage(nc, f"at{l}")
        st.lag = 0
        scale = 128 ** -0.5
        qT = [st.sb([128, T], BF16, "q") for _ in range(2)]
        kT = [st.sb([128, T], BF16, "k") for _ in range(2)]
        Vh = [st.sb([128, NT, 128], BF16, "v") for _ in range(2)]
        bt = [st.sb([128, 5, 896], F32, "b") for _ in range(2)]
        oT = [st.sb([128, T], BF16, "o") for _ in range(2)]
        rq = [Res() for _ in range(2)]
        rk = [Res() for _ in range(2)]
        rv = [Res() for _ in range(2)]
        rb = [Res() for _ in range(2)]
        ro = [Res() for _ in range(2)]
        psS = [st.ps([128, 1024]) for _ in range(2)]
        rpS = [Res() for _ in range(2)]
        psT = [st.ps([128, 7, 128], BF16) for _ in range(2)]
        rpT = [Res() for _ in range(2)]
        psO = [st.ps([128, 2, 128]) for _ in range(2)]
        rpO = [Res() for _ in range(2)]
        AD = 4
        sbS = [st.sb([128, 896], F32, "s") for _ in range(AD)]
        rsS = [Res() for _ in range(AD)]
        mx = [st.sb([128, 2], F32, "mx") for _ in range(AD)]
        rmx = [Res() for _ in range(AD)]
        pb = [st.sb([128, 896], BF16, "p") for _ in range(AD)]
        rpb_ = [Res() for _ in range(AD)]
        pT = [st.sb([128, 7, 128], BF16, "pT") for _ in range(AD)]
        rpTs = [Res() for _ in range(AD)]
        ri = [st.sb([128, 128], F32, "ri") for _ in range(AD)]
        rri = [Res() for _ in range(AD)]
        units = [(h, qt) for h in range(8) for qt in range(NT)]
        NU = len(units)

        def geom(qt):
            if qt < 16:
                tb = min(max(qt - 2, 0), 11)
                cls = {0: 0, 1: 1, 14: 3, 15: 4}.get(qt, 2)
                segs = [(0, tb * 128, 512), (512, tb * 128 + 512, 128), (640, SEQ, 256)]
                vts = [tb + j for j in range(5)] + [16, 17]
                return segs, vts, 896, cls
            return [(0, SEQ, 256)], [16, 17], 256, None

        def stA(u):
            h, qt = units[u]
            hs = h % 2
            q_, k_, v_, b_ = qT[hs], kT[hs], Vh[hs], bt[hs]
            if qt == 0:
                st.op("sp", _dma(q_[:], PROJ[h * 128:(h + 1) * 128, :]), writes=[rq[hs]], dma=f"q{hs}")
                st.op("sp", _dma(k_[:], PROJ[DB + h * 128:DB + (h + 1) * 128, :]), writes=[rk[hs]], dma=f"k{hs}")
                st.op("sp", _dma(v_[:], V_tm[:, h * 128:(h + 1) * 128].rearrange("(i p) d -> p i d", p=128)),
                      writes=[rv[hs]], dma=f"v{hs}")
                st.op("sp", _dma(b_[:], rpb_tab[l, h]), writes=[rb[hs]], dma=f"b{hs}")
            s, sB = u % 2, u % AD
            S_, sb_, mx_, p_ = psS[s], sbS[sB], mx[sB], pb[sB]
            segs, vts, nk, cls = geom(qt)

            def mmS(e, S_=S_, q_=q_, k_=k_, qt=qt, segs=segs):
                for (c0, k0, n) in segs:
                    ins = e.matmul(S_[:, c0:c0 + n], q_[:, qt * 128:(qt + 1) * 128], k_[:, k0:k0 + n], start=True, stop=True)
                return ins

            st.op("pe", mmS, reads=[rq[hs], rk[hs]], writes=[rpS[s]])
            if cls is not None:
                st.op("dve", lambda e, S_=S_, sb_=sb_, b_=b_, cls=cls: e.scalar_tensor_tensor(sb_[:, :896], S_[:, :896], scale, b_[:, cls, :], ALU.mult, ALU.add),
                      reads=[rpS[s], rb[hs]], writes=[rsS[sB]])
            else:
                st.op("dve", lambda e, S_=S_, sb_=sb_: e.tensor_scalar(sb_[:, :256], S_[:, :256], scale, None, ALU.mult),
                      reads=[rpS[s]], writes=[rsS[sB]])
            st.op("dve", lambda e, sb_=sb_, mx_=mx_, nk=nk: e.tensor_reduce(mx_[:, 0:1], sb_[:, :nk], AX.X, ALU.max, negate=True),
                  reads=[rsS[sB]], writes=[rmx[sB]])
            st.op("act", lambda e, sb_=sb_, mx_=mx_, p_=p_, nk=nk: e.activation(p_[:, :nk], sb_[:, :nk], AF.Exp, bias=mx_[:, 0:1]),
                  reads=[rsS[sB], rmx[sB]], writes=[rpb_[sB]])

        def stB(u):
            h, qt = units[u]
            s, sB = u % 2, u % AD
            Ts_, p_, pT_ = psT[s], pb[sB], pT[sB]
            segs, vts, nk, cls = geom(qt)
            nj = nk // 128

            def trp(e, Ts_=Ts_, p_=p_, nj=nj):
                for j in range(nj):
                    ins = e.transpose(Ts_[:, j, :], p_[:, j * 128:(j + 1) * 128], ident_b[:])
                return ins

            st.op("pe", trp, reads=[rpb_[sB]], writes=[rpT[s]])
            st.op("act", lambda e, Ts_=Ts_, pT_=pT_, nj=nj: e.copy(pT_[:, :nj, :], Ts_[:, :nj, :]),
                  reads=[rpT[s]], writes=[rpTs[sB]])

        def stC(u):
            h, qt = units[u]
            hs = h % 2
            v_, o_ = Vh[hs], oT[hs]
            s, sB = u % 2, u % AD
            O_, pT_, ri_ = psO[s], pT[sB], ri[sB]
            segs, vts, nk, cls = geom(qt)

            def mmO(e, O_=O_, v_=v_, pT_=pT_, vts=vts):
                n = len(vts)
                for j, vt in enumerate(vts):
                    e.matmul(O_[:, 0, :], v_[:, vt, :], pT_[:, j, :], start=(j == 0), stop=(j == n - 1))
                for j in range(n):
                    ins = e.matmul(O_[:, 1, :], ones_b[:], pT_[:, j, :], start=(j == 0), stop=(j == n - 1))
                return ins

            st.op("pe", mmO, reads=[rv[hs], rpTs[sB]], writes=[rpO[s]])
            st.op("dve", lambda e, O_=O_, ri_=ri_: e.reciprocal(ri_[:], O_[:, 1, :]), reads=[rpO[s]], writes=[rri[sB]])
            st.op("dve", lambda e, O_=O_, ri_=ri_, o_=o_, qt=qt: e.tensor_tensor(o_[:, qt * 128:(qt + 1) * 128], O_[:, 0, :], ri_[:], ALU.mult),
                  reads=[rpO[s], rri[sB]], writes=[ro[hs]])
            if qt == NT - 1:
                st.op("sp", _dma(BR[0, h * 128:(h + 1) * 128, :], o_[:]), reads=[ro[hs]], dma=f"o{hs}")

        for t in range(NU + 2):
            if t < NU:
                stA(t)
            if 0 <= t - 1 < NU:
                stB(t - 1)
            if 0 <= t - 2 < NU:
                stC(t - 2)
        st.run()

    def stage_conv(l):
        st = Stage(nc, f"cv{l}")
        st.lag = 3
        cw = st.sb([128, 8, 3], F32)
        r_cw = Res()
        st.op("sp", _dma(cw[:], conv_wT[l]), writes=[r_cw], dma="cw")
        xc = [st.sb([128, T], BF16, "xc") for _ in range(2)]
        bg = [st.sb([128, T], BF16, "bg") for _ in range(2)]
        cg = [st.sb([128, T], BF16, "cg") for _ in range(2)]
        z = [st.sb([128, T], F32, "z") for _ in range(2)]
        y = [st.sb([128, T], F32, "y") for _ in range(2)]
        o = [st.sb([128, T], BF16, "o") for _ in range(2)]
        rxc, rbg, rcg, rz, ry, ro = ([Res() for _ in range(2)] for _ in range(6))
        for cb in range(8):
            s = cb % 2
            xc_, bg_, cg_, z_, y_, o_ = xc[s], bg[s], cg[s], z[s], y[s], o[s]
            st.op("sp", _dma(xc_[:], PROJ[3072 + cb * 128:3072 + (cb + 1) * 128, :]), writes=[rxc[s]], dma=f"a{s}")
            st.op("sp", _dma(bg_[:], PROJ[4096 + cb * 128:4096 + (cb + 1) * 128, :]), writes=[rbg[s]], dma=f"b{s}")
            st.op("sp", _dma(cg_[:], PROJ[5120 + cb * 128:5120 + (cb + 1) * 128, :]), writes=[rcg[s]], dma=f"c{s}")
            st.op("pool", lambda e, z_=z_, cg_=cg_, xc_=xc_: e.tensor_tensor(z_[:], cg_[:], xc_[:], ALU.mult),
                  reads=[rxc[s], rcg[s]], writes=[rz[s]])
            st.op("dve", lambda e, y_=y_, z_=z_, cb=cb: e.tensor_scalar(y_[:], z_[:], cw[:, cb, 1:2], None, ALU.mult),
                  reads=[rz[s], r_cw], writes=[ry[s]])
            for (a, b) in ((0, SEQ), (SEQ, T)):
                st.op("dve", lambda e, y_=y_, z_=z_, cb=cb, a=a, b=b: e.scalar_tensor_tensor(y_[:, a + 1:b], z_[:, a:b - 1], cw[:, cb, 0:1], y_[:, a + 1:b], ALU.mult, ALU.add),
                      reads=[rz[s], ry[s], r_cw], writes=[ry[s]])
                st.op("dve", lambda e, y_=y_, z_=z_, cb=cb, a=a, b=b: e.scalar_tensor_tensor(y_[:, a:b - 1], z_[:, a + 1:b], cw[:, cb, 2:3], y_[:, a:b - 1], ALU.mult, ALU.add),
                      reads=[rz[s], ry[s], r_cw], writes=[ry[s]])
            st.op("pool", lambda e, o_=o_, bg_=bg_, y_=y_: e.tensor_tensor(o_[:], bg_[:], y_[:], ALU.mult),
                  reads=[rbg[s], ry[s]], writes=[ro[s]])
            st.op("sp", _dma(BR[1, cb * 128:(cb + 1) * 128, :], o_[:]), reads=[ro[s]], dma=f"o{s}")
        st.run()

    def gelu_ops(st, eng2, x_ap, tmp_ap, out_ap, rx, rtmp, rout):
        st.op(eng2, lambda e: e.tensor_tensor(tmp_ap, x_ap, x_ap, ALU.mult), reads=[rx], writes=[rtmp])
        st.op("dve", lambda e: e.tensor_scalar(tmp_ap, tmp_ap, 0.044715, 1.0, ALU.mult, ALU.add), reads=[rtmp], writes=[rtmp])
        st.op(eng2, lambda e: e.tensor_tensor(tmp_ap, tmp_ap, x_ap, ALU.mult), reads=[rtmp, rx], writes=[rtmp])
        st.op("act", lambda e: e.activation(tmp_ap, tmp_ap, AF.Sigmoid, scale=1.5957691216057308), reads=[rtmp], writes=[rtmp])
        st.op("dve", lambda e: e.tensor_tensor(out_ap, tmp_ap, x_ap, ALU.mult), reads=[rtmp, rx], writes=[rout])

    def stage_gmlp(l):
        st = Stage(nc, f"gm{l}")
        st.lag = 0
        lng = st.sb([128, DB], F32)
        bsb = st.sb([128, DB], F32)
        wsf = st.sb([128, DB], F32)
        wsb = st.sb([128, 8, 128], BF16)
        r_lng, r_bsb, r_wsf, r_wsb = Res(), Res(), Res(), Res()
        load_bc(st, lng[:], ln_g[l:l + 1, :], r_lng, "lng")
        load_bc(st, bsb[:], b_sp[l:l + 1, :], r_bsb, "bsb")
        st.op("sp", _dma(wsf[:], wsT[l].rearrange("q g p -> q (g p)")), writes=[r_wsf], dma="ws")
        st.op("dve", lambda e: e.tensor_copy(wsb[:].rearrange("q g p -> q (g p)"), wsf[:]), reads=[r_wsf], writes=[r_wsb])
        GD = 3

        def mk(shape, dt, nm):
            return [st.sb(shape, dt, nm) for _ in range(GD)], [Res() for _ in range(GD)]

        vb, rvb = mk([128, DB], BF16, "v")
        ub, rub = mk([128, DB], BF16, "u")
        tvL, r_tvL = mk([128, DB], F32, "tv")
        gvL, r_gvL = mk([128, DB], F32, "gv")
        tuL, r_tuL = mk([128, DB], F32, "tu")
        guL, r_guL = mk([128, DB], F32, "gu")
        sttL, r_sttL = mk([128, 2, 6], F32, "stt")
        mvL, r_mvL = mk([128, 4], F32, "mv")
        vn, rvn = mk([128, DB], BF16, "vn")
        soL, r_soL = mk([128, DB], F32, "so")
        ob, rob = mk([128, DB], BF16, "o")
        pss = [st.ps([128, 4, 128]) for _ in range(4)]
        rps = [Res() for _ in range(4)]

        def ph1(i):
            s = i % GD
            v_, u_ = vb[s], ub[s]
            st.op("sp", _dma(v_[:], VS_tm[i * 128:(i + 1) * 128, :]), writes=[rvb[s]], dma=f"v{s}")
            st.op("sp", _dma(u_[:].rearrange("p (b t) -> p b t", b=8),
                             PROJ[6144:7168, i * 128:(i + 1) * 128].rearrange("(b p) t -> p b t", p=128)),
                  writes=[rub[s]], dma=f"u{s}")
            gelu_ops(st, "pool", v_[:], tvL[s][:], gvL[s][:], rvb[s], r_tvL[s], r_gvL[s])
            gelu_ops(st, "pool", u_[:], tuL[s][:], guL[s][:], rub[s], r_tuL[s], r_guL[s])

        def ph2(i):
            s = i % GD
            gv, stt, mv, vn_ = gvL[s], sttL[s], mvL[s], vn[s]
            r_gv, r_stt, r_mv = r_gvL[s], r_sttL[s], r_mvL[s]
            for hh in range(2):
                st.op("dve", lambda e, hh=hh, stt=stt, gv=gv: e.bn_stats(stt[:, hh, :], gv[:, hh * 512:(hh + 1) * 512]), reads=[r_gv], writes=[r_stt])
            st.op("dve", lambda e, mv=mv, stt=stt: e.bn_aggr(mv[:, 0:2], stt[:].rearrange("p a b -> p (a b)")), reads=[r_stt], writes=[r_mv])
            st.op("act", lambda e, mv=mv: e.activation(mv[:, 2:3], mv[:, 1:2], AF.Sqrt, bias=EPS, scale=1.0), reads=[r_mv], writes=[r_mv])
            st.op("dve", lambda e, mv=mv: e.reciprocal(mv[:, 3:4], mv[:, 2:3]), reads=[r_mv], writes=[r_mv])
            st.op("dve", lambda e, gv=gv, mv=mv: e.tensor_scalar(gv[:], gv[:], mv[:, 0:1], mv[:, 3:4], ALU.subtract, ALU.mult), reads=[r_gv, r_mv], writes=[r_gv])
            st.op("pool", lambda e, vn_=vn_, gv=gv: e.tensor_tensor(vn_[:], gv[:], lng[:], ALU.mult), reads=[r_gv, r_lng], writes=[rvn[s]])
            for half in range(2):
                pp = pss[2 * (i % 2) + half]

                def mm(e, pp=pp, half=half, vn_=vn_):
                    for j in range(4):
                        gi = half * 4 + j
                        ins = e.matmul(pp[:, j, :], vn_[:, gi * 128:(gi + 1) * 128], wsb[:, gi, :], start=True, stop=True)
                    return ins

                st.op("pe", mm, reads=[rvn[s], r_wsb], writes=[rps[2 * (i % 2) + half]])

        def ph3(i):
            s = i % GD
            so, gu, o_ = soL[s], guL[s], ob[s]
            for half in range(2):
                pp = pss[2 * (i % 2) + half]
                st.op("dve", lambda e, pp=pp, half=half, so=so: e.tensor_tensor(so[:, half * 512:(half + 1) * 512], pp[:].rearrange("p a b -> p (a b)"), bsb[:, half * 512:(half + 1) * 512], ALU.add),
                      reads=[rps[2 * (i % 2) + half], r_bsb], writes=[r_soL[s]])
            st.op("pool", lambda e, o_=o_, so=so, gu=gu: e.tensor_tensor(o_[:], so[:], gu[:], ALU.mult), reads=[r_soL[s], r_guL[s]], writes=[rob[s]])
            st.op("sp", _dma(BR[2, :, i * 128:(i + 1) * 128].rearrange("(b p) t -> p b t", p=128),
                             o_[:].rearrange("p (b t) -> p b t", b=8)), reads=[rob[s]], dma=f"o{s}")

        for t in range(NT + 2):
            if t < NT:
                ph1(t)
            if 0 <= t - 1 < NT:
                ph2(t - 1)
            if 0 <= t - 2 < NT:
                ph3(t - 2)
        st.run()

    def stage_merge1(l):
        st = Stage(nc, f"m1{l}")
        st.lag = 2
        wb = [st.sb([128, 3, 8, 512], BF16, "w") for _ in range(2)]
        rwb = [Res() for _ in range(2)]
        br = [st.sb([128, 3, 8, 512], BF16, "br") for _ in range(2)]
        rbr = [Res() for _ in range(2)]
        MD = 4
        gl = [st.sb([128, 3, 512], BF16, "gl") for _ in range(MD)]
        rgl = [Res() for _ in range(MD)]
        sg = [st.sb([128, 3, 512], F32, "sg") for _ in range(MD)]
        rsg = [Res() for _ in range(MD)]
        pss = [st.ps([128, 512]) for _ in range(6)]
        rps = [Res() for _ in range(6)]
        ta = [st.sb([128, 512], F32, "ta") for _ in range(MD)]
        tb_ = [st.sb([128, 512], F32, "tb") for _ in range(MD)]
        rta, rtb = [Res() for _ in range(MD)], [Res() for _ in range(MD)]
        mb = [st.sb([128, 512], BF16, "mb") for _ in range(MD)]
        rmb = [Res() for _ in range(MD)]
        GLv = PROJ[8192:DPROJ, :].rearrange("(i r) t -> r i t", i=3)
        kk = 0
        kb = 0
        def ldw(dg):
            for i in range(3):
                st.op("pool", _dma(wb[dg % 2][:, i], w_branch[l, i, :, dg * 512:(dg + 1) * 512].rearrange("(kc p) d -> p kc d", p=128)),
                      writes=[rwb[dg % 2]], dma=f"w{dg % 2}{i}")

        ldw(0)
        for dg in range(4):
            ws_ = dg % 2
            if dg + 1 < 4:
                ldw(dg + 1)
            for (t0, n) in TGS:
                bs = kb % 2
                kb += 1
                for i in range(3):
                    st.op("sp", _dma(br[bs][:, i, :, :n], BR[i, :, t0:t0 + n].rearrange("(kc p) t -> p kc t", p=128)),
                          writes=[rbr[bs]], dma=f"b{bs}{i}")
                for db in range(4):
                    s = kk % 2
                    sB = kk % MD
                    kk += 1
                    r0 = dg * 512 + db * 128
                    gl_, sg_, ta_, tb2, mb_ = gl[sB], sg[sB], ta[sB], tb_[sB], mb[sB]
                    st.op("sp", _dma(gl_[:, :, :n], GLv[r0:r0 + 128, :, t0:t0 + n]), writes=[rgl[sB]], dma=f"g{sB}")
                    st.op("act", lambda e, gl_=gl_, sg_=sg_, n=n: e.activation(sg_[:, :, :n], gl_[:, :, :n], AF.Sigmoid),
                          reads=[rgl[sB]], writes=[rsg[sB]])
                    for i in range(3):
                        p = pss[3 * s + i]

                        def mm(e, p=p, i=i, ws_=ws_, bs=bs, db=db, n=n):
                            for kc in range(8):
                                ins = e.matmul(p[:, :n], wb[ws_][:, i, kc, db * 128:(db + 1) * 128], br[bs][:, i, kc, :n],
                                               start=(kc == 0), stop=(kc == 7))
                            return ins

                        st.op("pe", mm, reads=[rwb[ws_], rbr[bs]], writes=[rps[3 * s + i]])
                    p0, p1, p2 = pss[3 * s], pss[3 * s + 1], pss[3 * s + 2]
                    st.op("dve", lambda e, ta_=ta_, p0=p0, sg_=sg_, n=n: e.tensor_tensor(ta_[:, :n], p0[:, :n], sg_[:, 0, :n], ALU.mult),
                          reads=[rps[3 * s], rsg[sB]], writes=[rta[sB]])
                    st.op("dve", lambda e, tb2=tb2, p1=p1, sg_=sg_, n=n: e.tensor_tensor(tb2[:, :n], p1[:, :n], sg_[:, 1, :n], ALU.mult),
                          reads=[rps[3 * s + 1], rsg[sB]], writes=[rtb[sB]])
                    st.op("pool", lambda e, ta_=ta_, tb2=tb2, n=n: e.tensor_tensor(ta_[:, :n], ta_[:, :n], tb2[:, :n], ALU.add),
                          reads=[rta[sB], rtb[sB]], writes=[rta[sB]])
                    st.op("dve", lambda e, tb2=tb2, p2=p2, sg_=sg_, n=n: e.tensor_tensor(tb2[:, :n], p2[:, :n], sg_[:, 2, :n], ALU.mult),
                          reads=[rps[3 * s + 2], rsg[sB]], writes=[rtb[sB]])
                    st.op("pool", lambda e, ta_=ta_, tb2=tb2, mb_=mb_, n=n: e.tensor_tensor(mb_[:, :n], ta_[:, :n], tb2[:, :n], ALU.add),
                          reads=[rta[sB], rtb[sB]], writes=[rmb[sB]])
                    st.op("sp", _dma(MERGED[r0:r0 + 128, t0:t0 + n], mb_[:, :n]), reads=[rmb[sB]], dma=f"o{sB}")
        st.run()

    def stage_merge2(l, Xsrc):
        st = Stage(nc, f"m2{l}")
        st.lag = 2
        wo = [st.sb([128, 16, 512], BF16, "w") for _ in range(2)]
        rwo = [Res() for _ in range(2)]
        g1 = st.sb([128, 2, D], F32)
        r_g1 = Res()
        for r in range(2):
            load_bc(st, g1[:, r, :], MOD[r:r + 1, 2 * D:3 * D], r_g1, f"g{r}")
        mT = [st.sb([128, 16, 512], BF16, "m") for _ in range(2)]
        rmT = [Res() for _ in range(2)]
        xt = [st.sb([128, 512], F32, "x") for _ in range(4)]
        rxt = [Res() for _ in range(4)]
        xo = [st.sb([128, 512], F32, "xo") for _ in range(4)]
        rxo = [Res() for _ in range(4)]
        tt = [st.sb([128, 512], F32, "t") for _ in range(4)]
        rtt = [Res() for _ in range(4)]
        pss = [st.ps([128, 512]) for _ in range(4)]
        rps = [Res() for _ in range(4)]
        pk = 0
        mk = 0
        def ldwo(cg):
            st.op("pool", _dma(wo[cg % 2][:], w_out[l, :, cg * 512:(cg + 1) * 512].rearrange("(kc p) d -> p kc d", p=128)),
                  writes=[rwo[cg % 2]], dma=f"w{cg % 2}")

        ldwo(0)
        for cg in range(4):
            wsl = cg % 2
            w_ = wo[wsl]
            if cg + 1 < 4:
                ldwo(cg + 1)
            for gi, (t0, n) in enumerate(TGS):
                ms = mk % 2
                mk += 1
                m_ = mT[ms]
                st.op("sp", _dma(m_[:, :, :n], MERGED[:, t0:t0 + n].rearrange("(kc p) t -> p kc t", p=128)),
                      writes=[rmT[ms]], dma=f"m{ms}")
                for j in range(n // 128):
                    i = t0 // 128 + j
                    r = 0 if i < 16 else 1
                    s = pk % 4
                    p = pss[pk % 4]
                    rp = rps[pk % 4]
                    pk += 1
                    x_, xo_, t_ = xt[s], xo[s], tt[s]
                    st.op("sp", _dma(x_[:], Xsrc[i * 128:(i + 1) * 128, cg * 512:(cg + 1) * 512]), writes=[rxt[s]], dma=f"x{s}")

                    def mm(e, p=p, m_=m_, w_=w_, j=j):
                        for kc in range(16):
                            ins = e.matmul(p[:], m_[:, kc, j * 128:(j + 1) * 128], w_[:, kc, :],
                                           start=(kc == 0), stop=(kc == 15))
                        return ins

                    st.op("pe", mm, reads=[rmT[ms], rwo[wsl]], writes=[rp])
                    st.op("dve", lambda e, p=p, t_=t_, r=r, cg=cg: e.tensor_tensor(t_[:], p[:], g1[:, r, cg * 512:(cg + 1) * 512], ALU.mult),
                          reads=[rp, r_g1], writes=[rtt[s]])
                    st.op("pool", lambda e, t_=t_, x_=x_, xo_=xo_: e.tensor_tensor(xo_[:], t_[:], x_[:], ALU.add),
                          reads=[rtt[s], rxt[s]], writes=[rxo[s]])
                    st.op("sp", _dma(XA[i * 128:(i + 1) * 128, cg * 512:(cg + 1) * 512], xo_[:]), reads=[rxo[s]], dma=f"o{s}")
        st.run()

    def stage_route(l):
        st = Stage(nc, f"rt{l}")
        affT = st.sb([16, T], F32)
        r_affT = Res()
        pst = [st.ps([16, 512]) for _ in range(2)]
        rpst = [Res() for _ in range(2)]
        r_aff = Res()
        for q in range(5):
            p = pst[q % 2]
            ntile = 4 if q < 4 else 2

            def tr(e, p=p, q=q, ntile=ntile):
                for j in range(ntile):
                    ins = e.transpose(p[:, j * 128:(j + 1) * 128], AFF[:, q * 4 + j, :], ident_f[:])
                return ins

            st.op("pe", tr, reads=[r_aff], writes=[rpst[q % 2]])
            st.op("act", lambda e, p=p, q=q, ntile=ntile: e.copy(affT[:, q * 512:q * 512 + ntile * 128], p[:, :ntile * 128]),
                  reads=[rpst[q % 2]], writes=[r_affT])
        work = st.sb([16, SEQ], F32)
        workc = st.sb([16, CTX], F32)
        m8 = st.sb([16, 8], F32)
        m8c = st.sb([16, 8], F32)
        r_work, r_workc, r_m8, r_m8c = Res(), Res(), Res(), Res()
        st.op("dve", lambda e: e.tensor_copy(work[:], affT[:, :SEQ]), reads=[r_affT], writes=[r_work])
        st.op("pool", lambda e: e.tensor_copy(workc[:], affT[:, SEQ:]), reads=[r_affT], writes=[r_workc])
        for it in range(CAP // 8):
            st.op("dve", lambda e: e.max(m8[:], work[:]), reads=[r_work], writes=[r_m8])
            if it < CAP // 8 - 1:
                st.op("dve", lambda e: e.match_replace(work[:], m8[:], work[:], -1.0), reads=[r_work, r_m8], writes=[r_work])
        for it in range(CAPC // 8):
            st.op("dve", lambda e: e.max(m8c[:], workc[:]), reads=[r_workc], writes=[r_m8c])
            if it < CAPC // 8 - 1:
                st.op("dve", lambda e: e.match_replace(workc[:], m8c[:], workc[:], -1.0), reads=[r_workc, r_m8c], writes=[r_workc])
        maskT = st.sb([16, T], F32)
        gateT = st.sb([16, T], F32)
        posT = st.sb([16, T], F32)
        onesr = st.sb([16, SEQ], F32)
        r_mask, r_gate, r_pos, r_ones = Res(), Res(), Res(), Res()
        st.op("pool", lambda e: e.memset(onesr[:], 1.0), writes=[r_ones])
        st.op("dve", lambda e: e.tensor_scalar(maskT[:, :SEQ], affT[:, :SEQ], m8[:, 7:8], None, ALU.is_ge), reads=[r_affT, r_m8], writes=[r_mask])
        st.op("dve", lambda e: e.tensor_scalar(maskT[:, SEQ:], affT[:, SEQ:], m8c[:, 7:8], None, ALU.is_ge), reads=[r_affT, r_m8c], writes=[r_mask])
        st.op("dve", lambda e: e.tensor_tensor(gateT[:], maskT[:], affT[:], ALU.mult), reads=[r_mask, r_affT], writes=[r_gate])
        st.op("dve", lambda e: e.tensor_tensor_scan(posT[:, :SEQ], onesr[:, :SEQ], maskT[:, :SEQ], 0.0, ALU.mult, ALU.add),
              reads=[r_mask, r_ones], writes=[r_pos])
        st.op("dve", lambda e: e.tensor_tensor_scan(posT[:, SEQ:], onesr[:, :CTX], maskT[:, SEQ:], 0.0, ALU.mult, ALU.add),
              reads=[r_mask, r_ones], writes=[r_pos])
        st.op("dve", lambda e: e.tensor_tensor(posT[:], posT[:], maskT[:], ALU.subtract), reads=[r_pos, r_mask], writes=[r_pos])
        ptm = [st.ps([128, 2, NE]) for _ in range(2)]
        rptm = [Res() for _ in range(2)]
        r_P, r_M = Res(), Res()
        for i in range(NT):
            s = i % 2
            p = ptm[s]

            def tr2(e, p=p, i=i):
                e.transpose(p[:, 0, :], posT[:, i * 128:(i + 1) * 128], ident_f[:16, :16])
                return e.transpose(p[:, 1, :], maskT[:, i * 128:(i + 1) * 128], ident_f[:16, :16])

            st.op("pe", tr2, reads=[r_pos, r_mask], writes=[rptm[s]])
            st.op("act", lambda e, p=p, i=i: e.copy(POS[:, i, :], p[:, 0, :]), reads=[rptm[s]], writes=[r_P])
            st.op("dve", lambda e, p=p, i=i: e.tensor_copy(MSK[:, i, :], p[:, 1, :]), reads=[rptm[s]], writes=[r_M])
        sel = st.sb([16, 16, 128], F32)
        iop = st.sb([128, 2], F32)
        r_sel, r_iop = Res(), Res()
        st.op("sp", _dma(sel[:], sel_d), writes=[r_sel], dma="sel")
        selb = st.sb([16, 16, 128], BF16)
        posb = st.sb([16, T], BF16)
        gateb = st.sb([16, T], BF16)
        r_selb, r_posb, r_gateb = Res(), Res(), Res()
        st.op("act", lambda e: e.copy(selb[:].rearrange("a b c -> a (b c)"), sel[:].rearrange("a b c -> a (b c)")), reads=[r_sel], writes=[r_selb])
        st.op("act", lambda e: e.copy(posb[:], posT[:]), reads=[r_pos], writes=[r_posb])
        st.op("act", lambda e: e.copy(gateb[:], gateT[:]), reads=[r_gate], writes=[r_gateb])
        st.op("sp", _dma(iop[:], iop_d), writes=[r_iop], dma="iop")
        pbp = [st.ps([128, 512]) for _ in range(2)]
        pbg = [st.ps([128, 512]) for _ in range(2)]
        rpbp, rpbg = [Res() for _ in range(2)], [Res() for _ in range(2)]
        gbs = [st.sb([128, 512], F32, "gb") for _ in range(2)]
        rgbs = [Res() for _ in range(2)]
        gto = [st.sb([128, 2, 512], BF16, "gt") for _ in range(2)]
        rgto = [Res() for _ in range(2)]
        k = 0
        for ex in range(NE):
            for (t0, n) in TGS:
                s = k % 2
                k += 1
                pp, pg, gb_, go_ = pbp[s], pbg[s], gbs[s], gto[s]
                st.op("pe", lambda e, pp=pp, ex=ex, t0=t0, n=n: e.matmul(pp[:, :n], selb[:, ex, :], posb[:, t0:t0 + n], start=True, stop=True),
                      reads=[r_selb, r_posb], writes=[rpbp[s]])
                st.op("pe", lambda e, pg=pg, ex=ex, t0=t0, n=n: e.matmul(pg[:, :n], selb[:, ex, :], gateb[:, t0:t0 + n], start=True, stop=True),
                      reads=[r_selb, r_gateb], writes=[rpbg[s]])
                st.op("act", lambda e, pg=pg, gb_=gb_, n=n: e.copy(gb_[:, :n], pg[:, :n]), reads=[rpbg[s]], writes=[rgbs[s]])
                for sti in range(2):
                    st.op("dve", lambda e, pp=pp, gb_=gb_, go_=go_, sti=sti, n=n: e.scalar_tensor_tensor(go_[:, sti, :n], pp[:, :n], iop[:, sti:sti + 1], gb_[:, :n], ALU.is_equal, ALU.mult),
                          reads=[rpbp[s], rgbs[s], r_iop], writes=[rgto[s]])
                for sti in range(2):
                    st.op("sp", _dma(GT[t0 // 128:(t0 + n) // 128, :, ex, sti, :].rearrange("i p t -> p i t"),
                                     go_[:, sti, :n].rearrange("p (i t) -> p i t", t=128)), reads=[rgto[s]], dma=f"o{s}{sti}")
        st.run()

    def stage_gather(l):
        st = Stage(nc, f"ga{l}")
        h2 = st.sb([128, NT, D], BF16)
        r_h2 = Res()
        for q in range(6):
            st.op("sp", _dma(h2[:, q * 3:(q + 1) * 3, :], H2[q * 384:(q + 1) * 384, :].rearrange("(i p) d -> p i d", p=128)),
                  writes=[r_h2], dma=f"h{q}")
        io = st.sb([128, 256], F32)
        r_io = Res()
        st.op("sp", _dma(io[:], iota_d), writes=[r_io], dma="io")
        S = [st.sb([128, 16, 256], BF16, "S") for _ in range(3)]
        Sc = [st.sb([128, 2, 32], BF16, "Sc") for _ in range(3)]
        rS = [Res() for _ in range(3)]
        xs = [st.sb([128, 16, NS], BF16, "xs") for _ in range(3)]
        rxs = [Res() for _ in range(3)]
        pss = [st.ps([128, 512]) for _ in range(4)]
        rps = [Res() for _ in range(4)]
        r_P, r_M = Res(), Res()
        pk = 0
        for ex in range(NE):
            s = ex % 3
            S_, Sc_, xs_ = S[s], Sc[s], xs[s]
            for tt in range(16):
                eng = "dve"
                st.op(eng, lambda e, S_=S_, tt=tt, ex=ex: e.tensor_scalar(S_[:, tt, :], io[:], POS[:, tt, ex:ex + 1], MSK[:, tt, ex:ex + 1], ALU.is_equal, ALU.mult),
                      reads=[r_io, r_P, r_M], writes=[rS[s]])
            for j in range(2):
                st.op("dve", lambda e, Sc_=Sc_, j=j, ex=ex: e.tensor_scalar(Sc_[:, j, :], io[:, :32], POS[:, 16 + j, ex:ex + 1], MSK[:, 16 + j, ex:ex + 1], ALU.is_equal, ALU.mult),
                      reads=[r_io, r_P, r_M], writes=[rS[s]])
            for fb in range(16):
                p = pss[pk % 4]
                rp = rps[pk % 4]
                pk += 1

                def mm(e, p=p, S_=S_, Sc_=Sc_, fb=fb):
                    for tt in range(16):
                        e.matmul(p[:, :256], h2[:, tt, fb * 128:(fb + 1) * 128], S_[:, tt, :], start=(tt == 0), stop=(tt == 15))
                    for j in range(2):
                        ins = e.matmul(p[:, 256:NS], h2[:, 16 + j, fb * 128:(fb + 1) * 128], Sc_[:, j, :], start=(j == 0), stop=(j == 1))
                    return ins

                st.op("pe", mm, reads=[r_h2, rS[s]], writes=[rp])
                if fb % 2 == 0:
                    st.op("act", lambda e, p=p, xs_=xs_, fb=fb: e.copy(xs_[:, fb, :], p[:, :NS]), reads=[rp], writes=[rxs[s]])
                else:
                    st.op("dve", lambda e, p=p, xs_=xs_, fb=fb: e.tensor_copy(xs_[:, fb, :], p[:, :NS]), reads=[rp], writes=[rxs[s]])
            st.op("sp", _dma(XS[ex].rearrange("(kc p) s -> p kc s", p=128), xs_[:]), reads=[rxs[s]], dma=f"o{s}")
        st.run()

    def stage_experts(l):
        st = Stage(nc, f"ex{l}")
        st.lag = 1
        NW = 4
        ws = [st.sb([128, 16, 512], BF16, "w") for _ in range(NW)]
        rw = [Res() for _ in range(NW)]
        xs = [st.sb([128, 16, NS], BF16, "xs") for _ in range(2)]
        rxs = [Res() for _ in range(2)]
        aT = [st.sb([128, 16, NS], BF16, "aT") for _ in range(2)]
        raT = [Res() for _ in range(2)]
        yb = [st.sb([128, 3, D], BF16, "yb") for _ in range(2)]
        ryb = [Res() for _ in range(2)]
        sg = [st.sb([128, NS], F32, "sg") for _ in range(2)]
        rsg = [Res() for _ in range(2)]
        pss = [st.ps([128, 512]) for _ in range(8)]
        rps = [Res() for _ in range(8)]
        pieces = []
        for ex in range(NE):
            for fg in range(4):
                pieces.append((w_eg, ex, fg))
                pieces.append((w_eu, ex, fg))
            for dg in range(4):
                pieces.append((w_ed, ex, dg))
        NP = len(pieces)

        def ld(pi):
            wt, ex, cg = pieces[pi]
            st.op("pool", _dma(ws[pi % NW][:], wt[l, ex, :, cg * 512:(cg + 1) * 512].rearrange("(kc p) c -> p kc c", p=128)),
                  writes=[rw[pi % NW]], dma=f"w{pi % NW}")

        for pi in range(NW):
            ld(pi)
        pi = 0
        pk = 0
        sk = 0
        for ex in range(NE):
            s = ex % 2
            xs_, aT_, yb_ = xs[s], aT[s], yb[s]
            st.op("sp", _dma(xs_[:], XS[ex].rearrange("(kc p) s -> p kc s", p=128)), writes=[rxs[s]], dma=f"x{s}")
            for fg in range(4):
                pi += 2
                wg_, wu_ = ws[(pi - 2) % NW], ws[(pi - 1) % NW]
                rwg, rwu = rw[(pi - 2) % NW], rw[(pi - 1) % NW]
                for fb in range(4):
                    pg, rpg = pss[pk % 8], rps[pk % 8]
                    pu, rpu = pss[(pk + 1) % 8], rps[(pk + 1) % 8]
                    pk += 2
                    sg_, rsg_ = sg[sk % 2], rsg[sk % 2]
                    sk += 1

                    def mm(e, p=pg, w=wg_, fb=fb, xs_=xs_):
                        for kc in range(16):
                            ins = e.matmul(p[:, :NS], w[:, kc, fb * 128:(fb + 1) * 128], xs_[:, kc, :], start=(kc == 0), stop=(kc == 15))
                        return ins

                    def mm2(e, p=pu, w=wu_, fb=fb, xs_=xs_):
                        for kc in range(16):
                            ins = e.matmul(p[:, :NS], w[:, kc, fb * 128:(fb + 1) * 128], xs_[:, kc, :], start=(kc == 0), stop=(kc == 15))
                        return ins

                    st.op("pe", mm, reads=[rwg, rxs[s]], writes=[rpg])
                    st.op("pe", mm2, reads=[rwu, rxs[s]], writes=[rpu])
                    st.op("act", lambda e, pg=pg, sg_=sg_: e.activation(sg_[:], pg[:, :NS], AF.Silu), reads=[rpg], writes=[rsg_])
                    st.op("dve", lambda e, pu=pu, sg_=sg_, aT_=aT_, f=fg * 4 + fb: e.tensor_tensor(aT_[:, f, :], sg_[:], pu[:, :NS], ALU.mult),
                          reads=[rpu, rsg_], writes=[raT[s]])
                for nxt in (pi - 2 + NW, pi - 1 + NW):
                    if nxt < NP:
                        ld(nxt)
            for dg in range(4):
                pi += 1
                wd_, rwd = ws[(pi - 1) % NW], rw[(pi - 1) % NW]
                for sti, (s0, m) in enumerate(((0, 128), (128, 128), (256, 32))):
                    p, rp = pss[pk % 8], rps[pk % 8]
                    pk += 1

                    def mm3(e, p=p, w=wd_, s0=s0, m=m, aT_=aT_):
                        for fc in range(16):
                            ins = e.matmul(p[:m, :], aT_[:, fc, s0:s0 + m], w[:, fc, :], start=(fc == 0), stop=(fc == 15))
                        return ins

                    st.op("pe", mm3, reads=[rwd, raT[s]], writes=[rp])
                    if sti % 2 == 0:
                        st.op("act", lambda e, p=p, yb_=yb_, sti=sti, m=m, dg=dg: e.copy(yb_[:m, sti, dg * 512:(dg + 1) * 512], p[:m, :]),
                              reads=[rp], writes=[ryb[s]])
                    else:
                        st.op("dve", lambda e, p=p, yb_=yb_, sti=sti, m=m, dg=dg: e.tensor_copy(yb_[:m, sti, dg * 512:(dg + 1) * 512], p[:m, :]),
                              reads=[rp], writes=[ryb[s]])
                if pi - 1 + NW < NP:
                    ld(pi - 1 + NW)
            st.op("sp", _dma(Y[ex].rearrange("(s p) d -> p s d", p=128), yb_[:, 0:2, :]), reads=[ryb[s]], dma=f"y{s}")
            st.op("sp", _dma(YC[ex], yb_[:32, 2, :]), reads=[ryb[s]], dma=f"c{s}")
        st.run()

    def stage_combine(l, Xdst):
        st = Stage(nc, f"cb{l}")
        st.lag = 4
        g2 = st.sb([128, 2, D], F32)
        r_g2 = Res()
        for r in range(2):
            load_bc(st, g2[:, r, :], MOD[r:r + 1, 5 * D:6 * D], r_g2, f"g{r}")
        YfL = [st.sb([128, NE, 2, 512], BF16, "Yf") for _ in range(2)]
        YCfL = [st.sb([32, NE, 512], BF16, "YCf") for _ in range(2)]
        r_YfL, r_YCfL = [Res() for _ in range(2)], [Res() for _ in range(2)]

        def ldy(fg):
            b_ = fg % 2
            for q in range(4):
                st.op("sp", _dma(YfL[b_][:, q * 4:(q + 1) * 4], Y[q * 4:(q + 1) * 4, :, fg * 512:(fg + 1) * 512].rearrange("e (s p) d -> p e s d", p=128)),
                      writes=[r_YfL[b_]], dma=f"y{b_}{q}")
            st.op("sp", _dma(YCfL[b_][:], YC[:, :, fg * 512:(fg + 1) * 512].rearrange("e p d -> p e d")), writes=[r_YCfL[b_]], dma=f"yc{b_}")

        ldy(0)
        CD = 4
        gtt = [st.sb([128, NE, 2, 128], BF16, "gt") for _ in range(CD)]
        rgt = [Res() for _ in range(CD)]
        xa = [st.sb([128, 512], F32, "xa") for _ in range(CD)]
        rxa = [Res() for _ in range(CD)]
        tt = [st.sb([128, 512], F32, "t") for _ in range(CD)]
        rtt = [Res() for _ in range(CD)]
        xo = [st.sb([128, 512], F32, "xo") for _ in range(CD)]
        rxo = [Res() for _ in range(CD)]
        pss = [st.ps([128, 512]) for _ in range(CD)]
        rps = [Res() for _ in range(CD)]
        k = 0
        for fg in range(4):
            c0 = fg * 512
            Yf, YCf, r_Yf, r_YCf = YfL[fg % 2], YCfL[fg % 2], r_YfL[fg % 2], r_YCfL[fg % 2]
            if fg + 1 < 4:
                ldy(fg + 1)
            for i in range(NT):
                s = k % CD
                k += 1
                r = 0 if i < 16 else 1
                g_, xa_, t_, xo_, p = gtt[s], xa[s], tt[s], xo[s], pss[s]
                st.op("sp", _dma(g_[:], GT[i]), writes=[rgt[s]], dma=f"g{s}")
                st.op("sp", _dma(xa_[:], XA[i * 128:(i + 1) * 128, c0:c0 + 512]), writes=[rxa[s]], dma=f"x{s}")
                if i < 16:
                    def mm(e, p=p, g_=g_, Yf=Yf):
                        for j in range(32):
                            ex, sti = j // 2, j % 2
                            ins = e.matmul(p[:], g_[:, ex, sti, :], Yf[:, ex, sti, :], start=(j == 0), stop=(j == 31))
                        return ins
                    st.op("pe", mm, reads=[rgt[s], r_Yf], writes=[rps[s]])
                else:
                    def mmc(e, p=p, g_=g_, YCf=YCf):
                        for ex in range(NE):
                            ins = e.matmul(p[:], g_[:32, ex, 0, :], YCf[:, ex, :], start=(ex == 0), stop=(ex == NE - 1))
                        return ins
                    st.op("pe", mmc, reads=[rgt[s], r_YCf], writes=[rps[s]])
                st.op("dve", lambda e, p=p, t_=t_, r=r, c0=c0: e.tensor_tensor(t_[:], p[:], g2[:, r, c0:c0 + 512], ALU.mult),
                      reads=[rps[s], r_g2], writes=[rtt[s]])
                st.op("pool", lambda e, t_=t_, xa_=xa_, xo_=xo_: e.tensor_tensor(xo_[:], t_[:], xa_[:], ALU.add),
                      reads=[rtt[s], rxa[s]], writes=[rxo[s]])
                st.op("sp", _dma(Xdst[i * 128:(i + 1) * 128, c0:c0 + 512], xo_[:]), reads=[rxo[s]], dma=f"o{s}")
        st.run()

    def stage_final(Xsrc):
        st = Stage(nc, "fin")
        st.lag = 1
        gb = st.sb([128, D], F32)
        r_gb = Res()
        load_bc(st, gb[:], final_g[0:1, :], r_gb, "g")
        xt = [st.sb([128, D], F32, "x") for _ in range(2)]
        rx = [Res() for _ in range(2)]
        junk = st.sb([128, D], BF16)
        r_junk = Res()
        ss = [st.sb([128, 4], F32, "ss") for _ in range(2)]
        rss = [Res() for _ in range(2)]
        ot = [st.sb([128, D], F32, "o") for _ in range(2)]
        rot = [Res() for _ in range(2)]
        for i in range(16):
            s = i % 2
            x_, ss_, o_ = xt[s], ss[s], ot[s]
            st.op("sp", _dma(x_[:], Xsrc[i * 128:(i + 1) * 128, :]), writes=[rx[s]], dma=f"x{s}")
            st.op("act", lambda e, x_=x_, ss_=ss_: e.activation(junk[:], x_[:], AF.Square, accum_out=ss_[:, 0:1]),
                  reads=[rx[s]], writes=[r_junk, rss[s]])
            st.op("act", lambda e, ss_=ss_: e.activation(ss_[:, 1:2], ss_[:, 0:1], AF.Sqrt, bias=EPS, scale=1.0 / D),
                  reads=[rss[s]], writes=[rss[s]])
            st.op("dve", lambda e, ss_=ss_: e.reciprocal(ss_[:, 2:3], ss_[:, 1:2]), reads=[rss[s]], writes=[rss[s]])
            st.op("dve", lambda e, x_=x_, ss_=ss_, o_=o_: e.scalar_tensor_tensor(o_[:], x_[:], ss_[:, 2:3], gb[:], ALU.mult, ALU.mult),
                  reads=[rx[s], rss[s], r_gb], writes=[rot[s]])
            st.op("sp", _dma(out[i * 128:(i + 1) * 128, :], o_[:]), reads=[rot[s]], dma=f"o{s}")
        st.run()

    stop = getattr(build, "stop_after", None)

    def done(tag):
        return stop is not None and tag == stop

    Xcur = xin
    finished = False
    for l in range(nlayers):
        stage_mod(l)
        if done(f"mod{l}"):
            finished = True
            break
        with nc.sbuf_tensor(f"hT{l}", [128, 16, T], BF16) as hT:
            r_hT = Res()
            stage_norm(l, 1, Xcur, hT, r_hT)
            if done(f"n1{l}"):
                finished = True
                break
            stage_proj(l, hT, r_hT)
        if done(f"pj{l}"):
            finished = True
            break
        stage_rope(l)
        if done(f"rp{l}"):
            finished = True
            break
        stage_attn(l)
        stage_conv(l)
        stage_gmlp(l)
        if done(f"br{l}"):
            finished = True
            break
        stage_merge1(l)
        if done(f"m1{l}"):
            finished = True
            break
        stage_merge2(l, Xcur)
        if done(f"m2{l}"):
            finished = True
            break
        stage_norm(l, 2, XA, None, None)
        stage_route(l)
        if done(f"rt{l}"):
            finished = True
            break
        stage_gather(l)
        stage_experts(l)
        stage_combine(l, XB)
        if done(f"cb{l}"):
            finished = True
            break
        Xcur = XB
    if not finished:
        stage_final(XB)
    top.close()
    return nc


def _consts():
    ident = np.eye(128, dtype=np.float32)
    pm = np.zeros((128, 128), np.float32)
    for f in range(128):
        if (f % 64) < 32:
            pm[f + 32, f] = -1.0
        else:
            pm[f - 32, f] = 1.0
    pos = np.arange(SEQ)
    rc = np.stack([pos // 64, pos % 64], axis=-1).astype(np.float32)
    inv = (np.float32(10000.0) ** (-np.arange(0, 64, 2, dtype=np.float32) / np.float32(64))).astype(np.float32)
    ang = rc[:, :, None] * inv
    cos = np.zeros((128, SEQ), np.float32)
    sin = np.zeros((128, SEQ), np.float32)
    for ax in range(2):
        for half in range(2):
            f0 = ax * 64 + half * 32
            cos[f0:f0 + 32, :] = np.cos(ang[:, ax, :]).T
            sin[f0:f0 + 32, :] = np.sin(ang[:, ax, :]).T
    selT = np.zeros((16, 16, 128), np.float32)
    for e in range(16):
        selT[e, e, :] = 1.0
    iota_s = np.broadcast_to(np.arange(256, dtype=np.float32)[None, :], (128, 256)).copy()
    iop = np.stack([np.arange(128), np.arange(128) + 128], axis=1).astype(np.float32)
    return dict(ident=ident, pm=pm, cos=cos, sin=sin, selT=selT, iota_s=iota_s, iop=iop)


def _rpb_index():
    gs = [0, 1, 5, 14, 15]
    dr = np.zeros((5, 128, 640), np.int64)
    dc = np.zeros((5, 128, 640), np.int64)
    ok = np.zeros((5, 128, 640), bool)
    ql = np.arange(128)
    kl = np.arange(640)
    for ci, g in enumerate(gs):
        tb = min(max(g - 2, 0), 11)
        r = 2 * g + ql // 64
        qc = ql % 64
        rs = np.clip(r - 4, 0, 24)
        ws = np.clip(qc - 8, 0, 48)
        kr = 2 * tb + kl // 64
        kc = kl % 64
        rowok = (kr[None, :] >= rs[:, None]) & (kr[None, :] < rs[:, None] + 8)
        colok = (kc[None, :] >= ws[:, None]) & (kc[None, :] < ws[:, None] + 16)
        ok[ci] = rowok & colok
        dr[ci] = np.clip(kr[None, :] - r[:, None] + 7, 0, 14)
        dc[ci] = np.clip(kc[None, :] - qc[:, None] + 15, 0, 30)
    return dr, dc, ok


def _host_inputs(inputs):
    f = lambda a: np.ascontiguousarray(np.asarray(a, dtype=np.float32))
    x, c, ctx, c_ctx = f(inputs["x"]), f(inputs["c"]), f(inputs["ctx"]), f(inputs["c_ctx"])
    shared = {k: f(inputs[k]) for k in ["w_mod", "b_mod", "norm1_g", "w_in", "gmlp_ln_g", "w_branch", "w_out", "norm2_g",
                                        "w_router", "b_router", "w_e_gate", "w_e_up", "w_e_down"]}
    shared["final_g"] = f(inputs["final_g"]).reshape(1, D)
    shared["b_spatial"] = f(inputs["b_spatial"]).reshape(L, DB)
    shared["wsT"] = np.ascontiguousarray(f(inputs["w_spatial"]).transpose(0, 3, 1, 2))
    shared["conv_wT"] = np.ascontiguousarray(f(inputs["conv_w"]).reshape(L, 3, 8, 128).transpose(0, 3, 2, 1))
    rpb = f(inputs["na_rpb"])
    dr, dc, ok = _rpb_index()
    tab = np.zeros((L, 8, 5, 128, 896), np.float32)
    gathered = rpb[:, :, dr, dc]
    tab[..., :640] = np.where(ok[None, None], gathered, np.float32(NEG))
    shared["rpb_tab"] = np.ascontiguousarray(tab.transpose(0, 1, 3, 2, 4))
    shared.update(_consts())
    in_maps = []
    for b in range(x.shape[0]):
        m = dict(shared)
        m["xin"] = np.ascontiguousarray(np.concatenate([x[b], ctx[b]], axis=0))
        c2 = np.stack([c[b], c_ctx], axis=0)
        m["c2T"] = np.ascontiguousarray(c2.reshape(2, 16, 128).transpose(2, 1, 0))
        in_maps.append(m)
    return in_maps


def kernel(**inputs):
    in_maps = _host_inputs(inputs)
    nc = bass.Bass("TRN2", target_bir_lowering=False)
    build(nc)
    res = run_bass_kernel_spmd(nc, in_maps, core_ids=list(range(len(in_maps))))
    outs = [np.asarray(r["out"], dtype=np.float32) for r in res.results]
    return np.stack(outs, axis=0)
```
